# Optimizing a Trainium2 kernel written in Bass

```python
import jax, jax.numpy as jnp
from jax import lax
import numpy as np

D_MODEL = 1024
BATCH = 8
SEQ = 4096
DEPTH = 2

RMS_EPS = 1e-5
N_BRANCHES = 3
GMLP_WIDTH = D_MODEL
GMLP_HEADS = 8
GMLP_HEAD_DIM = GMLP_WIDTH // GMLP_HEADS
CHUNK = 128
POOL_WIDTH = D_MODEL
POOL_WINDOWS = (2, 4, 8, 16)
POOL_GROUPS = len(POOL_WINDOWS)
POOL_GROUP_DIM = POOL_WIDTH // POOL_GROUPS
CONV_WIDTH = D_MODEL
CONV_K = 3
SPLIT_POINTS = (
    GMLP_WIDTH,
    2 * GMLP_WIDTH,
    2 * GMLP_WIDTH + POOL_WIDTH,
    2 * GMLP_WIDTH + POOL_WIDTH + CONV_WIDTH,
    2 * GMLP_WIDTH + POOL_WIDTH + 2 * CONV_WIDTH,
    2 * GMLP_WIDTH + POOL_WIDTH + 3 * CONV_WIDTH,
)
IN_COLS = 2 * GMLP_WIDTH + POOL_WIDTH + 3 * CONV_WIDTH + N_BRANCHES * D_MODEL
N_EXPERTS = 32
TOP_K = 4
D_FF = D_MODEL
SWIGLU_LIMIT = 7.0
SWIGLU_ALPHA = 1.702
MOE_BLOCK = 128

kernel_name = "hybrid_gmlp_pool_conv_moe_adaln"


def rms_norm(x, g):
    xf = x.astype(jnp.float32)
    y = xf * lax.rsqrt(jnp.mean(xf * xf, axis=-1, keepdims=True) + RMS_EPS)
    return (y * g.astype(jnp.float32)).astype(x.dtype)


def gmlp_branch(u, v, norm_g, ws, bs, w_proj):
    bsz, seq, _ = v.shape
    u = jax.nn.gelu(u)
    v = rms_norm(jax.nn.gelu(v), norm_g)
    vb = v.reshape(bsz, seq // CHUNK, CHUNK, GMLP_HEADS, GMLP_HEAD_DIM)
    causal = jnp.tril(jnp.ones((CHUNK, CHUNK), dtype=bool))
    ws_m = jnp.where(causal[None], ws, jnp.zeros_like(ws))
    s = jnp.einsum('hts,bcshd->bcthd', ws_m, vb) + jnp.transpose(bs)[:, :, None]
    return (u * s.reshape(bsz, seq, GMLP_WIDTH)) @ w_proj


def pool_branch(p, pool_w, pool_scale):
    bsz, seq, _ = p.shape
    pf = p.astype(jnp.float32)
    cs = jnp.cumsum(pf, axis=1)
    pos = jnp.arange(seq)
    diffs = []
    for gi, w in enumerate(POOL_WINDOWS):
        sl = slice(gi * POOL_GROUP_DIM, (gi + 1) * POOL_GROUP_DIM)
        cs_g = cs[:, :, sl]
        lag = jnp.pad(cs_g, ((0, 0), (w, 0), (0, 0)))[:, :seq]
        cnt = jnp.minimum(pos + 1, w).astype(jnp.float32)[None, :, None]
        diffs.append((cs_g - lag) / cnt - pf[:, :, sl])
    d = jnp.stack(diffs, axis=2).astype(p.dtype)
    y = jnp.einsum('bsgc,gce->bsge', d, pool_w).reshape(bsz, seq, POOL_WIDTH)
    return y * pool_scale


def conv_branch(xc, b_gate, c_gate, conv_w, w_proj):
    z = c_gate * xc
    zp = jnp.pad(z, ((0, 0), (CONV_K - 1, 0), (0, 0)))
    seq = z.shape[1]
    conv = sum(conv_w[k] * zp[:, k:k + seq] for k in range(CONV_K))
    return (b_gate * conv) @ w_proj


def hybrid_mixer(h, w_in, gmlp_norm_g, gmlp_ws, gmlp_bs, w_proj_a, pool_w, pool_scale,
                 conv_w, w_proj_c, w_out):
    z = jnp.einsum('bsd,dn->bsn', h, w_in)
    u, v, p, xc, b_gate, c_gate, gates = jnp.split(z, SPLIT_POINTS, axis=-1)
    ya = gmlp_branch(u, v, gmlp_norm_g, gmlp_ws, gmlp_bs, w_proj_a)
    yb = pool_branch(p, pool_w, pool_scale)
    yc = conv_branch(xc, b_gate, c_gate, conv_w, w_proj_c)
    ga, gb, gc = jnp.split(jax.nn.sigmoid(gates), N_BRANCHES, axis=-1)
    return (ga * ya + gb * yb + gc * yc) @ w_out


def moe_ffn(h, router_w, router_b, w_gu, b_gu, w_down, b_down):
    bsz, seq, d = h.shape
    n_tok = bsz * seq
    n_assign = n_tok * TOP_K
    n_rows = n_assign + N_EXPERTS * MOE_BLOCK
    n_blocks = n_rows // MOE_BLOCK
    t = h.reshape(n_tok, d)
    logits = (t @ router_w + router_b).astype(jnp.float32)
    top_vals, top_idx = lax.top_k(logits, TOP_K)
    gate_w = jax.nn.softmax(top_vals, axis=-1)
    flat_e = top_idx.reshape(-1).astype(jnp.int32)
    flat_tok = jnp.repeat(jnp.arange(n_tok, dtype=jnp.int32), TOP_K)
    order = jnp.argsort(flat_e)
    sorted_e = flat_e[order]
    sorted_tok = flat_tok[order]
    w_sorted = gate_w.reshape(-1)[order].astype(h.dtype)
    counts = jnp.bincount(flat_e, length=N_EXPERTS).astype(jnp.int32)
    padded = ((counts + MOE_BLOCK - 1) // MOE_BLOCK) * MOE_BLOCK
    pad_end = jnp.cumsum(padded)
    pad_start = pad_end - padded
    grp_start = jnp.cumsum(counts) - counts
    rank = jnp.arange(n_assign, dtype=jnp.int32) - grp_start[sorted_e]
    dest = pad_start[sorted_e] + rank
    row_tok = jnp.zeros((n_rows,), jnp.int32).at[dest].set(sorted_tok)
    xs = t[row_tok].reshape(n_blocks, MOE_BLOCK, d)
    block_start = jnp.arange(n_blocks, dtype=jnp.int32) * MOE_BLOCK
    block_e = jnp.minimum(jnp.searchsorted(pad_end, block_start, side='right'), N_EXPERTS - 1)

    def expert_block(args):
        xb, e = args
        gu = xb @ w_gu[e] + b_gu[e]
        gate, up = jnp.split(gu, 2, axis=-1)
        gate = jnp.minimum(gate, SWIGLU_LIMIT)
        up = jnp.clip(up, -SWIGLU_LIMIT, SWIGLU_LIMIT)
        glu = gate * jax.nn.sigmoid(SWIGLU_ALPHA * gate)
        return ((up + 1.0) * glu) @ w_down[e] + b_down[e]

    ys = lax.map(expert_block, (xs, block_e)).reshape(n_rows, d)
    y_assign = ys[dest] * w_sorted[:, None]
    out = jax.ops.segment_sum(y_assign, sorted_tok, num_segments=n_tok)
    return out.reshape(bsz, seq, d)


def setup_inputs(seed: int = 0) -> dict:
    key = jax.random.key(seed)
    ks = jax.random.split(key, 24)
    f32 = jnp.float32
    L, D = DEPTH, D_MODEL

    def nrm(k, shape, s):
        return jax.random.normal(k, shape, f32) * s

    return {
        "x": nrm(ks[0], (BATCH, SEQ, D), 1.0),
        "c": nrm(ks[1], (BATCH, D), 1.0),
        "norm1_g": 1.0 + nrm(ks[2], (L, D), 0.05),
        "ada_w": nrm(ks[3], (L, D, 6 * D), 0.2 * D ** -0.5),
        "ada_b": nrm(ks[4], (L, 6 * D), 0.02),
        "w_in": nrm(ks[5], (L, D, IN_COLS), D ** -0.5),
        "gmlp_norm_g": 1.0 + nrm(ks[6], (L, GMLP_WIDTH), 0.05),
        "gmlp_ws": nrm(ks[7], (L, GMLP_HEADS, CHUNK, CHUNK), CHUNK ** -0.5),
        "gmlp_bs": 1.0 + nrm(ks[8], (L, GMLP_HEADS, CHUNK), 0.05),
        "w_proj_a": nrm(ks[9], (L, GMLP_WIDTH, D), GMLP_WIDTH ** -0.5),
        "pool_w": nrm(ks[10], (L, POOL_GROUPS, POOL_GROUP_DIM, POOL_GROUP_DIM), POOL_GROUP_DIM ** -0.5),
        "pool_scale": 1.0 + nrm(ks[11], (L, POOL_WIDTH), 0.05),
        "conv_w": nrm(ks[12], (L, CONV_K, CONV_WIDTH), CONV_K ** -0.5),
        "w_proj_c": nrm(ks[13], (L, CONV_WIDTH, D), CONV_WIDTH ** -0.5),
        "w_out": nrm(ks[14], (L, D, D), D ** -0.5),
        "norm2_g": 1.0 + nrm(ks[15], (L, D), 0.05),
        "router_w": nrm(ks[16], (L, D, N_EXPERTS), D ** -0.5),
        "router_b": nrm(ks[17], (L, N_EXPERTS), 0.01),
        "exp_w_gu": nrm(ks[18], (L, N_EXPERTS, D, 2 * D_FF), D ** -0.5),
        "exp_b_gu": nrm(ks[19], (L, N_EXPERTS, 2 * D_FF), 0.02),
        "exp_w_down": nrm(ks[20], (L, N_EXPERTS, D_FF, D), D_FF ** -0.5),
        "exp_b_down": nrm(ks[21], (L, N_EXPERTS, D), 0.02),
        "final_g": 1.0 + nrm(ks[22], (D,), 0.05),
    }


def reference(x, c, norm1_g, ada_w, ada_b, w_in, gmlp_norm_g, gmlp_ws, gmlp_bs, w_proj_a,
              pool_w, pool_scale, conv_w, w_proj_c, w_out, norm2_g, router_w, router_b,
              exp_w_gu, exp_b_gu, exp_w_down, exp_b_down, final_g):
    cond = jax.nn.silu(c)
    for l in range(DEPTH):
        mods = cond @ ada_w[l] + ada_b[l]
        sh1, sc1, g1, sh2, sc2, g2 = [m[:, None, :] for m in jnp.split(mods, 6, axis=-1)]
        h = rms_norm(x, norm1_g[l]) * (1.0 + sc1) + sh1
        x = x + g1 * hybrid_mixer(h, w_in[l], gmlp_norm_g[l], gmlp_ws[l], gmlp_bs[l], w_proj_a[l],
                                  pool_w[l], pool_scale[l], conv_w[l], w_proj_c[l], w_out[l])
        h = rms_norm(x, norm2_g[l]) * (1.0 + sc2) + sh2
        x = x + g2 * moe_ffn(h, router_w[l], router_b[l], exp_w_gu[l], exp_b_gu[l],
                             exp_w_down[l], exp_b_down[l])
    return rms_norm(x, final_g)
```

```python
from contextlib import ExitStack
import numpy as np
import concourse.bass as bass
import concourse.mybir as mybir
from concourse.bass_utils import run_bass_kernel_spmd

F32 = mybir.dt.float32
BF16 = mybir.dt.bfloat16
U8 = mybir.dt.uint8
I32 = mybir.dt.int32
AF = mybir.ActivationFunctionType
ALU = mybir.AluOpType

D = 1024
SEQ = 4096
NE = 32
EPS = 1e-5
POOL_WINDOWS = (2, 4, 8, 16)
ARENA_BYTES = 210944
CAP = 1536
SPARSE = True


class Sched:
    ENGS = ("pe", "act", "dve", "pool", "sp")

    def __init__(self, nc):
        self.nc = nc
        self.ops = []
        self.lastw = {}
        self.readers = {}
        self.stream = {e: [] for e in self.ENGS}
        self.dma_keys = {}

    def op(self, eng, fn, reads=(), writes=(), dma=None, extra=()):
        i = len(self.ops)
        deps = set(extra)
        for r in reads:
            w = self.lastw.get(r)
            if w is not None:
                deps.add(w)
        for w_ in writes:
            w = self.lastw.get(w_)
            if w is not None:
                deps.add(w)
            for rd in self.readers.get(w_, ()):
                deps.add(rd)
        deps.discard(i)
        for r in reads:
            self.readers.setdefault(r, []).append(i)
        for w_ in writes:
            self.lastw[w_] = i
            self.readers[w_] = []
        o = dict(i=i, eng=eng, fn=fn, dma=dma, deps=deps, pos=len(self.stream[eng]),
                 inc=False, waits=[])
        self.ops.append(o)
        self.stream[eng].append(i)
        if dma is not None:
            self.dma_keys.setdefault(dma, []).append(i)
            o["dcount"] = 16 * len(self.dma_keys[dma])
        return i

    def barrier(self):
        lasts = []
        for e in self.ENGS:
            for i in reversed(self.stream[e]):
                if self.ops[i]["dma"] is None and self.ops[i]["fn"] is not None:
                    lasts.append(i)
                    break
        for k, lst in self.dma_keys.items():
            lasts.append(lst[-1])
        for e in self.ENGS:
            self.op(e, None, extra=lasts)
        self.lastw = {}
        self.readers = {}

    def finalize(self):
        ops = self.ops
        need = {}
        for o in ops:
            lst = []
            for p in o["deps"]:
                P = ops[p]
                if P["fn"] is None:
                    continue
                if P["dma"] is not None:
                    lst.append(p)
                elif P["eng"] == o["eng"] and o["dma"] is None:
                    if o["eng"] == "pe":
                        continue
                    if o["pos"] - P["pos"] <= 3:
                        lst.append(p)
                        P["inc"] = True
                else:
                    lst.append(p)
                    P["inc"] = True
            need[o["i"]] = lst
        for e in self.ENGS:
            c = 0
            for i in self.stream[e]:
                o = ops[i]
                if o["dma"] is None and o["inc"]:
                    c += 1
                    o["count"] = c
        waited = {e: {} for e in self.ENGS}
        for e in self.ENGS:
            for i in self.stream[e]:
                o = ops[i]
                ws = {}
                for p in need[i]:
                    P = ops[p]
                    if P["dma"] is not None:
                        s, v = "D_" + P["dma"], P["dcount"]
                    else:
                        s, v = "E_" + P["eng"], P["count"]
                    if v > ws.get(s, 0):
                        ws[s] = v
                for s, v in ws.items():
                    if waited[e].get(s, 0) >= v:
                        continue
                    waited[e][s] = v
                    o["waits"].append((s, v))

    def emit(self):
        self.finalize()
        nc = self.nc
        names = ["E_" + e for e in self.ENGS] + ["D_" + k for k in self.dma_keys]
        with ExitStack() as es:
            sems = {n: es.enter_context(nc.semaphore(n)) for n in names}
            block = es.enter_context(nc.Block())
            ops = self.ops

            def make(en):
                def body(e):
                    for i in self.stream[en]:
                        o = ops[i]
                        for s, v in o["waits"]:
                            e.wait_ge(sems[s], v)
                        if o["fn"] is None:
                            continue
                        ins = o["fn"](e)
                        if o["dma"] is not None:
                            ins.then_inc(sems["D_" + o["dma"]], 16)
                        elif o["inc"]:
                            ins.then_inc(sems["E_" + en], 1)
                return body

            block.tensor(make("pe"))
            block.scalar(make("act"))
            block.vector(make("dve"))
            block.gpsimd(make("pool"))
            block.sync(make("sp"))


class DummySched:
    def op(self, *a, **k):
        return 0

    def barrier(self):
        pass


class Ring:
    def __init__(self, name, aps, names=None):
        self.name, self.aps, self.i, self.names = name, aps, 0, names

    def next(self):
        k = self.i % len(self.aps)
        self.i += 1
        return self.aps[k], (self.names[k] if self.names else "%s%d" % (self.name, k))


class WStream:
    def __init__(self, S, slots, plan=None):
        self.S, self.slots, self.n = S, slots, len(slots)
        self.plan = plan
        self.rec = []
        self.issued = 0
        self.cons = 0
        self.released = set()

    def _pump(self):
        if self.plan is None:
            return
        while self.issued < len(self.plan) and self.issued < self.cons + self.n:
            k = self.issued
            if k >= self.n and (k - self.n) not in self.released:
                break
            slot = k % self.n
            dst = self.slots[slot]
            s_ = self.plan[k]
            self.S.op("pool", lambda e, dst=dst, s_=s_: e.dma_start(out=dst, in_=s_),
                      writes=["W%d" % slot], dma="W%d" % slot)
            self.issued += 1

    def get(self, src):
        i = self.cons
        self.cons += 1
        if self.plan is None:
            self.rec.append(src)
        else:
            self._pump()
            assert self.issued > i, "weight piece not issued (missing release?)"
        return self.slots[i % self.n], "W%d" % (i % self.n), i

    def release(self, *hs):
        for h in hs:
            self.released.add(h)
        self._pump()


class Arena:
    def __init__(self, ar):
        self.ar, self.cur = ar, 0

    def alloc(self, shape, dt, parts=128):
        esz = 4 if dt in (F32, I32) else 2
        n = 1
        for s in shape[1:]:
            n *= s
        nb = n * esz
        nb_al = (nb + 63) // 64 * 64
        off = self.cur
        self.cur += nb_al
        assert self.cur <= ARENA_BYTES, ("SBUF arena overflow", self.cur)
        v = self.ar[0:parts, off:off + nb].bitcast(dt)
        if len(shape) == 3:
            v = v.rearrange("p (a b) -> p a b", a=shape[1])
        elif len(shape) == 4:
            v = v.rearrange("p (a b c) -> p a b c", a=shape[1], b=shape[2])
        return v


def build(layers=(0, 1), final=True, n_tiles=8, n_super=4, n_exp=NE, do_mixer=True, do_moe=True, debug=False):
    nc = bass.Bass("TRN2", target_bir_lowering=False)

    def din(name, shape, dt=F32):
        return nc.dram_tensor(name, list(shape), dt, kind="ExternalInput").ap()

    x_in = din("x", [SEQ, D])
    cT = din("cT", [128, 8])
    ident_d = din("ident", [128, 128])
    triu_d = din("triu", [128, 128])
    rc_d = din("rc", [128, 4, 16])
    ustr_d = din("ustr", [128, 128])
    ebase_d = din("ebase", [128, NE])
    tokid_d = din("tokid", [128, 32], I32)
    finalg_d = din("finalgB", [128, D])
    L = {}
    for l in layers:
        L[l] = dict(
            ada_w=din("ada_w%d" % l, [D, 6 * D]),
            ada_bP=din("ada_bP%d" % l, [128, 48]),
            adab_g1B=din("adab_g1B%d" % l, [128, D]),
            adab_g2B=din("adab_g2B%d" % l, [128, D]),
            n1gP=din("n1gP%d" % l, [128, 8]),
            n2gP=din("n2gP%d" % l, [128, 8]),
            w_in=din("w_in%d" % l, [D, 9 * D]),
            gnormB=din("gnormB%d" % l, [128, D]),
            ws=din("ws%d" % l, [8, 128, 128]),
            bsB=din("bsB%d" % l, [128, D]),
            w_proj_a=din("w_proj_a%d" % l, [D, D]),
            pool_w=din("pool_w%d" % l, [4, 256, 256]),
            pscP=din("pscP%d" % l, [128, 8]),
            convP=din("convP%d" % l, [128, 3, 8]),
            w_proj_c=din("w_proj_c%d" % l, [D, D]),
            w_out=din("w_out%d" % l, [D, D]),
            router_w=din("router_w%d" % l, [D, NE]),
            rbB=din("rbB%d" % l, [128, NE]),
            w_gu=din("w_gu%d" % l, [NE, D, 2 * D]),
            bguP=din("bguP%d" % l, [128, NE, 16]),
            w_down=din("w_down%d" % l, [NE, D, D]),
            b_down=din("b_down%d" % l, [NE, D]),
        )
    out_d = nc.dram_tensor("out", [SEQ, D], F32, kind="ExternalOutput").ap()
    xs_d = nc.dram_tensor("xs_scratch", [SEQ, D], F32, kind="Internal").ap()
    ys_d = nc.dram_tensor("ys_scratch", [NE * CAP, D], F32, kind="Internal").ap()
    tokslot_d = nc.dram_tensor("tokslot_scratch", [NE * CAP, 1], I32, kind="Internal").ap()

    dbg_t = {}

    def dump(S, name, ap, reads):
        if not debug:
            return
        if name not in dbg_t:
            dbg_t[name] = nc.dram_tensor("dbg_" + name, list(ap.shape), ap.dtype, kind="ExternalOutput").ap()
        d = dbg_t[name]
        S.op("sp", lambda e: e.dma_start(out=d, in_=ap), reads=reads, writes=["dbg_" + name], dma="dbg_" + name)

    def kview(w):
        return w.rearrange("(kc p) n -> p kc n", p=128)

    with ExitStack() as es:
        arena_t = es.enter_context(nc.sbuf_tensor("arena", [128, ARENA_BYTES], U8))
        banks = [es.enter_context(nc.psum_tensor("pb%d" % i, [128, 512], F32)) for i in range(6)]
        tpb = es.enter_context(nc.psum_tensor("tpb", [128, 8, 128], BF16))
        tpb2 = es.enter_context(nc.psum_tensor("tpb2", [128, 8, 128], BF16))

        def program(S, plan):
            A = Arena(arena_t)
            pool_regs = {}
            wslots = [A.alloc([128, 8, 512], BF16) for _ in range(6)]
            W = WStream(S, wslots, plan)
            identf = A.alloc([128, 128], F32)
            identb = A.alloc([128, 128], BF16)
            triu = A.alloc([128, 128], F32)
            rc = A.alloc([128, 4, 16], F32)
            cTf = A.alloc([128, 8], F32)
            condf = A.alloc([128, 8], F32)
            condb = A.alloc([128, 8], BF16)
            condB = A.alloc([128, 8, 128], BF16)
            modsP = A.alloc([128, 48], F32)
            adabP = A.alloc([128, 48], F32)
            n1g = A.alloc([128, 8], F32)
            n2g = A.alloc([128, 8], F32)
            A1 = A.alloc([128, 8], F32)
            A2 = A.alloc([128, 8], F32)
            GB = A.alloc([128, D], F32)
            abrow = A.alloc([128, D], F32)
            sscol = A.alloc([128, 64], F32)
            rscol = A.alloc([128, 64], F32)
            statc = [0]
            mark_common = A.cur

            PB = Ring("P", [b[:] for b in banks])

            def stat_col():
                k = statc[0] % 64
                statc[0] += 1
                return k

            def mm_group(out_ap, out_res, pairs, reads):
                n = len(pairs)
                for i, (lt, rh) in enumerate(pairs):
                    S.op("pe", lambda e, o=out_ap, lt=lt, rh=rh, st=(i == 0), sp_=(i == n - 1):
                         e.matmul(o, lhsT=lt, rhs=rh, start=st, stop=sp_),
                         reads=reads, writes=[out_res])

            S.op("sp", lambda e: e.dma_start(out=identf, in_=ident_d), writes=["identf"], dma="identf")
            S.op("sp", lambda e: e.dma_start(out=triu, in_=triu_d), writes=["triu"], dma="triu")
            S.op("sp", lambda e: e.dma_start(out=rc, in_=rc_d), writes=["rc"], dma="rc")
            S.op("sp", lambda e: e.dma_start(out=cTf, in_=cT), writes=["cTf"], dma="cTf")
            S.op("dve", lambda e: e.tensor_copy(out=identb, in_=identf), reads=["identf"], writes=["identb"])
            S.op("act", lambda e: e.activation(out=condf, in_=cTf, func=AF.Silu), reads=["cTf"], writes=["condf"])
            S.op("dve", lambda e: e.tensor_copy(out=condb, in_=condf), reads=["condf"], writes=["condb"])
            for kc in range(8):
                S.op("dve", lambda e, kc=kc: e.tensor_copy(out=condB[:, kc, :], in_=condf[:, kc:kc + 1].to_broadcast([128, 128])),
                     reads=["condf"], writes=["condB"])

            def rms_rstd(src_ap, src_res, junk_ap, junk_res):
                k = stat_col()
                ssa, rsa = sscol[:, k:k + 1], rscol[:, k:k + 1]
                sres, rres = "ss%d" % k, "rs%d" % k
                S.op("dve", lambda e: e.memset(ssa, 0.0), writes=[sres])
                S.op("act", lambda e: e.activation(out=junk_ap, in_=src_ap, func=AF.Square, accum_out=ssa),
                     reads=[src_res], writes=[junk_res, sres])
                S.op("dve", lambda e: e.tensor_scalar(out=rsa, in0=ssa, scalar1=1.0 / D, scalar2=EPS, op0=ALU.mult, op1=ALU.add),
                     reads=[sres], writes=[rres])
                S.op("act", lambda e: e.activation(out=rsa, in_=rsa, func=AF.Sqrt), reads=[rres], writes=[rres])
                S.op("dve", lambda e: e.reciprocal(out=rsa, in_=rsa), reads=[rres], writes=[rres])
                return rsa, rres

            def mods_rows(l, qs):
                src_b = L[l]["adab_g1B"] if qs[0] == 4 else L[l]["adab_g2B"]
                S.op("sp", lambda e: e.dma_start(out=abrow, in_=src_b), writes=["abrow"], dma="abrow")
                for hi, q in enumerate(qs):
                    wp, wres, wh = W.get(kview(L[l]["ada_w"])[:, :, q * 512:(q + 1) * 512])
                    pb, pres = PB.next()
                    mm_group(pb, pres, [(condB[:, kc, :], wp[:, kc, :]) for kc in range(8)], [wres, "condB"])
                    W.release(wh)
                    S.op("dve", lambda e, pb=pb, hi=hi: e.tensor_tensor(out=GB[:, hi * 512:(hi + 1) * 512], in0=pb,
                                                                          in1=abrow[:, hi * 512:(hi + 1) * 512], op=ALU.add),
                         reads=[pres, "abrow"], writes=["GB"])

            def layer_prologue(l):
                P = L[l]
                S.op("sp", lambda e: e.dma_start(out=adabP, in_=P["ada_bP"]), writes=["adabP"], dma="adabP")
                S.op("sp", lambda e: e.dma_start(out=n1g, in_=P["n1gP"]), writes=["n1g"], dma="n1g")
                S.op("sp", lambda e: e.dma_start(out=n2g, in_=P["n2gP"]), writes=["n2g"], dma="n2g")
                mp, mres = PB.next()
                for q in range(12):
                    wp, wres, wh = W.get(kview(P["ada_w"])[:, :, q * 512:(q + 1) * 512])
                    for jj in range(4):
                        j = 4 * q + jj
                        mm_group(mp[:, j:j + 1], mres,
                                 [(wp[:, kc, jj * 128:(jj + 1) * 128], condb[:, kc:kc + 1]) for kc in range(8)],
                                 [wres, "condb"])
                    W.release(wh)
                S.op("dve", lambda e: e.tensor_tensor(out=modsP, in0=mp[:, 0:48], in1=adabP, op=ALU.add),
                     reads=[mres, "adabP"], writes=["modsP"])
                S.op("dve", lambda e: e.scalar_tensor_tensor(out=A1, in0=modsP[:, 8:16], scalar=1.0, in1=n1g, op0=ALU.add, op1=ALU.mult),
                     reads=["modsP", "n1g"], writes=["A1"])
                S.op("dve", lambda e: e.scalar_tensor_tensor(out=A2, in0=modsP[:, 32:40], scalar=1.0, in1=n2g, op0=ALU.add, op1=ALU.mult),
                     reads=["modsP", "n2g"], writes=["A2"])

            def norm_transpose(xa, xres, xn_ring, junk, Ascale, Ares, shoff, hT, hres, col0):
                rsa, rres = rms_rstd(xa, xres, junk, "junk")
                xn, xnres = xn_ring.next()
                S.op("act", lambda e: e.activation(out=xn, in_=xa, func=AF.Identity, scale=rsa),
                     reads=[xres, rres], writes=[xnres])
                for kc in range(8):
                    S.op("pe", lambda e, kc=kc: e.transpose(out=tpb[:, kc, :], in_=xn[:, kc * 128:(kc + 1) * 128], identity=identb),
                         reads=[xnres, "identb"], writes=["tpb"])
                for kc in range(8):
                    dst = hT[:, kc, col0:col0 + 128]
                    if kc % 2 == 0:
                        S.op("dve", lambda e, kc=kc, dst=dst: e.tensor_scalar(out=dst, in0=tpb[:, kc, :], scalar1=Ascale[:, kc:kc + 1],
                                                                               scalar2=modsP[:, shoff + kc:shoff + kc + 1], op0=ALU.mult, op1=ALU.add),
                             reads=["tpb", Ares, "modsP"], writes=[hres])
                    else:
                        S.op("act", lambda e, kc=kc, dst=dst: e.activation(out=dst, in_=tpb[:, kc, :], func=AF.Identity,
                                                                            bias=modsP[:, shoff + kc:shoff + kc + 1], scale=Ascale[:, kc:kc + 1]),
                             reads=["tpb", Ares, "modsP"], writes=[hres])

            def mixer_phase(l, src_d, src_name, mdst_d):
                P = L[l]
                A.cur = mark_common
                xsub = Ring("xsub", [A.alloc([128, D], F32) for _ in range(5)])
                xnr = Ring("xn", [A.alloc([128, D], BF16) for _ in range(2)])
                junk = A.alloc([128, D], BF16)
                hT = A.alloc([128, 8, 512], BF16)
                gvr = Ring("gv", [A.alloc([128, D], F32) for _ in range(2)])
                vn = A.alloc([128, 4, D], BF16)
                gu = A.alloc([128, 8, 512], BF16)
                a8 = Ring("a8", [A.alloc([128, 8, 512], BF16) for _ in range(2)])
                mix = A.alloc([128, 8, 512], F32)
                T2 = Ring("T2", [A.alloc([128, 528], F32) for _ in range(9)])
                sgr = Ring("sg", [A.alloc([128, 512], BF16) for _ in range(3)])
                phalo = A.alloc([128, 8, 16], F32)
                zhalo = A.alloc([128, 8, 2], F32)
                poolW = A.alloc([128, 8, 256], BF16)
                WsT = A.alloc([128, 8, 128], BF16)
                wsb = A.alloc([128, 8, 128], BF16)
                gnormB = A.alloc([128, D], F32)
                bsb = A.alloc([128, 8, 128], F32)
                pscP = A.alloc([128, 8], F32)
                convP = A.alloc([128, 3, 8], F32)
                w_in_v = kview(P["w_in"])

                mods_rows(l, (4, 5))
                wsraw, wsres = gvr.next()
                wsraw3 = wsraw.rearrange("p (h s) -> p h s", h=8)
                S.op("sp", lambda e: e.dma_start(out=wsraw3, in_=P["ws"].rearrange("h t s -> t h s")), writes=[wsres], dma="wsraw")
                S.op("sp", lambda e: e.dma_start(out=gnormB, in_=P["gnormB"]), writes=["gnormB"], dma="gnormB")
                S.op("sp", lambda e: e.dma_start(out=bsb.rearrange("p h t -> p (h t)"), in_=P["bsB"]), writes=["bsb"], dma="bsb")
                S.op("sp", lambda e: e.dma_start(out=pscP, in_=P["pscP"]), writes=["pscP"], dma="pscP")
                S.op("sp", lambda e: e.dma_start(out=convP, in_=P["convP"]), writes=["convP"], dma="convP")
                S.op("pool", lambda e: e.dma_start(out=poolW, in_=P["pool_w"].rearrange("g (k p) n -> p (g k) n", p=128)),
                     writes=["poolW"], dma="poolW")
                S.op("dve", lambda e: e.tensor_copy(out=wsb, in_=wsraw3), reads=[wsres], writes=["wsb"])
                for h in range(8):
                    S.op("pe", lambda e, h=h: e.transpose(out=tpb[:, h, :], in_=wsb[:, h, :], identity=identb),
                         reads=["wsb", "identb"], writes=["tpb"])
                S.op("dve", lambda e: e.tensor_tensor(out=WsT, in0=tpb[:], in1=triu[:, None, :].to_broadcast([128, 8, 128]), op=ALU.mult),
                     reads=["tpb", "triu"], writes=["WsT"])
                S.op("dve", lambda e: e.memset(phalo, 0.0), writes=["phalo"])
                S.op("dve", lambda e: e.memset(zhalo, 0.0), writes=["zhalo"])

                def zchunk(wp, wres, jj):
                    pb, pres = PB.next()
                    mm_group(pb, pres, [(wp[:, kc, jj * 128:(jj + 1) * 128], hT[:, kc, :]) for kc in range(8)], [wres, "hT"])
                    return pb, pres

                for tt in range(n_tiles):
                    t0 = tt * 512
                    xs_list = []
                    for s in range(4):
                        xa, xres = xsub.next()
                        r0 = t0 + s * 128
                        dres = "%s%d" % (src_name, r0 // 128)
                        S.op("sp", lambda e, xa=xa, r0=r0: e.dma_start(out=xa, in_=src_d[r0:r0 + 128, :]),
                             reads=[dres], writes=[xres], dma=xres)
                        xs_list.append((xa, xres))
                        norm_transpose(xa, xres, xnr, junk, A1, "A1", 0, hT, "hT", s * 128)
                    if tt == 0:
                        dump(S, "modsP", modsP, ["modsP"]); dump(S, "GB", GB, ["GB"]); dump(S, "hT", hT, ["hT"])
                        dump(S, "WsT", WsT, ["WsT"]); dump(S, "A1", A1, ["A1"])
                    wv = [W.get(w_in_v[:, :, (2 + hf) * 512:(3 + hf) * 512]) for hf in range(2)]
                    for s in range(4):
                        gv, gvres = gvr.next()
                        for hf in range(2):
                            pb, pres = PB.next()
                            mm_group(pb, pres, [(hT[:, kc, s * 128:(s + 1) * 128], wv[hf][0][:, kc, :]) for kc in range(8)],
                                     [wv[hf][1], "hT"])
                            S.op("act", lambda e, pb=pb, gv=gv, hf=hf: e.activation(out=gv[:, hf * 512:(hf + 1) * 512], in_=pb, func=AF.Gelu_apprx_tanh),
                                 reads=[pres], writes=[gvres])
                        rsa, rres = rms_rstd(gv, gvres, junk, "junk")
                        S.op("dve", lambda e, gv=gv, rsa=rsa, s=s: e.scalar_tensor_tensor(out=vn[:, s, :], in0=gv, scalar=rsa, in1=gnormB,
                                                                                            op0=ALU.mult, op1=ALU.mult),
                             reads=[gvres, rres, "gnormB"], writes=["vn%d" % s])
                    W.release(wv[0][2], wv[1][2])
                    for hf in range(2):
                        wp, wres, wh = W.get(w_in_v[:, :, hf * 512:(hf + 1) * 512])
                        for jj in range(4):
                            j = hf * 4 + jj
                            pb, pres = zchunk(wp, wres, jj)
                            S.op("act", lambda e, pb=pb, j=j: e.activation(out=gu[:, j, :], in_=pb, func=AF.Gelu_apprx_tanh),
                                 reads=[pres], writes=["gu"])
                        W.release(wh)
                    if tt == 0:
                        dump(S, "vn", vn, ["vn0", "vn1", "vn2", "vn3"]); dump(S, "gu", gu, ["gu"])
                    ain, ares = a8.next()
                    for s in range(4):
                        for hg in range(2):
                            pb, pres = PB.next()
                            for hh in range(4):
                                h = hg * 4 + hh
                                S.op("pe", lambda e, pb=pb, hh=hh, h=h, s=s: e.matmul(pb[:, hh * 128:(hh + 1) * 128], lhsT=vn[:, s, h * 128:(h + 1) * 128],
                                                                                       rhs=WsT[:, h, :], start=True, stop=True),
                                     reads=["vn%d" % s, "WsT"], writes=[pres])
                            t1, t1res = T2.next()
                            S.op("dve", lambda e, pb=pb, t1=t1, hg=hg: e.tensor_tensor(out=t1[:, 0:512], in0=pb,
                                                                                        in1=bsb[:, hg * 4:(hg + 1) * 4, :].rearrange("p h t -> p (h t)"), op=ALU.add),
                                 reads=[pres, "bsb"], writes=[t1res])
                            S.op("dve", lambda e, t1=t1, hg=hg, s=s, ain=ain: e.tensor_tensor(
                                out=ain[:, hg * 4:(hg + 1) * 4, s * 128:(s + 1) * 128],
                                in0=t1[:, 0:512].rearrange("p (h t) -> p h t", h=4),
                                in1=gu[:, hg * 4:(hg + 1) * 4, s * 128:(s + 1) * 128], op=ALU.mult),
                                 reads=[t1res, "gu"], writes=[ares])
                    for n in range(8):
                        if n % 4 == 0:
                            if n:
                                W.release(wa1[2], wg1[2])
                            wa1 = W.get(kview(P["w_proj_a"])[:, :, (n // 4) * 512:(n // 4 + 1) * 512])
                            wg1 = W.get(w_in_v[:, :, (12 + n // 4) * 512:(13 + n // 4) * 512])
                            wa = {n // 4: wa1}
                            wg = {n // 4: wg1}
                        gp, gres = zchunk(wg[n // 4][0], wg[n // 4][1], n % 4)
                        sg, sgres = sgr.next()
                        S.op("act", lambda e, gp=gp, sg=sg: e.activation(out=sg, in_=gp, func=AF.Sigmoid), reads=[gres], writes=[sgres])
                        pb, pres = PB.next()
                        mm_group(pb, pres, [(wa[n // 4][0][:, kc, (n % 4) * 128:(n % 4 + 1) * 128], ain[:, kc, :]) for kc in range(8)],
                                 [wa[n // 4][1], ares])
                        S.op("dve", lambda e, pb=pb, sg=sg, n=n: e.tensor_tensor(out=mix[:, n, :], in0=pb, in1=sg, op=ALU.mult),
                             reads=[pres, sgres], writes=["mix%d" % n])
                    W.release(wa1[2], wg1[2])
                    if tt == 0:
                        dump(S, "ain", ain, [ares]); dump(S, "mixA", mix, ["mix%d" % n for n in range(8)])
                    dT, dres = a8.next()
                    for hf in range(2):
                        wp, wres, wph = W.get(w_in_v[:, :, (4 + hf) * 512:(5 + hf) * 512])
                        for jj in range(4):
                            j = hf * 4 + jj
                            wdw = POOL_WINDOWS[j // 2]
                            pp, ppres = zchunk(wp, wres, jj)
                            pbuf, pbres = T2.next()
                            S.op("dve", lambda e, pbuf=pbuf, j=j: e.tensor_copy(out=pbuf[:, 0:16], in_=phalo[:, j, :]), reads=["phalo"], writes=[pbres])
                            S.op("act", lambda e, pbuf=pbuf, pp=pp: e.activation(out=pbuf[:, 16:528], in_=pp, func=AF.Copy), reads=[ppres], writes=[pbres])
                            S.op("dve", lambda e, pbuf=pbuf, j=j: e.tensor_copy(out=phalo[:, j, :], in_=pbuf[:, 512:528]), reads=[pbres], writes=["phalo"])
                            cur, cres = pbuf, pbres
                            sh, lo = 1, 0
                            while sh < wdw:
                                nx, nres = T2.next()
                                S.op("pool", lambda e, nx=nx, cur=cur, sh=sh, lo=lo: e.tensor_tensor(out=nx[:, lo + sh:528], in0=cur[:, lo + sh:528],
                                                                                                      in1=cur[:, lo:528 - sh], op=ALU.add),
                                     reads=[cres], writes=[nres])
                                cur, cres = nx, nres
                                lo += sh
                                sh *= 2
                            S.op("dve", lambda e, cur=cur, pbuf=pbuf, j=j, wdw=wdw, dT=dT: e.scalar_tensor_tensor(
                                out=dT[:, j, :], in0=cur[:, 16:528], scalar=1.0 / wdw, in1=pbuf[:, 16:528], op0=ALU.mult, op1=ALU.subtract),
                                 reads=[cres, pbres], writes=[dres])
                            if tt == 0:
                                fx, fres = T2.next()
                                S.op("dve", lambda e, fx=fx, cur=cur, j=j: e.tensor_tensor(out=fx[:, 0:16], in0=cur[:, 16:32], in1=rc[:, j // 2, :], op=ALU.mult),
                                     reads=[cres, "rc"], writes=[fres])
                                S.op("dve", lambda e, fx=fx, pbuf=pbuf, j=j, dT=dT: e.tensor_tensor(out=dT[:, j, 0:16], in0=fx[:, 0:16], in1=pbuf[:, 16:32], op=ALU.subtract),
                                     reads=[fres, pbres, dres], writes=[dres])
                        W.release(wph)
                    for n in range(8):
                        g = n // 2
                        if n % 4 == 0:
                            if n:
                                W.release(wgb1[2])
                            wgb1 = W.get(w_in_v[:, :, (14 + n // 4) * 512:(15 + n // 4) * 512])
                            wgb = {n // 4: wgb1}
                        gp, gres = zchunk(wgb[n // 4][0], wgb[n // 4][1], n % 4)
                        sg, sgres = sgr.next()
                        S.op("act", lambda e, gp=gp, sg=sg: e.activation(out=sg, in_=gp, func=AF.Sigmoid), reads=[gres], writes=[sgres])
                        pb, pres = PB.next()
                        mm_group(pb, pres, [(poolW[:, 2 * g + k2, (n % 2) * 128:(n % 2 + 1) * 128], dT[:, 2 * g + k2, :]) for k2 in range(2)],
                                 ["poolW", dres])
                        tm, tres = T2.next()
                        S.op("dve", lambda e, pb=pb, sg=sg, n=n, tm=tm: e.scalar_tensor_tensor(out=tm[:, 0:512], in0=pb, scalar=pscP[:, n:n + 1], in1=sg,
                                                                                                op0=ALU.mult, op1=ALU.mult),
                             reads=[pres, sgres, "pscP"], writes=[tres])
                        S.op("pool", lambda e, tm=tm, n=n: e.tensor_tensor(out=mix[:, n, :], in0=mix[:, n, :], in1=tm[:, 0:512], op=ALU.add),
                             reads=[tres, "mix%d" % n], writes=["mix%d" % n])
                    W.release(wgb1[2])
                    if tt == 0:
                        dump(S, "dT", dT, [dres]); dump(S, "mixB", mix, ["mix%d" % n for n in range(8)])
                    bc, bcres = a8.next()
                    for hf in range(2):
                        wx, wxres, wxh = W.get(w_in_v[:, :, (6 + hf) * 512:(7 + hf) * 512])
                        wb, wbres, wbh = W.get(w_in_v[:, :, (8 + hf) * 512:(9 + hf) * 512])
                        wc, wcres, wch = W.get(w_in_v[:, :, (10 + hf) * 512:(11 + hf) * 512])
                        for jj in range(4):
                            j = hf * 4 + jj
                            cp, cpres = zchunk(wc, wcres, jj)
                            cs, csres = T2.next()
                            S.op("act", lambda e, cs=cs, cp=cp: e.activation(out=cs[:, 0:512], in_=cp, func=AF.Copy), reads=[cpres], writes=[csres])
                            xp, xpres = zchunk(wx, wxres, jj)
                            zc, zcres = T2.next()
                            S.op("dve", lambda e, zc=zc, j=j: e.tensor_copy(out=zc[:, 0:2], in_=zhalo[:, j, :]), reads=["zhalo"], writes=[zcres])
                            S.op("dve", lambda e, zc=zc, xp=xp, cs=cs: e.tensor_tensor(out=zc[:, 2:514], in0=xp, in1=cs[:, 0:512], op=ALU.mult),
                                 reads=[xpres, csres], writes=[zcres])
                            S.op("dve", lambda e, zc=zc, j=j: e.tensor_copy(out=zhalo[:, j, :], in_=zc[:, 512:514]), reads=[zcres], writes=["zhalo"])
                            ca, cares = T2.next()
                            S.op("dve", lambda e, ca=ca, zc=zc, j=j: e.tensor_scalar(out=ca[:, 0:512], in0=zc[:, 2:514], scalar1=convP[:, 2, j:j + 1], scalar2=None, op0=ALU.mult),
                                 reads=[zcres, "convP"], writes=[cares])
                            S.op("dve", lambda e, ca=ca, zc=zc, j=j: e.scalar_tensor_tensor(out=ca[:, 0:512], in0=zc[:, 1:513], scalar=convP[:, 1, j:j + 1], in1=ca[:, 0:512],
                                                                                             op0=ALU.mult, op1=ALU.add),
                                 reads=[zcres, "convP", cares], writes=[cares])
                            S.op("dve", lambda e, ca=ca, zc=zc, j=j: e.scalar_tensor_tensor(out=ca[:, 0:512], in0=zc[:, 0:512], scalar=convP[:, 0, j:j + 1], in1=ca[:, 0:512],
                                                                                             op0=ALU.mult, op1=ALU.add),
                                 reads=[zcres, "convP", cares], writes=[cares])
                            bp, bpres = zchunk(wb, wbres, jj)
                            S.op("dve", lambda e, bp=bp, ca=ca, j=j, bc=bc: e.tensor_tensor(out=bc[:, j, :], in0=bp, in1=ca[:, 0:512], op=ALU.mult),
                                 reads=[bpres, cares], writes=[bcres])
                        W.release(wxh, wbh, wch)
                    mixb, mbres = a8.next()
                    for n in range(8):
                        if n % 4 == 0:
                            if n:
                                W.release(wcc1[2], wgc1[2])
                            wcc1 = W.get(kview(P["w_proj_c"])[:, :, (n // 4) * 512:(n // 4 + 1) * 512])
                            wgc1 = W.get(w_in_v[:, :, (16 + n // 4) * 512:(17 + n // 4) * 512])
                            wcc = {n // 4: wcc1}
                            wgc = {n // 4: wgc1}
                        gp, gres = zchunk(wgc[n // 4][0], wgc[n // 4][1], n % 4)
                        sg, sgres = sgr.next()
                        S.op("act", lambda e, gp=gp, sg=sg: e.activation(out=sg, in_=gp, func=AF.Sigmoid), reads=[gres], writes=[sgres])
                        pb, pres = PB.next()
                        mm_group(pb, pres, [(wcc[n // 4][0][:, kc, (n % 4) * 128:(n % 4 + 1) * 128], bc[:, kc, :]) for kc in range(8)],
                                 [wcc[n // 4][1], bcres])
                        tm, tres = T2.next()
                        S.op("dve", lambda e, pb=pb, sg=sg, tm=tm: e.tensor_tensor(out=tm[:, 0:512], in0=pb, in1=sg, op=ALU.mult),
                             reads=[pres, sgres], writes=[tres])
                        S.op("pool", lambda e, tm=tm, n=n, mixb=mixb: e.tensor_tensor(out=mixb[:, n, :], in0=mix[:, n, :], in1=tm[:, 0:512], op=ALU.add),
                             reads=[tres, "mix%d" % n], writes=[mbres])
                    W.release(wcc1[2], wgc1[2])
                    if tt == 0:
                        dump(S, "bc", bc, [bcres]); dump(S, "mixb", mixb, [mbres])
                    wo = [W.get(kview(P["w_out"])[:, :, hf * 512:(hf + 1) * 512]) for hf in range(2)]
                    for s in range(4):
                        xa, xres = xs_list[s]
                        for hf in range(2):
                            pb, pres = PB.next()
                            mm_group(pb, pres, [(mixb[:, kc, s * 128:(s + 1) * 128], wo[hf][0][:, kc, :]) for kc in range(8)],
                                     [wo[hf][1], mbres])
                            tm, tres = T2.next()
                            S.op("dve", lambda e, pb=pb, tm=tm, hf=hf: e.tensor_tensor(out=tm[:, 0:512], in0=pb, in1=GB[:, hf * 512:(hf + 1) * 512], op=ALU.mult),
                                 reads=[pres, "GB"], writes=[tres])
                            S.op("dve", lambda e, tm=tm, xa=xa, hf=hf: e.tensor_tensor(out=xa[:, hf * 512:(hf + 1) * 512], in0=xa[:, hf * 512:(hf + 1) * 512],
                                                                                        in1=tm[:, 0:512], op=ALU.add),
                                 reads=[tres, xres], writes=[xres])
                        r0 = t0 + s * 128
                        S.op("sp", lambda e, xa=xa, r0=r0: e.dma_start(out=mdst_d[r0:r0 + 128, :], in_=xa),
                             reads=[xres], writes=["xs%d" % (r0 // 128)], dma="st_" + xres)
                    W.release(wo[0][2], wo[1][2])

            def moe_phase(l, dst_d, dst_name, do_final):
                P = L[l]
                A.cur = mark_common
                xsub = Ring("xsub", [A.alloc([128, D], F32) for _ in range(3)])
                xnr = Ring("xn", [A.alloc([128, D], BF16) for _ in range(2)])
                junk = A.alloc([128, D], BF16)
                h2T = A.alloc([128, 8, 1024], BF16)
                actT = A.alloc([128, 8, 1024], BF16)
                acc = A.alloc([128, 8, D], F32)
                T2 = Ring("T2", [A.alloc([128, 512], F32) for _ in range(10)])
                Gtok = A.alloc([128, 8, NE], F32)
                GT = A.alloc([128, 8, 128], F32)
                lgr = Ring("lg", [A.alloc([128, NE], F32) for _ in range(2)])
                exr = Ring("ex", [A.alloc([128, NE], F32) for _ in range(2)])
                mkr = Ring("mk", [A.alloc([128, NE], F32) for _ in range(2)])
                m8r = Ring("m8", [A.alloc([128, 8], F32) for _ in range(2)])
                smr = Ring("sm", [A.alloc([128, 4], F32) for _ in range(2)])
                routerW = A.alloc([128, 8, NE], BF16)
                rbB = A.alloc([128, NE], F32)
                bguP = A.alloc([128, NE, 16], F32)
                bdown = A.alloc([128, D], F32)
                finalg = A.alloc([128, D], F32) if do_final else None

                mods_rows(l, (10, 11))
                S.op("pool", lambda e: e.dma_start(out=routerW, in_=kview(P["router_w"])), writes=["routerW"], dma="routerW")
                S.op("sp", lambda e: e.dma_start(out=rbB, in_=P["rbB"]), writes=["rbB"], dma="rbB")
                S.op("sp", lambda e: e.dma_start(out=bguP, in_=P["bguP"]), writes=["bguP"], dma="bguP")
                S.op("sp", lambda e: e.dma_start(out=bdown[0:NE, :], in_=P["b_down"]), writes=["bdown"], dma="bdown")
                if do_final:
                    S.op("sp", lambda e: e.dma_start(out=finalg, in_=finalg_d), writes=["finalg"], dma="finalg")

                for st in range(n_super):
                    t0 = st * 1024
                    for s in range(8):
                        xa, xres = xsub.next()
                        r0 = t0 + s * 128
                        S.op("sp", lambda e, xa=xa, r0=r0: e.dma_start(out=xa, in_=xs_d[r0:r0 + 128, :]),
                             reads=["xs%d" % (r0 // 128)], writes=[xres], dma=xres)
                        norm_transpose(xa, xres, xnr, junk, A2, "A2", 24, h2T, "h2T", s * 128)
                        pb, pres = PB.next()
                        mm_group(pb[:, 0:NE], pres, [(h2T[:, kc, s * 128:(s + 1) * 128], routerW[:, kc, :]) for kc in range(8)],
                                 ["h2T", "routerW"])
                        lg, lgres = lgr.next()
                        ex, exres = exr.next()
                        mk, mkres = mkr.next()
                        m8, m8res = m8r.next()
                        sm, smres = smr.next()
                        S.op("dve", lambda e, lg=lg, pb=pb: e.tensor_tensor(out=lg, in0=pb[:, 0:NE], in1=rbB, op=ALU.add),
                             reads=[pres, "rbB"], writes=[lgres])
                        S.op("dve", lambda e, lg=lg, m8=m8: e.max(out=m8, in_=lg), reads=[lgres], writes=[m8res])
                        S.op("dve", lambda e, lg=lg, m8=m8, mk=mk: e.tensor_scalar(out=mk, in0=lg, scalar1=m8[:, 3:4], scalar2=None, op0=ALU.is_ge),
                             reads=[lgres, m8res], writes=[mkres])
                        S.op("dve", lambda e, sm=sm, m8=m8: e.tensor_scalar(out=sm[:, 0:1], in0=m8[:, 0:1], scalar1=-1.0, scalar2=None, op0=ALU.mult),
                             reads=[m8res], writes=[smres])
                        S.op("act", lambda e, ex=ex, lg=lg, sm=sm: e.activation(out=ex, in_=lg, func=AF.Exp, bias=sm[:, 0:1], scale=1.0),
                             reads=[lgres, smres], writes=[exres])
                        S.op("dve", lambda e, sm=sm: e.memset(sm[:, 1:2], 0.0), reads=[smres], writes=[smres])
                        S.op("dve", lambda e, ex=ex, mk=mk, sm=sm: e.scalar_tensor_tensor(out=ex, in0=ex, scalar=1.0, in1=mk, op0=ALU.mult, op1=ALU.mult,
                                                                                           accum_out=sm[:, 1:2]),
                             reads=[exres, mkres, smres], writes=[exres, smres])
                        S.op("dve", lambda e, sm=sm: e.reciprocal(out=sm[:, 2:3], in_=sm[:, 1:2]), reads=[smres], writes=[smres])
                        S.op("dve", lambda e, ex=ex, sm=sm, s=s: e.tensor_scalar(out=Gtok[:, s, :], in0=ex, scalar1=sm[:, 2:3], scalar2=None, op0=ALU.mult),
                             reads=[exres, smres], writes=["Gtok%d" % s])
                        pt, ptres = PB.next()
                        S.op("pe", lambda e, pt=pt, s=s: e.transpose(out=pt[0:NE, 0:128], in_=Gtok[:, s, :], identity=identf),
                             reads=["Gtok%d" % s, "identf"], writes=[ptres])
                        S.op("act", lambda e, pt=pt, s=s: e.activation(out=GT[0:NE, s, :], in_=pt[0:NE, 0:128], func=AF.Copy),
                             reads=[ptres], writes=["GT%d" % s])
                        for hf in range(2):
                            pb2, p2res = PB.next()
                            S.op("pe", lambda e, pb2=pb2, s=s, hf=hf: e.matmul(pb2, lhsT=GT[0:NE, s, :], rhs=bdown[0:NE, hf * 512:(hf + 1) * 512],
                                                                                start=True, stop=True),
                                 reads=["GT%d" % s, "bdown"], writes=[p2res])
                            S.op("act", lambda e, pb2=pb2, s=s, hf=hf: e.activation(out=acc[:, s, hf * 512:(hf + 1) * 512], in_=pb2, func=AF.Copy),
                                 reads=[p2res], writes=["acc%d_%d" % (s, hf)])
                    for ex_i in range(n_exp):
                        wgv = kview(P["w_gu"][ex_i])
                        wdv = kview(P["w_down"][ex_i])
                        for hfj in range(2):
                            gw, gwres, gwh = W.get(wgv[:, :, hfj * 512:(hfj + 1) * 512])
                            uw, uwres, uwh = W.get(wgv[:, :, (2 + hfj) * 512:(3 + hfj) * 512])
                            for th in range(2):
                                for jj in range(4):
                                    j = hfj * 4 + jj
                                    gp, gpres = PB.next()
                                    mm_group(gp, gpres, [(gw[:, kc, jj * 128:(jj + 1) * 128], h2T[:, kc, th * 512:(th + 1) * 512]) for kc in range(8)],
                                             [gwres, "h2T"])
                                    up, upres = PB.next()
                                    mm_group(up, upres, [(uw[:, kc, jj * 128:(jj + 1) * 128], h2T[:, kc, th * 512:(th + 1) * 512]) for kc in range(8)],
                                             [uwres, "h2T"])
                                    gc, gcres = T2.next()
                                    sg, sgres = T2.next()
                                    uc, ucres = T2.next()
                                    rr, rrres = T2.next()
                                    S.op("dve", lambda e, gc=gc, gp=gp, j=j, ex_i=ex_i: e.tensor_scalar(out=gc, in0=gp, scalar1=bguP[:, ex_i, j:j + 1], scalar2=7.0,
                                                                                                        op0=ALU.add, op1=ALU.min),
                                         reads=[gpres, "bguP"], writes=[gcres])
                                    S.op("act", lambda e, sg=sg, gc=gc: e.activation(out=sg, in_=gc, func=AF.Silu, scale=1.702),
                                         reads=[gcres], writes=[sgres])
                                    S.op("dve", lambda e, uc=uc, up=up, j=j, ex_i=ex_i: e.tensor_scalar(out=uc, in0=up, scalar1=bguP[:, ex_i, 8 + j:9 + j], scalar2=7.0,
                                                                                                        op0=ALU.add, op1=ALU.min),
                                         reads=[upres, "bguP"], writes=[ucres])
                                    S.op("dve", lambda e, rr=rr, uc=uc: e.tensor_scalar(out=rr, in0=uc, scalar1=-7.0, scalar2=1.0, op0=ALU.max, op1=ALU.add),
                                         reads=[ucres], writes=[rrres])
                                    S.op("dve", lambda e, rr=rr, sg=sg, j=j, th=th: e.scalar_tensor_tensor(out=actT[:, j, th * 512:(th + 1) * 512], in0=rr,
                                                                                                        scalar=1.0 / 1.702, in1=sg, op0=ALU.mult, op1=ALU.mult),
                                         reads=[rrres, sgres], writes=["actT%d" % th])
                            W.release(gwh, uwh)
                        dpieces = [W.get(wdv[:, :, hf * 512:(hf + 1) * 512]) for hf in range(2)]
                        for s in range(8):
                            th = s // 4
                            for hf in range(2):
                                pb, pres = PB.next()
                                mm_group(pb, pres, [(actT[:, kc, s * 128:(s + 1) * 128], dpieces[hf][0][:, kc, :]) for kc in range(8)],
                                         [dpieces[hf][1], "actT%d" % th])
                                ar_ = "acc%d_%d" % (s, hf)
                                S.op("dve", lambda e, pb=pb, s=s, hf=hf, ex_i=ex_i: e.scalar_tensor_tensor(
                                    out=acc[:, s, hf * 512:(hf + 1) * 512], in0=pb, scalar=Gtok[:, s, ex_i:ex_i + 1],
                                    in1=acc[:, s, hf * 512:(hf + 1) * 512], op0=ALU.mult, op1=ALU.add),
                                     reads=[pres, "Gtok%d" % s, ar_], writes=[ar_])
                        W.release(dpieces[0][2], dpieces[1][2])
                    for s in range(8):
                        xa, xres = xsub.next()
                        r0 = t0 + s * 128
                        S.op("sp", lambda e, xa=xa, r0=r0: e.dma_start(out=xa, in_=xs_d[r0:r0 + 128, :]),
                             reads=["xs%d" % (r0 // 128)], writes=[xres], dma=xres)
                        S.op("dve", lambda e, s=s: e.tensor_tensor(out=acc[:, s, :], in0=acc[:, s, :], in1=GB, op=ALU.mult),
                             reads=["acc%d_0" % s, "acc%d_1" % s, "GB"], writes=["acc%d_0" % s, "acc%d_1" % s])
                        S.op("dve", lambda e, s=s, xa=xa: e.tensor_tensor(out=xa, in0=xa, in1=acc[:, s, :], op=ALU.add),
                             reads=["acc%d_0" % s, "acc%d_1" % s, xres], writes=[xres])
                        if do_final:
                            rsa, rres = rms_rstd(xa, xres, junk, "junk")
                            S.op("dve", lambda e, xa=xa, rsa=rsa: e.scalar_tensor_tensor(out=xa, in0=xa, scalar=rsa, in1=finalg, op0=ALU.mult, op1=ALU.mult),
                                 reads=[xres, rres, "finalg"], writes=[xres])
                        S.op("sp", lambda e, xa=xa, r0=r0: e.dma_start(out=dst_d[r0:r0 + 128, :], in_=xa),
                             reads=[xres], writes=["%s%d" % (dst_name, r0 // 128)], dma="st_" + xres)


            def moe_sparse_phase(l, dst_d, dst_name, do_final):
                P = L[l]
                A.cur = mark_common
                NT = SEQ // 128
                lgall = A.alloc([128, NT, NE], F32)
                m8all = A.alloc([128, NT, 8], F32)
                w4all = A.alloc([128, NT, 4], F32)
                mkb = A.alloc([128, NT, NE], BF16)
                slotf = A.alloc([128, NT, 4], F32)
                sloti = A.alloc([128, NT, 4], I32)
                tokid = A.alloc([128, NT], I32)
                ebase = A.alloc([128, NE], F32)
                ustrf = A.alloc([128, 128], F32)
                onesb = A.alloc([128, 128], BF16)
                Ub = A.alloc([128, 128], BF16)
                onesf = A.alloc([128, 128], F32)
                zeros_i = A.alloc([128, NE * CAP // 128], I32)
                smr = Ring("sm", [A.alloc([128, 4], F32) for _ in range(2)])
                routerW = A.alloc([128, 8, NE], BF16)
                rbB = A.alloc([128, NE], F32)
                bguP = A.alloc([128, NE, 16], F32)
                finalg = A.alloc([128, D], F32) if do_final else None
                junk32 = A.alloc([128, NE], F32)
                mark_persist = A.cur
                xsub = Ring("xsub", [A.alloc([128, D], F32) for _ in range(4)])
                xnr = Ring("xn", [A.alloc([128, D], BF16) for _ in range(8)])
                junk = A.alloc([128, D], BF16)
                hTr = Ring("hTr", [A.alloc([128, 8, 128], BF16) for _ in range(2)])
                h2r = Ring("h2c", [A.alloc([128, 8, 512], BF16) for _ in range(4)])
                actr = Ring("actc", [A.alloc([128, 8, 512], BF16) for _ in range(2)])
                tpr = Ring("tp", [tpb, tpb2], names=["tpb", "tpb2"])
                T2 = Ring("T2", [A.alloc([128, 512], F32) for _ in range(8)])
                tslr = Ring("tsl", [A.alloc([128, 1], I32) for _ in range(4)])
                ysr = Ring("ysr", [A.alloc([128, D], F32) for _ in range(4)])
                bdrow = Ring("bdrow", [A.alloc([128, D], BF16) for _ in range(2)])
                NSLOT = NE * CAP
                if "bc" not in pool_regs:
                    pool_regs["bc"] = None

                    def _init_bc(e):
                        r = e.alloc_register("bc")
                        pool_regs["bc"] = r
                        return e.reg_mov(r, NSLOT - 1)
                    S.op("pool", _init_bc)

                mods_rows(l, (10, 11))
                S.op("pool", lambda e: e.dma_start(out=routerW, in_=kview(P["router_w"])), writes=["routerW"], dma="routerW")
                S.op("sp", lambda e: e.dma_start(out=rbB, in_=P["rbB"]), writes=["rbB"], dma="rbB")
                S.op("sp", lambda e: e.dma_start(out=bguP, in_=P["bguP"]), writes=["bguP"], dma="bguP")
                S.op("sp", lambda e: e.dma_start(out=ebase, in_=ebase_d), writes=["ebase"], dma="ebase")
                S.op("sp", lambda e: e.dma_start(out=ustrf, in_=ustr_d), writes=["ustrf"], dma="ustrf")
                S.op("sp", lambda e: e.dma_start(out=tokid, in_=tokid_d), writes=["tokid"], dma="tokid")
                if do_final:
                    S.op("sp", lambda e: e.dma_start(out=finalg, in_=finalg_d), writes=["finalg"], dma="finalg")
                S.op("dve", lambda e: e.tensor_copy(out=Ub, in_=ustrf), reads=["ustrf"], writes=["Ub"])
                S.op("dve", lambda e: e.memset(onesb, 1.0), writes=["onesb"])
                S.op("dve", lambda e: e.memset(onesf, 1.0), writes=["onesf"])
                S.op("dve", lambda e: e.memset(zeros_i, 0), writes=["zeros_i"])
                S.op("sp", lambda e: e.dma_start(out=tokslot_d.rearrange("(p f) o -> p (f o)", p=128), in_=zeros_i),
                     reads=["zeros_i"], writes=["tokslot0"], dma="tokslot0")

                def partA1(e_, c_):
                    xs_ = []
                    for sb in range(4):
                        tsl, tslres = tslr.next()
                        s0 = e_ * CAP + c_ * 512 + sb * 128
                        S.op("sp", lambda e, tsl=tsl, s0=s0: e.dma_start(out=tsl, in_=tokslot_d[s0:s0 + 128, :]), reads=scat_res + ["tokslot0"], writes=[tslres], dma=tslres)
                        xa, xres = xsub.next()
                        S.op("pool", lambda e, xa=xa, tsl=tsl: e.indirect_dma_start(out=xa, out_offset=None, in_=xs_d[:, :],
                                                                                     in_offset=bass.IndirectOffsetOnAxis(ap=tsl[:, 0:1], axis=0)),
                             reads=[tslres, "XS_G"], writes=[xres], dma=xres)
                        xs_.append((xa, xres))
                    return xs_

                def partA2(xs_):
                    outs = []
                    for xa, xres in xs_:
                        xn, xnres = xnr.next()
                        rsa, rres = rms_rstd(xa, xres, xn, xnres)
                        S.op("act", lambda e, xn=xn, xa=xa, rsa=rsa: e.activation(out=xn, in_=xa, func=AF.Identity, scale=rsa),
                             reads=[xres, rres], writes=[xnres])
                        outs.append((xn, xnres))
                    return outs

                def partA(e_, c_):
                    return partA2(partA1(e_, c_))

                def partB(outs, hc, hres):
                    for sb, (xn, xnres) in enumerate(outs):
                        tp, tpres = tpr.next()
                        for kc in range(8):
                            S.op("pe", lambda e, kc=kc, tp=tp, xn=xn: e.transpose(out=tp[:, kc, :], in_=xn[:, kc * 128:(kc + 1) * 128], identity=identb),
                                 reads=[xnres, "identb"], writes=[tpres])
                        for kc in range(8):
                            dst = hc[:, kc, sb * 128:(sb + 1) * 128]
                            if False:
                                S.op("dve", lambda e, kc=kc, dst=dst, tp=tp: e.tensor_scalar(out=dst, in0=tp[:, kc, :], scalar1=A2[:, kc:kc + 1],
                                                                                              scalar2=modsP[:, 24 + kc:25 + kc], op0=ALU.mult, op1=ALU.add),
                                     reads=[tpres, "A2", "modsP"], writes=[hres])
                            else:
                                S.op("act", lambda e, kc=kc, dst=dst, tp=tp: e.activation(out=dst, in_=tp[:, kc, :], func=AF.Identity,
                                                                                           bias=modsP[:, 24 + kc:25 + kc], scale=A2[:, kc:kc + 1]),
                                     reads=[tpres, "A2", "modsP"], writes=[hres])

                for tt in range(NT):
                    xa, xres = xsub.next()
                    r0 = tt * 128
                    S.op("sp", lambda e, xa=xa, r0=r0: e.dma_start(out=xa, in_=xs_d[r0:r0 + 128, :]), reads=["xs%d" % tt], writes=[xres], dma=xres)
                    hs, hsres = hTr.next()
                    partB(partA2([(xa, xres)]), hs, hsres)
                    pb, pres = PB.next()
                    mm_group(pb[:, 0:NE], pres, [(hs[:, kc, :], routerW[:, kc, :]) for kc in range(8)], [hsres, "routerW"])
                    lg = lgall[:, tt, :]
                    m8 = m8all[:, tt, :]
                    sm, smres = smr.next()
                    rt = "rt%d" % tt
                    S.op("dve", lambda e, lg=lg, pb=pb: e.tensor_tensor(out=lg, in0=pb[:, 0:NE], in1=rbB, op=ALU.add), reads=[pres, "rbB"], writes=[rt])
                    S.op("dve", lambda e, lg=lg, m8=m8: e.max(out=m8, in_=lg), reads=[rt], writes=[rt])
                    S.op("dve", lambda e, lg=lg, m8=m8, tt=tt: e.tensor_scalar(out=mkb[:, tt, :], in0=lg, scalar1=m8[:, 3:4], scalar2=None, op0=ALU.is_ge),
                         reads=[rt], writes=["mkb%d" % tt])
                    S.op("dve", lambda e, sm=sm, m8=m8: e.tensor_scalar(out=sm[:, 0:1], in0=m8[:, 0:1], scalar1=-1.0, scalar2=None, op0=ALU.mult),
                         reads=[rt], writes=[smres])
                    S.op("dve", lambda e, sm=sm: e.memset(sm[:, 1:2], 0.0), reads=[smres], writes=[smres])
                    S.op("act", lambda e, m8=m8, sm=sm, tt=tt: e.activation(out=w4all[:, tt, :], in_=m8[:, 0:4], func=AF.Exp, bias=sm[:, 0:1], scale=1.0,
                                                                          accum_out=sm[:, 1:2]),
                         reads=[rt, smres], writes=["w4_%d" % tt, smres])
                    S.op("dve", lambda e, sm=sm: e.reciprocal(out=sm[:, 2:3], in_=sm[:, 1:2]), reads=[smres], writes=[smres])
                    S.op("dve", lambda e, sm=sm, tt=tt: e.tensor_scalar(out=w4all[:, tt, :], in0=w4all[:, tt, :], scalar1=sm[:, 2:3], scalar2=None, op0=ALU.mult),
                         reads=["w4_%d" % tt, smres], writes=["w4_%d" % tt])
                scat_res = []
                for tt in range(NT):
                    pr, prres = PB.next()
                    pairs = [(onesb, mkb[:, t2, :]) for t2 in range(tt)] + [(Ub, mkb[:, tt, :])]
                    mm_group(pr[:, 0:NE], prres, pairs, ["mkb%d" % t2 for t2 in range(tt + 1)] + ["onesb", "Ub"])
                    sf, sfres = T2.next()
                    ov, ovres = T2.next()
                    S.op("dve", lambda e, sf=sf, pr=pr: e.tensor_tensor(out=sf[:, 0:NE], in0=pr[:, 0:NE], in1=ebase, op=ALU.add), reads=[prres, "ebase"], writes=[sfres])
                    S.op("dve", lambda e, ov=ov, pr=pr: e.tensor_scalar(out=ov[:, 0:NE], in0=pr[:, 0:NE], scalar1=float(CAP), scalar2=1.0e7, op0=ALU.is_ge, op1=ALU.mult),
                         reads=[prres], writes=[ovres])
                    S.op("dve", lambda e, sf=sf, ov=ov: e.tensor_tensor(out=sf[:, 0:NE], in0=sf[:, 0:NE], in1=ov[:, 0:NE], op=ALU.add), reads=[sfres, ovres], writes=[sfres])
                    S.op("dve", lambda e, tt=tt: e.memset(slotf[:, tt, :], 0.0), writes=["slotf%d" % tt])
                    for k in range(4):
                        S.op("dve", lambda e, tt=tt, k=k, sf=sf: e.scalar_tensor_tensor(out=junk32, in0=lgall[:, tt, :], scalar=m8all[:, tt, k:k + 1], in1=sf[:, 0:NE],
                                                                                         op0=ALU.is_equal, op1=ALU.mult, accum_out=slotf[:, tt, k:k + 1]),
                             reads=["rt%d" % tt, sfres, "slotf%d" % tt], writes=["junk32", "slotf%d" % tt])
                    S.op("dve", lambda e, tt=tt: e.tensor_copy(out=sloti[:, tt, :], in_=slotf[:, tt, :]), reads=["slotf%d" % tt], writes=["sloti%d" % tt])
                    for k in range(4):
                        sr = "scat%d_%d" % (tt, k)
                        S.op("pool", lambda e, tt=tt, k=k: e.indirect_dma_start(out=tokslot_d[:, :], out_offset=bass.IndirectOffsetOnAxis(ap=sloti[:, tt, k:k + 1], axis=0),
                                                                               in_=tokid[:, tt:tt + 1], in_offset=None, bounds_check=pool_regs["bc"], oob_is_err=False),
                             reads=["sloti%d" % tt, "tokid", "tokslot0"], writes=[sr], dma="scat")
                        scat_res.append(sr)
                ys_res = []
                NCH = CAP // 512
                chunks = [(e_, c_) for e_ in range(n_exp) for c_ in range(NCH)]

                pend = {}
                hslot = {}
                pend[0] = partA(*chunks[0])
                hslot[0] = h2r.next()
                partB(pend.pop(0), *hslot[0])
                if len(chunks) > 1:
                    pend[1] = partA(*chunks[1])
                wp = {}
                for n, (ex_i, c_) in enumerate(chunks):
                    pa1 = partA1(*chunks[n + 2]) if n + 2 < len(chunks) else None
                    hc, hres = hslot.pop(n)
                    wgv = kview(P["w_gu"][ex_i])
                    wdv = kview(P["w_down"][ex_i])
                    if c_ == 0:
                        for hfj in range(2):
                            wp["g%d" % hfj] = W.get(wgv[:, :, hfj * 512:(hfj + 1) * 512])
                            wp["u%d" % hfj] = W.get(wgv[:, :, (2 + hfj) * 512:(3 + hfj) * 512])
                        bdr, bdres = bdrow.next()
                        S.op("pool", lambda e, bdr=bdr, ex_i=ex_i: e.dma_start(out=bdr[0:1, :], in_=P["b_down"][ex_i:ex_i + 1, :]), writes=[bdres], dma=bdres)
                    ac, acres = actr.next()
                    for hfj in range(2):
                        if hfj == 1 and pa1 is not None:
                            pend[n + 2] = partA2(pa1)
                        gw, gwres, gwh = wp["g%d" % hfj]
                        uw, uwres, uwh = wp["u%d" % hfj]
                        for jj in range(4):
                            j = hfj * 4 + jj
                            gp, gpres = PB.next()
                            mm_group(gp, gpres, [(gw[:, kc, jj * 128:(jj + 1) * 128], hc[:, kc, :]) for kc in range(8)], [gwres, hres])
                            up, upres = PB.next()
                            mm_group(up, upres, [(uw[:, kc, jj * 128:(jj + 1) * 128], hc[:, kc, :]) for kc in range(8)], [uwres, hres])
                            gc, gcres = T2.next()
                            sg, sgres = T2.next()
                            uc, ucres = T2.next()
                            S.op("dve", lambda e, gc=gc, gp=gp, j=j, ex_i=ex_i: e.tensor_scalar(out=gc, in0=gp, scalar1=bguP[:, ex_i, j:j + 1], scalar2=7.0, op0=ALU.add, op1=ALU.min),
                                 reads=[gpres, "bguP"], writes=[gcres])
                            S.op("act", lambda e, sg=sg, gc=gc: e.activation(out=sg, in_=gc, func=AF.Silu, scale=1.702), reads=[gcres], writes=[sgres])
                            S.op("dve", lambda e, uc=uc, up=up, j=j, ex_i=ex_i: e.tensor_scalar(out=uc, in0=up, scalar1=bguP[:, ex_i, 8 + j:9 + j], scalar2=7.0, op0=ALU.add, op1=ALU.min),
                                 reads=[upres, "bguP"], writes=[ucres])
                            S.op("dve", lambda e, uc=uc: e.tensor_scalar(out=uc, in0=uc, scalar1=-7.0, scalar2=1.0, op0=ALU.max, op1=ALU.add), reads=[ucres], writes=[ucres])
                            S.op("dve", lambda e, uc=uc, sg=sg, j=j, ac=ac: e.scalar_tensor_tensor(out=ac[:, j, :], in0=uc, scalar=1.0 / 1.702, in1=sg, op0=ALU.mult, op1=ALU.mult),
                                 reads=[ucres, sgres], writes=[acres])
                    if c_ == NCH - 1:
                        W.release(wp["g0"][2], wp["u0"][2], wp["g1"][2], wp["u1"][2])
                    if n + 1 < len(chunks):
                        hslot[n + 1] = h2r.next()
                        partB(pend.pop(n + 1), *hslot[n + 1])
                    if c_ == 0:
                        wp["d"] = [W.get(wdv[:, :, hf * 512:(hf + 1) * 512]) for hf in range(2)]
                    dpieces = wp["d"]
                    for sb in range(4):
                        yt, ytres = ysr.next()
                        for hf in range(2):
                            pb, pres = PB.next()
                            pairs = [(onesb[0:1, :], bdr[0:1, hf * 512:(hf + 1) * 512])] + \
                                    [(ac[:, kc, sb * 128:(sb + 1) * 128], dpieces[hf][0][:, kc, :]) for kc in range(8)]
                            mm_group(pb, pres, pairs, [dpieces[hf][1], acres, bdres, "onesb"])
                            S.op("act", lambda e, pb=pb, yt=yt, hf=hf: e.activation(out=yt[:, hf * 512:(hf + 1) * 512], in_=pb, func=AF.Copy), reads=[pres], writes=[ytres])
                        s0 = ex_i * CAP + c_ * 512 + sb * 128
                        yr = "ys_%d_%d_%d" % (ex_i, c_, sb)
                        S.op("sp", lambda e, yt=yt, s0=s0: e.dma_start(out=ys_d[s0:s0 + 128, :], in_=yt), reads=[ytres], writes=[yr], dma="st_" + ytres)
                        ys_res.append(yr)
                    if c_ == NCH - 1:
                        W.release(dpieces[0][2], dpieces[1][2])
                S.barrier()
                A.cur = mark_persist
                ykr = Ring("ykr", [A.alloc([128, D], F32) for _ in range(12)])
                xsub = Ring("xsubc", [A.alloc([128, D], F32) for _ in range(4)])
                accr = Ring("accr", [A.alloc([128, D], F32) for _ in range(3)])
                junk = A.alloc([128, D], BF16)
                for tt in range(NT):
                    acc, accres = accr.next()
                    for k in range(4):
                        yk, ykres = ykr.next()
                        S.op("dve", lambda e, yk=yk: e.memset(yk, 0.0), writes=[ykres])
                        S.op("pool", lambda e, yk=yk, tt=tt, k=k: e.indirect_dma_start(out=yk, out_offset=None, in_=ys_d[:, :],
                                                                                        in_offset=bass.IndirectOffsetOnAxis(ap=sloti[:, tt, k:k + 1], axis=0),
                                                                                        bounds_check=pool_regs["bc"], oob_is_err=False),
                             reads=[ykres], writes=[ykres], dma=ykres)
                        if k == 0:
                            S.op("dve", lambda e, yk=yk, acc=acc, tt=tt: e.tensor_scalar(out=acc, in0=yk, scalar1=w4all[:, tt, 0:1], scalar2=None, op0=ALU.mult),
                                 reads=[ykres], writes=[accres])
                        else:
                            S.op("dve", lambda e, yk=yk, acc=acc, tt=tt, k=k: e.scalar_tensor_tensor(out=acc, in0=yk, scalar=w4all[:, tt, k:k + 1], in1=acc, op0=ALU.mult, op1=ALU.add),
                                 reads=[ykres, accres], writes=[accres])
                    xa, xres = xsub.next()
                    r0 = tt * 128
                    S.op("sp", lambda e, xa=xa, r0=r0: e.dma_start(out=xa, in_=xs_d[r0:r0 + 128, :]), reads=["xs%d" % tt], writes=[xres], dma=xres)
                    S.op("dve", lambda e, acc=acc: e.tensor_tensor(out=acc, in0=acc, in1=GB, op=ALU.mult), reads=[accres, "GB"], writes=[accres])
                    S.op("dve", lambda e, acc=acc, xa=xa: e.tensor_tensor(out=xa, in0=xa, in1=acc, op=ALU.add), reads=[accres, xres], writes=[xres])
                    if do_final:
                        rsa, rres = rms_rstd(xa, xres, junk, "junk")
                        S.op("dve", lambda e, xa=xa, rsa=rsa: e.scalar_tensor_tensor(out=xa, in0=xa, scalar=rsa, in1=finalg, op0=ALU.mult, op1=ALU.mult),
                             reads=[xres, rres, "finalg"], writes=[xres])
                    S.op("sp", lambda e, xa=xa, r0=r0: e.dma_start(out=dst_d[r0:r0 + 128, :], in_=xa),
                         reads=[xres], writes=["%s%d" % (dst_name, tt), "XS_G"], dma="st_" + xres)

            first = True
            for li, l in enumerate(layers):
                layer_prologue(l)
                if do_mixer:
                    mixer_phase(l, x_in if first else xs_d, "xin" if first else "xs", xs_d if do_moe else out_d)
                    S.barrier()
                last = (li == len(layers) - 1)
                if do_moe:
                    (moe_sparse_phase if SPARSE else moe_phase)(l, out_d if last else xs_d, "out" if last else "xs", do_final=(final and last))
                    S.barrier()
                first = False
            return W

        Wd = program(DummySched(), None)
        S = Sched(nc)
        program(S, Wd.rec)
        S.emit()
        build.last_stats = {e: len(S.stream[e]) for e in S.ENGS}
    return nc


def _pp(v):
    return np.ascontiguousarray(v.reshape(-1, 128).T)


def _bc(v):
    return np.ascontiguousarray(np.broadcast_to(v[None, :], (128, v.shape[0])))


def _consts():
    ident = np.eye(128, dtype=np.float32)
    triu = np.triu(np.ones((128, 128), dtype=np.float32))
    rc = np.zeros((128, 4, 16), dtype=np.float32)
    for gi, w in enumerate(POOL_WINDOWS):
        rc[:, gi, :] = 1.0 / np.minimum(np.arange(16) + 1, w)
    return ident, triu, rc


def _consts2():
    ustr = np.triu(np.ones((128, 128), dtype=np.float32), k=1)
    ebase = np.ascontiguousarray(np.broadcast_to((np.arange(NE, dtype=np.float32) * CAP)[None, :], (128, NE)))
    tokid = (np.arange(32, dtype=np.int32)[None, :] * 128 + np.arange(128, dtype=np.int32)[:, None]).astype(np.int32)
    return {"ustr": ustr, "ebase": ebase, "tokid": np.ascontiguousarray(tokid)}


def layer_inputs(inp, l):
    f = lambda k: np.asarray(inp[k][l], dtype=np.float32)
    ada_b = f("ada_b")
    d = {
        "ada_w%d" % l: f("ada_w"),
        "ada_bP%d" % l: np.ascontiguousarray(ada_b.reshape(48, 128).T),
        "adab_g1B%d" % l: _bc(ada_b[2048:3072]),
        "adab_g2B%d" % l: _bc(ada_b[5120:6144]),
        "n1gP%d" % l: _pp(f("norm1_g")),
        "n2gP%d" % l: _pp(f("norm2_g")),
        "w_in%d" % l: f("w_in"),
        "gnormB%d" % l: _bc(f("gmlp_norm_g")),
        "ws%d" % l: f("gmlp_ws"),
        "bsB%d" % l: _bc(f("gmlp_bs").reshape(-1)),
        "w_proj_a%d" % l: f("w_proj_a"),
        "pool_w%d" % l: f("pool_w"),
        "pscP%d" % l: _pp(f("pool_scale")),
        "convP%d" % l: np.ascontiguousarray(f("conv_w").reshape(3, 8, 128).transpose(2, 0, 1)),
        "w_proj_c%d" % l: f("w_proj_c"),
        "w_out%d" % l: f("w_out"),
        "router_w%d" % l: f("router_w"),
        "rbB%d" % l: _bc(f("router_b")),
        "w_gu%d" % l: f("exp_w_gu"),
        "bguP%d" % l: np.ascontiguousarray(f("exp_b_gu").reshape(NE, 16, 128).transpose(2, 0, 1)),
        "w_down%d" % l: f("exp_w_down"),
        "b_down%d" % l: f("exp_b_down"),
    }
    return d


_NC_CACHE = {}


def _get_nc(key, **kw):
    if key not in _NC_CACHE:
        _NC_CACHE[key] = build(**kw)
    return _NC_CACHE[key]


FUSED = True


def kernel(**inputs):
    x = np.asarray(inputs["x"], dtype=np.float32)
    c = np.asarray(inputs["c"], dtype=np.float32)
    ident, triu, rc = _consts()
    n = x.shape[0]
    common = {"ident": ident, "triu": triu, "rc": rc, "finalgB": _bc(np.asarray(inputs["final_g"], dtype=np.float32))}
    common.update(_consts2())
    if FUSED:
        plan = [((0, 1), True)]
    else:
        plan = [((0,), False), ((1,), True)]
    cur = [np.ascontiguousarray(x[b]) for b in range(n)]
    for layers, fin in plan:
        nc = _get_nc((layers, fin), layers=layers, final=fin)
        shared = dict(common)
        for l in layers:
            shared.update(layer_inputs(inputs, l))
        in_maps = []
        for b in range(n):
            m = dict(shared)
            m["x"] = cur[b]
            m["cT"] = _pp(c[b])
            in_maps.append(m)
        res = run_bass_kernel_spmd(nc, in_maps, core_ids=list(range(n)))
        cur = [np.asarray(res.results[b]["out"], dtype=np.float32) for b in range(n)]
    return np.stack(cur, axis=0)
```

```python
from contextlib import ExitStack
import numpy as np
import concourse.bass as bass
import concourse.mybir as mybir
from concourse.bass_utils import run_bass_kernel_spmd

F32 = mybir.dt.float32
BF16 = mybir.dt.bfloat16
U8 = mybir.dt.uint8
I32 = mybir.dt.int32
AF = mybir.ActivationFunctionType
ALU = mybir.AluOpType

D = 1024
SEQ = 4096
NE = 32
EPS = 1e-5
POOL_WINDOWS = (2, 4, 8, 16)
ARENA_BYTES = 210944
CAP = 1536
SPARSE = True


class Sched:
    ENGS = ("pe", "act", "dve", "pool", "sp")

    def __init__(self, nc):
        self.nc = nc
        self.ops = []
        self.lastw = {}
        self.readers = {}
        self.stream = {e: [] for e in self.ENGS}
        self.dma_keys = {}

    def op(self, eng, fn, reads=(), writes=(), dma=None, extra=()):
        i = len(self.ops)
        deps = set(extra)
        for r in reads:
            w = self.lastw.get(r)
            if w is not None:
                deps.add(w)
        for w_ in writes:
            w = self.lastw.get(w_)
            if w is not None:
                deps.add(w)
            for rd in self.readers.get(w_, ()):
                deps.add(rd)
        deps.discard(i)
        for r in reads:
            self.readers.setdefault(r, []).append(i)
        for w_ in writes:
            self.lastw[w_] = i
            self.readers[w_] = []
        o = dict(i=i, eng=eng, fn=fn, dma=dma, deps=deps, pos=len(self.stream[eng]),
                 inc=False, waits=[])
        self.ops.append(o)
        self.stream[eng].append(i)
        if dma is not None:
            self.dma_keys.setdefault(dma, []).append(i)
            o["dcount"] = 16 * len(self.dma_keys[dma])
        return i

    def barrier(self):
        lasts = []
        for e in self.ENGS:
            for i in reversed(self.stream[e]):
                if self.ops[i]["dma"] is None and self.ops[i]["fn"] is not None:
                    lasts.append(i)
                    break
        for k, lst in self.dma_keys.items():
            lasts.append(lst[-1])
        for e in self.ENGS:
            self.op(e, None, extra=lasts)
        self.lastw = {}
        self.readers = {}

    def finalize(self):
        ops = self.ops
        need = {}
        for o in ops:
            lst = []
            for p in o["deps"]:
                P = ops[p]
                if P["fn"] is None:
                    continue
                if P["dma"] is not None:
                    lst.append(p)
                elif P["eng"] == o["eng"] and o["dma"] is None:
                    if o["eng"] == "pe":
                        continue
                    if o["pos"] - P["pos"] <= 3:
                        lst.append(p)
                        P["inc"] = True
                else:
                    lst.append(p)
                    P["inc"] = True
            need[o["i"]] = lst
        for e in self.ENGS:
            c = 0
            for i in self.stream[e]:
                o = ops[i]
                if o["dma"] is None and o["inc"]:
                    c += 1
                    o["count"] = c
        waited = {e: {} for e in self.ENGS}
        for e in self.ENGS:
            for i in self.stream[e]:
                o = ops[i]
                ws = {}
                for p in need[i]:
                    P = ops[p]
                    if P["dma"] is not None:
                        s, v = "D_" + P["dma"], P["dcount"]
                    else:
                        s, v = "E_" + P["eng"], P["count"]
                    if v > ws.get(s, 0):
                        ws[s] = v
                for s, v in ws.items():
                    if waited[e].get(s, 0) >= v:
                        continue
                    waited[e][s] = v
                    o["waits"].append((s, v))

    def emit(self):
        self.finalize()
        nc = self.nc
        names = ["E_" + e for e in self.ENGS] + ["D_" + k for k in self.dma_keys]
        with ExitStack() as es:
            sems = {n: es.enter_context(nc.semaphore(n)) for n in names}
            block = es.enter_context(nc.Block())
            ops = self.ops

            def make(en):
                def body(e):
                    for i in self.stream[en]:
                        o = ops[i]
                        for s, v in o["waits"]:
                            e.wait_ge(sems[s], v)
                        if o["fn"] is None:
                            continue
                        ins = o["fn"](e)
                        if o["dma"] is not None:
                            ins.then_inc(sems["D_" + o["dma"]], 16)
                        elif o["inc"]:
                            ins.then_inc(sems["E_" + en], 1)
                return body

            block.tensor(make("pe"))
            block.scalar(make("act"))
            block.vector(make("dve"))
            block.gpsimd(make("pool"))
            block.sync(make("sp"))


class DummySched:
    def op(self, *a, **k):
        return 0

    def barrier(self):
        pass


class Ring:
    def __init__(self, name, aps, names=None):
        self.name, self.aps, self.i, self.names = name, aps, 0, names

    def next(self):
        k = self.i % len(self.aps)
        self.i += 1
        return self.aps[k], (self.names[k] if self.names else "%s%d" % (self.name, k))


class WStream:
    def __init__(self, S, slots, plan=None):
        self.S, self.slots, self.n = S, slots, len(slots)
        self.plan = plan
        self.rec = []
        self.issued = 0
        self.cons = 0
        self.released = set()

    def _pump(self):
        if self.plan is None:
            return
        while self.issued < len(self.plan) and self.issued < self.cons + self.n:
            k = self.issued
            if k >= self.n and (k - self.n) not in self.released:
                break
            slot = k % self.n
            dst = self.slots[slot]
            s_ = self.plan[k]
            self.S.op("pool", lambda e, dst=dst, s_=s_: e.dma_start(out=dst, in_=s_),
                      writes=["W%d" % slot], dma="W%d" % slot)
            self.issued += 1

    def get(self, src):
        i = self.cons
        self.cons += 1
        if self.plan is None:
            self.rec.append(src)
        else:
            self._pump()
            assert self.issued > i, "weight piece not issued (missing release?)"
        return self.slots[i % self.n], "W%d" % (i % self.n), i

    def release(self, *hs):
        for h in hs:
            self.released.add(h)
        self._pump()


class Arena:
    def __init__(self, ar):
        self.ar, self.cur = ar, 0

    def alloc(self, shape, dt, parts=128):
        esz = 4 if dt in (F32, I32) else 2
        n = 1
        for s in shape[1:]:
            n *= s
        nb = n * esz
        nb_al = (nb + 63) // 64 * 64
        off = self.cur
        self.cur += nb_al
        assert self.cur <= ARENA_BYTES, ("SBUF arena overflow", self.cur)
        v = self.ar[0:parts, off:off + nb].bitcast(dt)
        if len(shape) == 3:
            v = v.rearrange("p (a b) -> p a b", a=shape[1])
        elif len(shape) == 4:
            v = v.rearrange("p (a b c) -> p a b c", a=shape[1], b=shape[2])
        return v


def build(layers=(0, 1), final=True, n_tiles=8, n_super=4, n_exp=NE, do_mixer=True, do_moe=True, debug=False):
    nc = bass.Bass("TRN2", target_bir_lowering=False)

    def din(name, shape, dt=F32):
        return nc.dram_tensor(name, list(shape), dt, kind="ExternalInput").ap()

    x_in = din("x", [SEQ, D])
    cT = din("cT", [128, 8])
    ident_d = din("ident", [128, 128])
    triu_d = din("triu", [128, 128])
    rc_d = din("rc", [128, 4, 16])
    ustr_d = din("ustr", [128, 128])
    ebase_d = din("ebase", [128, NE])
    tokid_d = din("tokid", [128, 32], I32)
    finalg_d = din("finalgB", [128, D])
    L = {}
    for l in layers:
        L[l] = dict(
            ada_w=din("ada_w%d" % l, [D, 6 * D]),
            ada_bP=din("ada_bP%d" % l, [128, 48]),
            adab_g1B=din("adab_g1B%d" % l, [128, D]),
            adab_g2B=din("adab_g2B%d" % l, [128, D]),
            n1gP=din("n1gP%d" % l, [128, 8]),
            n2gP=din("n2gP%d" % l, [128, 8]),
            w_in=din("w_in%d" % l, [D, 9 * D]),
            gnormB=din("gnormB%d" % l, [128, D]),
            ws=din("ws%d" % l, [8, 128, 128]),
            bsB=din("bsB%d" % l, [128, D]),
            w_proj_a=din("w_proj_a%d" % l, [D, D]),
            pool_w=din("pool_w%d" % l, [4, 256, 256]),
            pscP=din("pscP%d" % l, [128, 8]),
            convP=din("convP%d" % l, [128, 3, 8]),
            w_proj_c=din("w_proj_c%d" % l, [D, D]),
            w_out=din("w_out%d" % l, [D, D]),
            router_w=din("router_w%d" % l, [D, NE]),
            rbB=din("rbB%d" % l, [128, NE]),
            w_gu=din("w_gu%d" % l, [NE, D, 2 * D]),
            bguP=din("bguP%d" % l, [128, NE, 16]),
            w_down=din("w_down%d" % l, [NE, D, D]),
            b_down=din("b_down%d" % l, [NE, D]),
        )
    out_d = nc.dram_tensor("out", [SEQ, D], F32, kind="ExternalOutput").ap()
    xs_d = nc.dram_tensor("xs_scratch", [SEQ, D], F32, kind="Internal").ap()
    ys_d = nc.dram_tensor("ys_scratch", [NE * CAP, D], F32, kind="Internal").ap()
    tokslot_d = nc.dram_tensor("tokslot_scratch", [NE * CAP, 1], I32, kind="Internal").ap()

    dbg_t = {}

    def dump(S, name, ap, reads):
        if not debug:
            return
        if name not in dbg_t:
            dbg_t[name] = nc.dram_tensor("dbg_" + name, list(ap.shape), ap.dtype, kind="ExternalOutput").ap()
        d = dbg_t[name]
        S.op("sp", lambda e: e.dma_start(out=d, in_=ap), reads=reads, writes=["dbg_" + name], dma="dbg_" + name)

    def kview(w):
        return w.rearrange("(kc p) n -> p kc n", p=128)

    with ExitStack() as es:
        arena_t = es.enter_context(nc.sbuf_tensor("arena", [128, ARENA_BYTES], U8))
        banks = [es.enter_context(nc.psum_tensor("pb%d" % i, [128, 512], F32)) for i in range(6)]
        tpb = es.enter_context(nc.psum_tensor("tpb", [128, 8, 128], BF16))
        tpb2 = es.enter_context(nc.psum_tensor("tpb2", [128, 8, 128], BF16))

        def program(S, plan):
            A = Arena(arena_t)
            pool_regs = {}
            wslots = [A.alloc([128, 8, 512], BF16) for _ in range(6)]
            W = WStream(S, wslots, plan)
            identf = A.alloc([128, 128], F32)
            identb = A.alloc([128, 128], BF16)
            triu = A.alloc([128, 128], F32)
            rc = A.alloc([128, 4, 16], F32)
            cTf = A.alloc([128, 8], F32)
            condf = A.alloc([128, 8], F32)
            condb = A.alloc([128, 8], BF16)
            condB = A.alloc([128, 8, 128], BF16)
            modsP = A.alloc([128, 48], F32)
            adabP = A.alloc([128, 48], F32)
            n1g = A.alloc([128, 8], F32)
            n2g = A.alloc([128, 8], F32)
            A1 = A.alloc([128, 8], F32)
            A2 = A.alloc([128, 8], F32)
            GB = A.alloc([128, D], F32)
            abrow = A.alloc([128, D], F32)
            sscol = A.alloc([128, 64], F32)
            rscol = A.alloc([128, 64], F32)
            statc = [0]
            mark_common = A.cur

            PB = Ring("P", [b[:] for b in banks])

            def stat_col():
                k = statc[0] % 64
                statc[0] += 1
                return k

            def mm_group(out_ap, out_res, pairs, reads):
                n = len(pairs)
                for i, (lt, rh) in enumerate(pairs):
                    S.op("pe", lambda e, o=out_ap, lt=lt, rh=rh, st=(i == 0), sp_=(i == n - 1):
                         e.matmul(o, lhsT=lt, rhs=rh, start=st, stop=sp_),
                         reads=reads, writes=[out_res])

            S.op("sp", lambda e: e.dma_start(out=identf, in_=ident_d), writes=["identf"], dma="identf")
            S.op("sp", lambda e: e.dma_start(out=triu, in_=triu_d), writes=["triu"], dma="triu")
            S.op("sp", lambda e: e.dma_start(out=rc, in_=rc_d), writes=["rc"], dma="rc")
            S.op("sp", lambda e: e.dma_start(out=cTf, in_=cT), writes=["cTf"], dma="cTf")
            S.op("dve", lambda e: e.tensor_copy(out=identb, in_=identf), reads=["identf"], writes=["identb"])
            S.op("act", lambda e: e.activation(out=condf, in_=cTf, func=AF.Silu), reads=["cTf"], writes=["condf"])
            S.op("dve", lambda e: e.tensor_copy(out=condb, in_=condf), reads=["condf"], writes=["condb"])
            for kc in range(8):
                S.op("dve", lambda e, kc=kc: e.tensor_copy(out=condB[:, kc, :], in_=condf[:, kc:kc + 1].to_broadcast([128, 128])),
                     reads=["condf"], writes=["condB"])

            def rms_rstd(src_ap, src_res, junk_ap, junk_res):
                k = stat_col()
                ssa, rsa = sscol[:, k:k + 1], rscol[:, k:k + 1]
                sres, rres = "ss%d" % k, "rs%d" % k
                S.op("dve", lambda e: e.memset(ssa, 0.0), writes=[sres])
                S.op("act", lambda e: e.activation(out=junk_ap, in_=src_ap, func=AF.Square, accum_out=ssa),
                     reads=[src_res], writes=[junk_res, sres])
                S.op("dve", lambda e: e.tensor_scalar(out=rsa, in0=ssa, scalar1=1.0 / D, scalar2=EPS, op0=ALU.mult, op1=ALU.add),
                     reads=[sres], writes=[rres])
                S.op("act", lambda e: e.activation(out=rsa, in_=rsa, func=AF.Sqrt), reads=[rres], writes=[rres])
                S.op("dve", lambda e: e.reciprocal(out=rsa, in_=rsa), reads=[rres], writes=[rres])
                return rsa, rres

            def mods_rows(l, qs):
                src_b = L[l]["adab_g1B"] if qs[0] == 4 else L[l]["adab_g2B"]
                S.op("sp", lambda e: e.dma_start(out=abrow, in_=src_b), writes=["abrow"], dma="abrow")
                for hi, q in enumerate(qs):
                    wp, wres, wh = W.get(kview(L[l]["ada_w"])[:, :, q * 512:(q + 1) * 512])
                    pb, pres = PB.next()
                    mm_group(pb, pres, [(condB[:, kc, :], wp[:, kc, :]) for kc in range(8)], [wres, "condB"])
                    W.release(wh)
                    S.op("dve", lambda e, pb=pb, hi=hi: e.tensor_tensor(out=GB[:, hi * 512:(hi + 1) * 512], in0=pb,
                                                                          in1=abrow[:, hi * 512:(hi + 1) * 512], op=ALU.add),
                         reads=[pres, "abrow"], writes=["GB"])

            def layer_prologue(l):
                P = L[l]
                S.op("sp", lambda e: e.dma_start(out=adabP, in_=P["ada_bP"]), writes=["adabP"], dma="adabP")
                S.op("sp", lambda e: e.dma_start(out=n1g, in_=P["n1gP"]), writes=["n1g"], dma="n1g")
                S.op("sp", lambda e: e.dma_start(out=n2g, in_=P["n2gP"]), writes=["n2g"], dma="n2g")
                mp, mres = PB.next()
                for q in range(12):
                    wp, wres, wh = W.get(kview(P["ada_w"])[:, :, q * 512:(q + 1) * 512])
                    for jj in range(4):
                        j = 4 * q + jj
                        mm_group(mp[:, j:j + 1], mres,
                                 [(wp[:, kc, jj * 128:(jj + 1) * 128], condb[:, kc:kc + 1]) for kc in range(8)],
                                 [wres, "condb"])
                    W.release(wh)
                S.op("dve", lambda e: e.tensor_tensor(out=modsP, in0=mp[:, 0:48], in1=adabP, op=ALU.add),
                     reads=[mres, "adabP"], writes=["modsP"])
                S.op("dve", lambda e: e.scalar_tensor_tensor(out=A1, in0=modsP[:, 8:16], scalar=1.0, in1=n1g, op0=ALU.add, op1=ALU.mult),
                     reads=["modsP", "n1g"], writes=["A1"])
                S.op("dve", lambda e: e.scalar_tensor_tensor(out=A2, in0=modsP[:, 32:40], scalar=1.0, in1=n2g, op0=ALU.add, op1=ALU.mult),
                     reads=["modsP", "n2g"], writes=["A2"])

            def norm_transpose(xa, xres, xn_ring, junk, Ascale, Ares, shoff, hT, hres, col0):
                rsa, rres = rms_rstd(xa, xres, junk, "junk")
                xn, xnres = xn_ring.next()
                S.op("act", lambda e: e.activation(out=xn, in_=xa, func=AF.Identity, scale=rsa),
                     reads=[xres, rres], writes=[xnres])
                for kc in range(8):
                    S.op("pe", lambda e, kc=kc: e.transpose(out=tpb[:, kc, :], in_=xn[:, kc * 128:(kc + 1) * 128], identity=identb),
                         reads=[xnres, "identb"], writes=["tpb"])
                for kc in range(8):
                    dst = hT[:, kc, col0:col0 + 128]
                    if kc % 2 == 0:
                        S.op("dve", lambda e, kc=kc, dst=dst: e.tensor_scalar(out=dst, in0=tpb[:, kc, :], scalar1=Ascale[:, kc:kc + 1],
                                                                               scalar2=modsP[:, shoff + kc:shoff + kc + 1], op0=ALU.mult, op1=ALU.add),
                             reads=["tpb", Ares, "modsP"], writes=[hres])
                    else:
                        S.op("act", lambda e, kc=kc, dst=dst: e.activation(out=dst, in_=tpb[:, kc, :], func=AF.Identity,
                                                                            bias=modsP[:, shoff + kc:shoff + kc + 1], scale=Ascale[:, kc:kc + 1]),
                             reads=["tpb", Ares, "modsP"], writes=[hres])

            def mixer_phase(l, src_d, src_name, mdst_d):
                P = L[l]
                A.cur = mark_common
                xsub = Ring("xsub", [A.alloc([128, D], F32) for _ in range(5)])
                xnr = Ring("xn", [A.alloc([128, D], BF16) for _ in range(2)])
                junk = A.alloc([128, D], BF16)
                hT = A.alloc([128, 8, 512], BF16)
                gvr = Ring("gv", [A.alloc([128, D], F32) for _ in range(2)])
                vn = A.alloc([128, 4, D], BF16)
                gu = A.alloc([128, 8, 512], BF16)
                a8 = Ring("a8", [A.alloc([128, 8, 512], BF16) for _ in range(2)])
                mix = A.alloc([128, 8, 512], F32)
                T2 = Ring("T2", [A.alloc([128, 528], F32) for _ in range(9)])
                sgr = Ring("sg", [A.alloc([128, 512], BF16) for _ in range(3)])
                phalo = A.alloc([128, 8, 16], F32)
                zhalo = A.alloc([128, 8, 2], F32)
                poolW = A.alloc([128, 8, 256], BF16)
                WsT = A.alloc([128, 8, 128], BF16)
                wsb = A.alloc([128, 8, 128], BF16)
                gnormB = A.alloc([128, D], F32)
                bsb = A.alloc([128, 8, 128], F32)
                pscP = A.alloc([128, 8], F32)
                convP = A.alloc([128, 3, 8], F32)
                w_in_v = kview(P["w_in"])

                mods_rows(l, (4, 5))
                wsraw, wsres = gvr.next()
                wsraw3 = wsraw.rearrange("p (h s) -> p h s", h=8)
                S.op("sp", lambda e: e.dma_start(out=wsraw3, in_=P["ws"].rearrange("h t s -> t h s")), writes=[wsres], dma="wsraw")
                S.op("sp", lambda e: e.dma_start(out=gnormB, in_=P["gnormB"]), writes=["gnormB"], dma="gnormB")
                S.op("sp", lambda e: e.dma_start(out=bsb.rearrange("p h t -> p (h t)"), in_=P["bsB"]), writes=["bsb"], dma="bsb")
                S.op("sp", lambda e: e.dma_start(out=pscP, in_=P["pscP"]), writes=["pscP"], dma="pscP")
                S.op("sp", lambda e: e.dma_start(out=convP, in_=P["convP"]), writes=["convP"], dma="convP")
                S.op("pool", lambda e: e.dma_start(out=poolW, in_=P["pool_w"].rearrange("g (k p) n -> p (g k) n", p=128)),
                     writes=["poolW"], dma="poolW")
                S.op("dve", lambda e: e.tensor_copy(out=wsb, in_=wsraw3), reads=[wsres], writes=["wsb"])
                for h in range(8):
                    S.op("pe", lambda e, h=h: e.transpose(out=tpb[:, h, :], in_=wsb[:, h, :], identity=identb),
                         reads=["wsb", "identb"], writes=["tpb"])
                S.op("dve", lambda e: e.tensor_tensor(out=WsT, in0=tpb[:], in1=triu[:, None, :].to_broadcast([128, 8, 128]), op=ALU.mult),
                     reads=["tpb", "triu"], writes=["WsT"])
                S.op("dve", lambda e: e.memset(phalo, 0.0), writes=["phalo"])
                S.op("dve", lambda e: e.memset(zhalo, 0.0), writes=["zhalo"])

                def zchunk(wp, wres, jj):
                    pb, pres = PB.next()
                    mm_group(pb, pres, [(wp[:, kc, jj * 128:(jj + 1) * 128], hT[:, kc, :]) for kc in range(8)], [wres, "hT"])
                    return pb, pres

                for tt in range(n_tiles):
                    t0 = tt * 512
                    xs_list = []
                    for s in range(4):
                        xa, xres = xsub.next()
                        r0 = t0 + s * 128
                        dres = "%s%d" % (src_name, r0 // 128)
                        S.op("sp", lambda e, xa=xa, r0=r0: e.dma_start(out=xa, in_=src_d[r0:r0 + 128, :]),
                             reads=[dres], writes=[xres], dma=xres)
                        xs_list.append((xa, xres))
                        norm_transpose(xa, xres, xnr, junk, A1, "A1", 0, hT, "hT", s * 128)
                    if tt == 0:
                        dump(S, "modsP", modsP, ["modsP"]); dump(S, "GB", GB, ["GB"]); dump(S, "hT", hT, ["hT"])
                        dump(S, "WsT", WsT, ["WsT"]); dump(S, "A1", A1, ["A1"])
                    wv = [W.get(w_in_v[:, :, (2 + hf) * 512:(3 + hf) * 512]) for hf in range(2)]
                    for s in range(4):
                        gv, gvres = gvr.next()
                        for hf in range(2):
                            pb, pres = PB.next()
                            mm_group(pb, pres, [(hT[:, kc, s * 128:(s + 1) * 128], wv[hf][0][:, kc, :]) for kc in range(8)],
                                     [wv[hf][1], "hT"])
                            S.op("act", lambda e, pb=pb, gv=gv, hf=hf: e.activation(out=gv[:, hf * 512:(hf + 1) * 512], in_=pb, func=AF.Gelu_apprx_tanh),
                                 reads=[pres], writes=[gvres])
                        rsa, rres = rms_rstd(gv, gvres, junk, "junk")
                        S.op("dve", lambda e, gv=gv, rsa=rsa, s=s: e.scalar_tensor_tensor(out=vn[:, s, :], in0=gv, scalar=rsa, in1=gnormB,
                                                                                            op0=ALU.mult, op1=ALU.mult),
                             reads=[gvres, rres, "gnormB"], writes=["vn%d" % s])
                    W.release(wv[0][2], wv[1][2])
                    for hf in range(2):
                        wp, wres, wh = W.get(w_in_v[:, :, hf * 512:(hf + 1) * 512])
                        for jj in range(4):
                            j = hf * 4 + jj
                            pb, pres = zchunk(wp, wres, jj)
                            S.op("act", lambda e, pb=pb, j=j: e.activation(out=gu[:, j, :], in_=pb, func=AF.Gelu_apprx_tanh),
                                 reads=[pres], writes=["gu"])
                        W.release(wh)
                    if tt == 0:
                        dump(S, "vn", vn, ["vn0", "vn1", "vn2", "vn3"]); dump(S, "gu", gu, ["gu"])
                    ain, ares = a8.next()
                    for s in range(4):
                        for hg in range(2):
                            pb, pres = PB.next()
                            for hh in range(4):
                                h = hg * 4 + hh
                                S.op("pe", lambda e, pb=pb, hh=hh, h=h, s=s: e.matmul(pb[:, hh * 128:(hh + 1) * 128], lhsT=vn[:, s, h * 128:(h + 1) * 128],
                                                                                       rhs=WsT[:, h, :], start=True, stop=True),
                                     reads=["vn%d" % s, "WsT"], writes=[pres])
                            t1, t1res = T2.next()
                            S.op("dve", lambda e, pb=pb, t1=t1, hg=hg: e.tensor_tensor(out=t1[:, 0:512], in0=pb,
                                                                                        in1=bsb[:, hg * 4:(hg + 1) * 4, :].rearrange("p h t -> p (h t)"), op=ALU.add),
                                 reads=[pres, "bsb"], writes=[t1res])
                            S.op("dve", lambda e, t1=t1, hg=hg, s=s, ain=ain: e.tensor_tensor(
                                out=ain[:, hg * 4:(hg + 1) * 4, s * 128:(s + 1) * 128],
                                in0=t1[:, 0:512].rearrange("p (h t) -> p h t", h=4),
                                in1=gu[:, hg * 4:(hg + 1) * 4, s * 128:(s + 1) * 128], op=ALU.mult),
                                 reads=[t1res, "gu"], writes=[ares])
                    for n in range(8):
                        if n % 4 == 0:
                            if n:
                                W.release(wa1[2], wg1[2])
                            wa1 = W.get(kview(P["w_proj_a"])[:, :, (n // 4) * 512:(n // 4 + 1) * 512])
                            wg1 = W.get(w_in_v[:, :, (12 + n // 4) * 512:(13 + n // 4) * 512])
                            wa = {n // 4: wa1}
                            wg = {n // 4: wg1}
                        gp, gres = zchunk(wg[n // 4][0], wg[n // 4][1], n % 4)
                        sg, sgres = sgr.next()
                        S.op("act", lambda e, gp=gp, sg=sg: e.activation(out=sg, in_=gp, func=AF.Sigmoid), reads=[gres], writes=[sgres])
                        pb, pres = PB.next()
                        mm_group(pb, pres, [(wa[n // 4][0][:, kc, (n % 4) * 128:(n % 4 + 1) * 128], ain[:, kc, :]) for kc in range(8)],
                                 [wa[n // 4][1], ares])
                        S.op("dve", lambda e, pb=pb, sg=sg, n=n: e.tensor_tensor(out=mix[:, n, :], in0=pb, in1=sg, op=ALU.mult),
                             reads=[pres, sgres], writes=["mix%d" % n])
                    W.release(wa1[2], wg1[2])
                    if tt == 0:
                        dump(S, "ain", ain, [ares]); dump(S, "mixA", mix, ["mix%d" % n for n in range(8)])
                    dT, dres = a8.next()
                    for hf in range(2):
                        wp, wres, wph = W.get(w_in_v[:, :, (4 + hf) * 512:(5 + hf) * 512])
                        for jj in range(4):
                            j = hf * 4 + jj
                            wdw = POOL_WINDOWS[j // 2]
                            pp, ppres = zchunk(wp, wres, jj)
                            pbuf, pbres = T2.next()
                            S.op("dve", lambda e, pbuf=pbuf, j=j: e.tensor_copy(out=pbuf[:, 0:16], in_=phalo[:, j, :]), reads=["phalo"], writes=[pbres])
                            S.op("act", lambda e, pbuf=pbuf, pp=pp: e.activation(out=pbuf[:, 16:528], in_=pp, func=AF.Copy), reads=[ppres], writes=[pbres])
                            S.op("dve", lambda e, pbuf=pbuf, j=j: e.tensor_copy(out=phalo[:, j, :], in_=pbuf[:, 512:528]), reads=[pbres], writes=["phalo"])
                            cur, cres = pbuf, pbres
                            sh, lo = 1, 0
                            while sh < wdw:
                                nx, nres = T2.next()
                                S.op("pool", lambda e, nx=nx, cur=cur, sh=sh, lo=lo: e.tensor_tensor(out=nx[:, lo + sh:528], in0=cur[:, lo + sh:528],
                                                                                                      in1=cur[:, lo:528 - sh], op=ALU.add),
                                     reads=[cres], writes=[nres])
                                cur, cres = nx, nres
                                lo += sh
                                sh *= 2
                            S.op("dve", lambda e, cur=cur, pbuf=pbuf, j=j, wdw=wdw, dT=dT: e.scalar_tensor_tensor(
                                out=dT[:, j, :], in0=cur[:, 16:528], scalar=1.0 / wdw, in1=pbuf[:, 16:528], op0=ALU.mult, op1=ALU.subtract),
                                 reads=[cres, pbres], writes=[dres])
                            if tt == 0:
                                fx, fres = T2.next()
                                S.op("dve", lambda e, fx=fx, cur=cur, j=j: e.tensor_tensor(out=fx[:, 0:16], in0=cur[:, 16:32], in1=rc[:, j // 2, :], op=ALU.mult),
                                     reads=[cres, "rc"], writes=[fres])
                                S.op("dve", lambda e, fx=fx, pbuf=pbuf, j=j, dT=dT: e.tensor_tensor(out=dT[:, j, 0:16], in0=fx[:, 0:16], in1=pbuf[:, 16:32], op=ALU.subtract),
                                     reads=[fres, pbres, dres], writes=[dres])
                        W.release(wph)
                    for n in range(8):
                        g = n // 2
                        if n % 4 == 0:
                            if n:
                                W.release(wgb1[2])
                            wgb1 = W.get(w_in_v[:, :, (14 + n // 4) * 512:(15 + n // 4) * 512])
                            wgb = {n // 4: wgb1}
                        gp, gres = zchunk(wgb[n // 4][0], wgb[n // 4][1], n % 4)
                        sg, sgres = sgr.next()
                        S.op("act", lambda e, gp=gp, sg=sg: e.activation(out=sg, in_=gp, func=AF.Sigmoid), reads=[gres], writes=[sgres])
                        pb, pres = PB.next()
                        mm_group(pb, pres, [(poolW[:, 2 * g + k2, (n % 2) * 128:(n % 2 + 1) * 128], dT[:, 2 * g + k2, :]) for k2 in range(2)],
                                 ["poolW", dres])
                        tm, tres = T2.next()
                        S.op("dve", lambda e, pb=pb, sg=sg, n=n, tm=tm: e.scalar_tensor_tensor(out=tm[:, 0:512], in0=pb, scalar=pscP[:, n:n + 1], in1=sg,
                                                                                                op0=ALU.mult, op1=ALU.mult),
                             reads=[pres, sgres, "pscP"], writes=[tres])
                        S.op("pool", lambda e, tm=tm, n=n: e.tensor_tensor(out=mix[:, n, :], in0=mix[:, n, :], in1=tm[:, 0:512], op=ALU.add),
                             reads=[tres, "mix%d" % n], writes=["mix%d" % n])
                    W.release(wgb1[2])
                    if tt == 0:
                        dump(S, "dT", dT, [dres]); dump(S, "mixB", mix, ["mix%d" % n for n in range(8)])
                    bc, bcres = a8.next()
                    for hf in range(2):
                        wx, wxres, wxh = W.get(w_in_v[:, :, (6 + hf) * 512:(7 + hf) * 512])
                        wb, wbres, wbh = W.get(w_in_v[:, :, (8 + hf) * 512:(9 + hf) * 512])
                        wc, wcres, wch = W.get(w_in_v[:, :, (10 + hf) * 512:(11 + hf) * 512])
                        for jj in range(4):
                            j = hf * 4 + jj
                            cp, cpres = zchunk(wc, wcres, jj)
                            cs, csres = T2.next()
                            S.op("act", lambda e, cs=cs, cp=cp: e.activation(out=cs[:, 0:512], in_=cp, func=AF.Copy), reads=[cpres], writes=[csres])
                            xp, xpres = zchunk(wx, wxres, jj)
                            zc, zcres = T2.next()
                            S.op("dve", lambda e, zc=zc, j=j: e.tensor_copy(out=zc[:, 0:2], in_=zhalo[:, j, :]), reads=["zhalo"], writes=[zcres])
                            S.op("dve", lambda e, zc=zc, xp=xp, cs=cs: e.tensor_tensor(out=zc[:, 2:514], in0=xp, in1=cs[:, 0:512], op=ALU.mult),
                                 reads=[xpres, csres], writes=[zcres])
                            S.op("dve", lambda e, zc=zc, j=j: e.tensor_copy(out=zhalo[:, j, :], in_=zc[:, 512:514]), reads=[zcres], writes=["zhalo"])
                            ca, cares = T2.next()
                            S.op("dve", lambda e, ca=ca, zc=zc, j=j: e.tensor_scalar(out=ca[:, 0:512], in0=zc[:, 2:514], scalar1=convP[:, 2, j:j + 1], scalar2=None, op0=ALU.mult),
                                 reads=[zcres, "convP"], writes=[cares])
                            S.op("dve", lambda e, ca=ca, zc=zc, j=j: e.scalar_tensor_tensor(out=ca[:, 0:512], in0=zc[:, 1:513], scalar=convP[:, 1, j:j + 1], in1=ca[:, 0:512],
                                                                                             op0=ALU.mult, op1=ALU.add),
                                 reads=[zcres, "convP", cares], writes=[cares])
                            S.op("dve", lambda e, ca=ca, zc=zc, j=j: e.scalar_tensor_tensor(out=ca[:, 0:512], in0=zc[:, 0:512], scalar=convP[:, 0, j:j + 1], in1=ca[:, 0:512],
                                                                                             op0=ALU.mult, op1=ALU.add),
                                 reads=[zcres, "convP", cares], writes=[cares])
                            bp, bpres = zchunk(wb, wbres, jj)
                            S.op("dve", lambda e, bp=bp, ca=ca, j=j, bc=bc: e.tensor_tensor(out=bc[:, j, :], in0=bp, in1=ca[:, 0:512], op=ALU.mult),
                                 reads=[bpres, cares], writes=[bcres])
                        W.release(wxh, wbh, wch)
                    mixb, mbres = a8.next()
                    for n in range(8):
                        if n % 4 == 0:
                            if n:
                                W.release(wcc1[2], wgc1[2])
                            wcc1 = W.get(kview(P["w_proj_c"])[:, :, (n // 4) * 512:(n // 4 + 1) * 512])
                            wgc1 = W.get(w_in_v[:, :, (16 + n // 4) * 512:(17 + n // 4) * 512])
                            wcc = {n // 4: wcc1}
                            wgc = {n // 4: wgc1}
                        gp, gres = zchunk(wgc[n // 4][0], wgc[n // 4][1], n % 4)
                        sg, sgres = sgr.next()
                        S.op("act", lambda e, gp=gp, sg=sg: e.activation(out=sg, in_=gp, func=AF.Sigmoid), reads=[gres], writes=[sgres])
                        pb, pres = PB.next()
                        mm_group(pb, pres, [(wcc[n // 4][0][:, kc, (n % 4) * 128:(n % 4 + 1) * 128], bc[:, kc, :]) for kc in range(8)],
                                 [wcc[n // 4][1], bcres])
                        tm, tres = T2.next()
                        S.op("dve", lambda e, pb=pb, sg=sg, tm=tm: e.tensor_tensor(out=tm[:, 0:512], in0=pb, in1=sg, op=ALU.mult),
                             reads=[pres, sgres], writes=[tres])
                        S.op("pool", lambda e, tm=tm, n=n, mixb=mixb: e.tensor_tensor(out=mixb[:, n, :], in0=mix[:, n, :], in1=tm[:, 0:512], op=ALU.add),
                             reads=[tres, "mix%d" % n], writes=[mbres])
                    W.release(wcc1[2], wgc1[2])
                    if tt == 0:
                        dump(S, "bc", bc, [bcres]); dump(S, "mixb", mixb, [mbres])
                    wo = [W.get(kview(P["w_out"])[:, :, hf * 512:(hf + 1) * 512]) for hf in range(2)]
                    for s in range(4):
                        xa, xres = xs_list[s]
                        for hf in range(2):
                            pb, pres = PB.next()
                            mm_group(pb, pres, [(mixb[:, kc, s * 128:(s + 1) * 128], wo[hf][0][:, kc, :]) for kc in range(8)],
                                     [wo[hf][1], mbres])
                            tm, tres = T2.next()
                            S.op("dve", lambda e, pb=pb, tm=tm, hf=hf: e.tensor_tensor(out=tm[:, 0:512], in0=pb, in1=GB[:, hf * 512:(hf + 1) * 512], op=ALU.mult),
                                 reads=[pres, "GB"], writes=[tres])
                            S.op("dve", lambda e, tm=tm, xa=xa, hf=hf: e.tensor_tensor(out=xa[:, hf * 512:(hf + 1) * 512], in0=xa[:, hf * 512:(hf + 1) * 512],
                                                                                        in1=tm[:, 0:512], op=ALU.add),
                                 reads=[tres, xres], writes=[xres])
                        r0 = t0 + s * 128
                        S.op("sp", lambda e, xa=xa, r0=r0: e.dma_start(out=mdst_d[r0:r0 + 128, :], in_=xa),
                             reads=[xres], writes=["xs%d" % (r0 // 128)], dma="st_" + xres)
                    W.release(wo[0][2], wo[1][2])

            def moe_phase(l, dst_d, dst_name, do_final):
                P = L[l]
                A.cur = mark_common
                xsub = Ring("xsub", [A.alloc([128, D], F32) for _ in range(3)])
                xnr = Ring("xn", [A.alloc([128, D], BF16) for _ in range(2)])
                junk = A.alloc([128, D], BF16)
                h2T = A.alloc([128, 8, 1024], BF16)
                actT = A.alloc([128, 8, 1024], BF16)
                acc = A.alloc([128, 8, D], F32)
                T2 = Ring("T2", [A.alloc([128, 512], F32) for _ in range(10)])
                Gtok = A.alloc([128, 8, NE], F32)
                GT = A.alloc([128, 8, 128], F32)
                lgr = Ring("lg", [A.alloc([128, NE], F32) for _ in range(2)])
                exr = Ring("ex", [A.alloc([128, NE], F32) for _ in range(2)])
                mkr = Ring("mk", [A.alloc([128, NE], F32) for _ in range(2)])
                m8r = Ring("m8", [A.alloc([128, 8], F32) for _ in range(2)])
                smr = Ring("sm", [A.alloc([128, 4], F32) for _ in range(2)])
                routerW = A.alloc([128, 8, NE], BF16)
                rbB = A.alloc([128, NE], F32)
                bguP = A.alloc([128, NE, 16], F32)
                bdown = A.alloc([128, D], F32)
                finalg = A.alloc([128, D], F32) if do_final else None

                mods_rows(l, (10, 11))
                S.op("pool", lambda e: e.dma_start(out=routerW, in_=kview(P["router_w"])), writes=["routerW"], dma="routerW")
                S.op("sp", lambda e: e.dma_start(out=rbB, in_=P["rbB"]), writes=["rbB"], dma="rbB")
                S.op("sp", lambda e: e.dma_start(out=bguP, in_=P["bguP"]), writes=["bguP"], dma="bguP")
                S.op("sp", lambda e: e.dma_start(out=bdown[0:NE, :], in_=P["b_down"]), writes=["bdown"], dma="bdown")
                if do_final:
                    S.op("sp", lambda e: e.dma_start(out=finalg, in_=finalg_d), writes=["finalg"], dma="finalg")

                for st in range(n_super):
                    t0 = st * 1024
                    for s in range(8):
                        xa, xres = xsub.next()
                        r0 = t0 + s * 128
                        S.op("sp", lambda e, xa=xa, r0=r0: e.dma_start(out=xa, in_=xs_d[r0:r0 + 128, :]),
                             reads=["xs%d" % (r0 // 128)], writes=[xres], dma=xres)
                        norm_transpose(xa, xres, xnr, junk, A2, "A2", 24, h2T, "h2T", s * 128)
                        pb, pres = PB.next()
                        mm_group(pb[:, 0:NE], pres, [(h2T[:, kc, s * 128:(s + 1) * 128], routerW[:, kc, :]) for kc in range(8)],
                                 ["h2T", "routerW"])
                        lg, lgres = lgr.next()
                        ex, exres = exr.next()
                        mk, mkres = mkr.next()
                        m8, m8res = m8r.next()
                        sm, smres = smr.next()
                        S.op("dve", lambda e, lg=lg, pb=pb: e.tensor_tensor(out=lg, in0=pb[:, 0:NE], in1=rbB, op=ALU.add),
                             reads=[pres, "rbB"], writes=[lgres])
                        S.op("dve", lambda e, lg=lg, m8=m8: e.max(out=m8, in_=lg), reads=[lgres], writes=[m8res])
                        S.op("dve", lambda e, lg=lg, m8=m8, mk=mk: e.tensor_scalar(out=mk, in0=lg, scalar1=m8[:, 3:4], scalar2=None, op0=ALU.is_ge),
                             reads=[lgres, m8res], writes=[mkres])
                        S.op("dve", lambda e, sm=sm, m8=m8: e.tensor_scalar(out=sm[:, 0:1], in0=m8[:, 0:1], scalar1=-1.0, scalar2=None, op0=ALU.mult),
                             reads=[m8res], writes=[smres])
                        S.op("act", lambda e, ex=ex, lg=lg, sm=sm: e.activation(out=ex, in_=lg, func=AF.Exp, bias=sm[:, 0:1], scale=1.0),
                             reads=[lgres, smres], writes=[exres])
                        S.op("dve", lambda e, sm=sm: e.memset(sm[:, 1:2], 0.0), reads=[smres], writes=[smres])
                        S.op("dve", lambda e, ex=ex, mk=mk, sm=sm: e.scalar_tensor_tensor(out=ex, in0=ex, scalar=1.0, in1=mk, op0=ALU.mult, op1=ALU.mult,
                                                                                           accum_out=sm[:, 1:2]),
                             reads=[exres, mkres, smres], writes=[exres, smres])
                        S.op("dve", lambda e, sm=sm: e.reciprocal(out=sm[:, 2:3], in_=sm[:, 1:2]), reads=[smres], writes=[smres])
                        S.op("dve", lambda e, ex=ex, sm=sm, s=s: e.tensor_scalar(out=Gtok[:, s, :], in0=ex, scalar1=sm[:, 2:3], scalar2=None, op0=ALU.mult),
                             reads=[exres, smres], writes=["Gtok%d" % s])
                        pt, ptres = PB.next()
                        S.op("pe", lambda e, pt=pt, s=s: e.transpose(out=pt[0:NE, 0:128], in_=Gtok[:, s, :], identity=identf),
                             reads=["Gtok%d" % s, "identf"], writes=[ptres])
                        S.op("act", lambda e, pt=pt, s=s: e.activation(out=GT[0:NE, s, :], in_=pt[0:NE, 0:128], func=AF.Copy),
                             reads=[ptres], writes=["GT%d" % s])
                        for hf in range(2):
                            pb2, p2res = PB.next()
                            S.op("pe", lambda e, pb2=pb2, s=s, hf=hf: e.matmul(pb2, lhsT=GT[0:NE, s, :], rhs=bdown[0:NE, hf * 512:(hf + 1) * 512],
                                                                                start=True, stop=True),
                                 reads=["GT%d" % s, "bdown"], writes=[p2res])
                            S.op("act", lambda e, pb2=pb2, s=s, hf=hf: e.activation(out=acc[:, s, hf * 512:(hf + 1) * 512], in_=pb2, func=AF.Copy),
                                 reads=[p2res], writes=["acc%d_%d" % (s, hf)])
                    for ex_i in range(n_exp):
                        wgv = kview(P["w_gu"][ex_i])
                        wdv = kview(P["w_down"][ex_i])
                        for hfj in range(2):
                            gw, gwres, gwh = W.get(wgv[:, :, hfj * 512:(hfj + 1) * 512])
                            uw, uwres, uwh = W.get(wgv[:, :, (2 + hfj) * 512:(3 + hfj) * 512])
                            for th in range(2):
                                for jj in range(4):
                                    j = hfj * 4 + jj
                                    gp, gpres = PB.next()
                                    mm_group(gp, gpres, [(gw[:, kc, jj * 128:(jj + 1) * 128], h2T[:, kc, th * 512:(th + 1) * 512]) for kc in range(8)],
                                             [gwres, "h2T"])
                                    up, upres = PB.next()
                                    mm_group(up, upres, [(uw[:, kc, jj * 128:(jj + 1) * 128], h2T[:, kc, th * 512:(th + 1) * 512]) for kc in range(8)],
                                             [uwres, "h2T"])
                                    gc, gcres = T2.next()
                                    sg, sgres = T2.next()
                                    uc, ucres = T2.next()
                                    rr, rrres = T2.next()
                                    S.op("dve", lambda e, gc=gc, gp=gp, j=j, ex_i=ex_i: e.tensor_scalar(out=gc, in0=gp, scalar1=bguP[:, ex_i, j:j + 1], scalar2=7.0,
                                                                                                        op0=ALU.add, op1=ALU.min),
                                         reads=[gpres, "bguP"], writes=[gcres])
                                    S.op("act", lambda e, sg=sg, gc=gc: e.activation(out=sg, in_=gc, func=AF.Silu, scale=1.702),
                                         reads=[gcres], writes=[sgres])
                                    S.op("dve", lambda e, uc=uc, up=up, j=j, ex_i=ex_i: e.tensor_scalar(out=uc, in0=up, scalar1=bguP[:, ex_i, 8 + j:9 + j], scalar2=7.0,
                                                                                                        op0=ALU.add, op1=ALU.min),
                                         reads=[upres, "bguP"], writes=[ucres])
                                    S.op("dve", lambda e, rr=rr, uc=uc: e.tensor_scalar(out=rr, in0=uc, scalar1=-7.0, scalar2=1.0, op0=ALU.max, op1=ALU.add),
                                         reads=[ucres], writes=[rrres])
                                    S.op("dve", lambda e, rr=rr, sg=sg, j=j, th=th: e.scalar_tensor_tensor(out=actT[:, j, th * 512:(th + 1) * 512], in0=rr,
                                                                                                        scalar=1.0 / 1.702, in1=sg, op0=ALU.mult, op1=ALU.mult),
                                         reads=[rrres, sgres], writes=["actT%d" % th])
                            W.release(gwh, uwh)
                        dpieces = [W.get(wdv[:, :, hf * 512:(hf + 1) * 512]) for hf in range(2)]
                        for s in range(8):
                            th = s // 4
                            for hf in range(2):
                                pb, pres = PB.next()
                                mm_group(pb, pres, [(actT[:, kc, s * 128:(s + 1) * 128], dpieces[hf][0][:, kc, :]) for kc in range(8)],
                                         [dpieces[hf][1], "actT%d" % th])
                                ar_ = "acc%d_%d" % (s, hf)
                                S.op("dve", lambda e, pb=pb, s=s, hf=hf, ex_i=ex_i: e.scalar_tensor_tensor(
                                    out=acc[:, s, hf * 512:(hf + 1) * 512], in0=pb, scalar=Gtok[:, s, ex_i:ex_i + 1],
                                    in1=acc[:, s, hf * 512:(hf + 1) * 512], op0=ALU.mult, op1=ALU.add),
                                     reads=[pres, "Gtok%d" % s, ar_], writes=[ar_])
                        W.release(dpieces[0][2], dpieces[1][2])
                    for s in range(8):
                        xa, xres = xsub.next()
                        r0 = t0 + s * 128
                        S.op("sp", lambda e, xa=xa, r0=r0: e.dma_start(out=xa, in_=xs_d[r0:r0 + 128, :]),
                             reads=["xs%d" % (r0 // 128)], writes=[xres], dma=xres)
                        S.op("dve", lambda e, s=s: e.tensor_tensor(out=acc[:, s, :], in0=acc[:, s, :], in1=GB, op=ALU.mult),
                             reads=["acc%d_0" % s, "acc%d_1" % s, "GB"], writes=["acc%d_0" % s, "acc%d_1" % s])
                        S.op("dve", lambda e, s=s, xa=xa: e.tensor_tensor(out=xa, in0=xa, in1=acc[:, s, :], op=ALU.add),
                             reads=["acc%d_0" % s, "acc%d_1" % s, xres], writes=[xres])
                        if do_final:
                            rsa, rres = rms_rstd(xa, xres, junk, "junk")
                            S.op("dve", lambda e, xa=xa, rsa=rsa: e.scalar_tensor_tensor(out=xa, in0=xa, scalar=rsa, in1=finalg, op0=ALU.mult, op1=ALU.mult),
                                 reads=[xres, rres, "finalg"], writes=[xres])
                        S.op("sp", lambda e, xa=xa, r0=r0: e.dma_start(out=dst_d[r0:r0 + 128, :], in_=xa),
                             reads=[xres], writes=["%s%d" % (dst_name, r0 // 128)], dma="st_" + xres)


            def moe_sparse_phase(l, dst_d, dst_name, do_final):
                P = L[l]
                A.cur = mark_common
                NT = SEQ // 128
                lgall = A.alloc([128, NT, NE], F32)
                m8all = A.alloc([128, NT, 8], F32)
                w4all = A.alloc([128, NT, 4], F32)
                mkb = A.alloc([128, NT, NE], BF16)
                slotf = A.alloc([128, NT, 4], F32)
                sloti = A.alloc([128, NT, 4], I32)
                tokid = A.alloc([128, NT], I32)
                ebase = A.alloc([128, NE], F32)
                ustrf = A.alloc([128, 128], F32)
                onesb = A.alloc([128, 128], BF16)
                Ub = A.alloc([128, 128], BF16)
                onesf = A.alloc([128, 128], F32)
                zeros_i = A.alloc([128, NE * CAP // 128], I32)
                smr = Ring("sm", [A.alloc([128, 4], F32) for _ in range(2)])
                routerW = A.alloc([128, 8, NE], BF16)
                rbB = A.alloc([128, NE], F32)
                bguP = A.alloc([128, NE, 16], F32)
                finalg = A.alloc([128, D], F32) if do_final else None
                junk32 = A.alloc([128, NE], F32)
                mark_persist = A.cur
                xsub = Ring("xsub", [A.alloc([128, D], F32) for _ in range(4)])
                xnr = Ring("xn", [A.alloc([128, D], BF16) for _ in range(8)])
                junk = A.alloc([128, D], BF16)
                hTr = Ring("hTr", [A.alloc([128, 8, 128], BF16) for _ in range(2)])
                h2r = Ring("h2c", [A.alloc([128, 8, 512], BF16) for _ in range(4)])
                actr = Ring("actc", [A.alloc([128, 8, 512], BF16) for _ in range(2)])
                tpr = Ring("tp", [tpb, tpb2], names=["tpb", "tpb2"])
                T2 = Ring("T2", [A.alloc([128, 512], F32) for _ in range(8)])
                tslr = Ring("tsl", [A.alloc([128, 1], I32) for _ in range(4)])
                ysr = Ring("ysr", [A.alloc([128, D], F32) for _ in range(4)])
                bdrow = Ring("bdrow", [A.alloc([128, D], BF16) for _ in range(2)])
                NSLOT = NE * CAP
                if "bc" not in pool_regs:
                    pool_regs["bc"] = None

                    def _init_bc(e):
                        r = e.alloc_register("bc")
                        pool_regs["bc"] = r
                        return e.reg_mov(r, NSLOT - 1)
                    S.op("pool", _init_bc)

                mods_rows(l, (10, 11))
                S.op("pool", lambda e: e.dma_start(out=routerW, in_=kview(P["router_w"])), writes=["routerW"], dma="routerW")
                S.op("sp", lambda e: e.dma_start(out=rbB, in_=P["rbB"]), writes=["rbB"], dma="rbB")
                S.op("sp", lambda e: e.dma_start(out=bguP, in_=P["bguP"]), writes=["bguP"], dma="bguP")
                S.op("sp", lambda e: e.dma_start(out=ebase, in_=ebase_d), writes=["ebase"], dma="ebase")
                S.op("sp", lambda e: e.dma_start(out=ustrf, in_=ustr_d), writes=["ustrf"], dma="ustrf")
                S.op("sp", lambda e: e.dma_start(out=tokid, in_=tokid_d), writes=["tokid"], dma="tokid")
                if do_final:
                    S.op("sp", lambda e: e.dma_start(out=finalg, in_=finalg_d), writes=["finalg"], dma="finalg")
                S.op("dve", lambda e: e.tensor_copy(out=Ub, in_=ustrf), reads=["ustrf"], writes=["Ub"])
                S.op("dve", lambda e: e.memset(onesb, 1.0), writes=["onesb"])
                S.op("dve", lambda e: e.memset(onesf, 1.0), writes=["onesf"])
                S.op("dve", lambda e: e.memset(zeros_i, 0), writes=["zeros_i"])
                S.op("sp", lambda e: e.dma_start(out=tokslot_d.rearrange("(p f) o -> p (f o)", p=128), in_=zeros_i),
                     reads=["zeros_i"], writes=["tokslot0"], dma="tokslot0")

                def partA1(e_, c_):
                    xs_ = []
                    for sb in range(4):
                        tsl, tslres = tslr.next()
                        s0 = e_ * CAP + c_ * 512 + sb * 128
                        S.op("sp", lambda e, tsl=tsl, s0=s0: e.dma_start(out=tsl, in_=tokslot_d[s0:s0 + 128, :]), reads=scat_res + ["tokslot0"], writes=[tslres], dma=tslres)
                        xa, xres = xsub.next()
                        S.op("pool", lambda e, xa=xa, tsl=tsl: e.indirect_dma_start(out=xa, out_offset=None, in_=xs_d[:, :],
                                                                                     in_offset=bass.IndirectOffsetOnAxis(ap=tsl[:, 0:1], axis=0)),
                             reads=[tslres, "XS_G"], writes=[xres], dma=xres)
                        xs_.append((xa, xres))
                    return xs_

                def partA2(xs_):
                    outs = []
                    for xa, xres in xs_:
                        xn, xnres = xnr.next()
                        rsa, rres = rms_rstd(xa, xres, xn, xnres)
                        S.op("act", lambda e, xn=xn, xa=xa, rsa=rsa: e.activation(out=xn, in_=xa, func=AF.Identity, scale=rsa),
                             reads=[xres, rres], writes=[xnres])
                        outs.append((xn, xnres))
                    return outs

                def partA(e_, c_):
                    return partA2(partA1(e_, c_))

                def partB(outs, hc, hres):
                    for sb, (xn, xnres) in enumerate(outs):
                        tp, tpres = tpr.next()
                        for kc in range(8):
                            S.op("pe", lambda e, kc=kc, tp=tp, xn=xn: e.transpose(out=tp[:, kc, :], in_=xn[:, kc * 128:(kc + 1) * 128], identity=identb),
                                 reads=[xnres, "identb"], writes=[tpres])
                        for kc in range(8):
                            dst = hc[:, kc, sb * 128:(sb + 1) * 128]
                            if False:
                                S.op("dve", lambda e, kc=kc, dst=dst, tp=tp: e.tensor_scalar(out=dst, in0=tp[:, kc, :], scalar1=A2[:, kc:kc + 1],
                                                                                              scalar2=modsP[:, 24 + kc:25 + kc], op0=ALU.mult, op1=ALU.add),
                                     reads=[tpres, "A2", "modsP"], writes=[hres])
                            else:
                                S.op("act", lambda e, kc=kc, dst=dst, tp=tp: e.activation(out=dst, in_=tp[:, kc, :], func=AF.Identity,
                                                                                           bias=modsP[:, 24 + kc:25 + kc], scale=A2[:, kc:kc + 1]),
                                     reads=[tpres, "A2", "modsP"], writes=[hres])

                for tt in range(NT):
                    xa, xres = xsub.next()
                    r0 = tt * 128
                    S.op("sp", lambda e, xa=xa, r0=r0: e.dma_start(out=xa, in_=xs_d[r0:r0 + 128, :]), reads=["xs%d" % tt], writes=[xres], dma=xres)
                    hs, hsres = hTr.next()
                    partB(partA2([(xa, xres)]), hs, hsres)
                    pb, pres = PB.next()
                    mm_group(pb[:, 0:NE], pres, [(hs[:, kc, :], routerW[:, kc, :]) for kc in range(8)], [hsres, "routerW"])
                    lg = lgall[:, tt, :]
                    m8 = m8all[:, tt, :]
                    sm, smres = smr.next()
                    rt = "rt%d" % tt
                    S.op("dve", lambda e, lg=lg, pb=pb: e.tensor_tensor(out=lg, in0=pb[:, 0:NE], in1=rbB, op=ALU.add), reads=[pres, "rbB"], writes=[rt])
                    S.op("dve", lambda e, lg=lg, m8=m8: e.max(out=m8, in_=lg), reads=[rt], writes=[rt])
                    S.op("dve", lambda e, lg=lg, m8=m8, tt=tt: e.tensor_scalar(out=mkb[:, tt, :], in0=lg, scalar1=m8[:, 3:4], scalar2=None, op0=ALU.is_ge),
                         reads=[rt], writes=["mkb%d" % tt])
                    S.op("dve", lambda e, sm=sm, m8=m8: e.tensor_scalar(out=sm[:, 0:1], in0=m8[:, 0:1], scalar1=-1.0, scalar2=None, op0=ALU.mult),
                         reads=[rt], writes=[smres])
                    S.op("dve", lambda e, sm=sm: e.memset(sm[:, 1:2], 0.0), reads=[smres], writes=[smres])
                    S.op("act", lambda e, m8=m8, sm=sm, tt=tt: e.activation(out=w4all[:, tt, :], in_=m8[:, 0:4], func=AF.Exp, bias=sm[:, 0:1], scale=1.0,
                                                                          accum_out=sm[:, 1:2]),
                         reads=[rt, smres], writes=["w4_%d" % tt, smres])
                    S.op("dve", lambda e, sm=sm: e.reciprocal(out=sm[:, 2:3], in_=sm[:, 1:2]), reads=[smres], writes=[smres])
                    S.op("dve", lambda e, sm=sm, tt=tt: e.tensor_scalar(out=w4all[:, tt, :], in0=w4all[:, tt, :], scalar1=sm[:, 2:3], scalar2=None, op0=ALU.mult),
                         reads=["w4_%d" % tt, smres], writes=["w4_%d" % tt])
                scat_res = []
                for tt in range(NT):
                    pr, prres = PB.next()
                    pairs = [(onesb, mkb[:, t2, :]) for t2 in range(tt)] + [(Ub, mkb[:, tt, :])]
                    mm_group(pr[:, 0:NE], prres, pairs, ["mkb%d" % t2 for t2 in range(tt + 1)] + ["onesb", "Ub"])
                    sf, sfres = T2.next()
                    ov, ovres = T2.next()
                    S.op("dve", lambda e, sf=sf, pr=pr: e.tensor_tensor(out=sf[:, 0:NE], in0=pr[:, 0:NE], in1=ebase, op=ALU.add), reads=[prres, "ebase"], writes=[sfres])
                    S.op("dve", lambda e, ov=ov, pr=pr: e.tensor_scalar(out=ov[:, 0:NE], in0=pr[:, 0:NE], scalar1=float(CAP), scalar2=1.0e7, op0=ALU.is_ge, op1=ALU.mult),
                         reads=[prres], writes=[ovres])
                    S.op("dve", lambda e, sf=sf, ov=ov: e.tensor_tensor(out=sf[:, 0:NE], in0=sf[:, 0:NE], in1=ov[:, 0:NE], op=ALU.add), reads=[sfres, ovres], writes=[sfres])
                    S.op("dve", lambda e, tt=tt: e.memset(slotf[:, tt, :], 0.0), writes=["slotf%d" % tt])
                    for k in range(4):
                        S.op("dve", lambda e, tt=tt, k=k, sf=sf: e.scalar_tensor_tensor(out=junk32, in0=lgall[:, tt, :], scalar=m8all[:, tt, k:k + 1], in1=sf[:, 0:NE],
                                                                                         op0=ALU.is_equal, op1=ALU.mult, accum_out=slotf[:, tt, k:k + 1]),
                             reads=["rt%d" % tt, sfres, "slotf%d" % tt], writes=["junk32", "slotf%d" % tt])
                    S.op("dve", lambda e, tt=tt: e.tensor_copy(out=sloti[:, tt, :], in_=slotf[:, tt, :]), reads=["slotf%d" % tt], writes=["sloti%d" % tt])
                    for k in range(4):
                        sr = "scat%d_%d" % (tt, k)
                        S.op("pool", lambda e, tt=tt, k=k: e.indirect_dma_start(out=tokslot_d[:, :], out_offset=bass.IndirectOffsetOnAxis(ap=sloti[:, tt, k:k + 1], axis=0),
                                                                               in_=tokid[:, tt:tt + 1], in_offset=None, bounds_check=pool_regs["bc"], oob_is_err=False),
                             reads=["sloti%d" % tt, "tokid", "tokslot0"], writes=[sr], dma="scat")
                        scat_res.append(sr)
                ys_res = []
                NCH = CAP // 512
                chunks = [(e_, c_) for e_ in range(n_exp) for c_ in range(NCH)]

                pend = {}
                hslot = {}
                pend[0] = partA(*chunks[0])
                hslot[0] = h2r.next()
                partB(pend.pop(0), *hslot[0])
                if len(chunks) > 1:
                    pend[1] = partA(*chunks[1])
                wp = {}
                for n, (ex_i, c_) in enumerate(chunks):
                    pa1 = partA1(*chunks[n + 2]) if n + 2 < len(chunks) else None
                    hc, hres = hslot.pop(n)
                    wgv = kview(P["w_gu"][ex_i])
                    wdv = kview(P["w_down"][ex_i])
                    if c_ == 0:
                        for hfj in range(2):
                            wp["g%d" % hfj] = W.get(wgv[:, :, hfj * 512:(hfj + 1) * 512])
                            wp["u%d" % hfj] = W.get(wgv[:, :, (2 + hfj) * 512:(3 + hfj) * 512])
                        bdr, bdres = bdrow.next()
                        S.op("pool", lambda e, bdr=bdr, ex_i=ex_i: e.dma_start(out=bdr[0:1, :], in_=P["b_down"][ex_i:ex_i + 1, :]), writes=[bdres], dma=bdres)
                    ac, acres = actr.next()
                    deferred = []
                    for hfj in range(2):
                        if hfj == 1 and pa1 is not None:
                            pend[n + 2] = partA2(pa1)
                        gw, gwres, gwh = wp["g%d" % hfj]
                        uw, uwres, uwh = wp["u%d" % hfj]
                        for jj in range(4):
                            j = hfj * 4 + jj
                            gp, gpres = PB.next()
                            mm_group(gp, gpres, [(gw[:, kc, jj * 128:(jj + 1) * 128], hc[:, kc, :]) for kc in range(8)], [gwres, hres])
                            up, upres = PB.next()
                            mm_group(up, upres, [(uw[:, kc, jj * 128:(jj + 1) * 128], hc[:, kc, :]) for kc in range(8)], [uwres, hres])
                            gc, gcres = T2.next()
                            uc, ucres = T2.next()
                            S.op("dve", lambda e, gc=gc, gp=gp, j=j, ex_i=ex_i: e.tensor_scalar(out=gc, in0=gp, scalar1=bguP[:, ex_i, j:j + 1], scalar2=7.0, op0=ALU.add, op1=ALU.min),
                                 reads=[gpres, "bguP"], writes=[gcres])
                            S.op("act", lambda e, gc=gc: e.activation(out=gc, in_=gc, func=AF.Silu, scale=1.702), reads=[gcres], writes=[gcres])
                            S.op("dve", lambda e, uc=uc, up=up, j=j, ex_i=ex_i: e.tensor_scalar(out=uc, in0=up, scalar1=bguP[:, ex_i, 8 + j:9 + j], scalar2=7.0, op0=ALU.add, op1=ALU.min),
                                 reads=[upres, "bguP"], writes=[ucres])
                            S.op("dve", lambda e, uc=uc: e.tensor_scalar(out=uc, in0=uc, scalar1=-7.0, scalar2=1.0, op0=ALU.max, op1=ALU.add), reads=[ucres], writes=[ucres])
                            if deferred:
                                dj, duc, ducres, dgc, dgcres = deferred.pop()
                                S.op("dve", lambda e, duc=duc, dgc=dgc, dj=dj, ac=ac: e.scalar_tensor_tensor(out=ac[:, dj, :], in0=duc, scalar=1.0 / 1.702, in1=dgc, op0=ALU.mult, op1=ALU.mult),
                                     reads=[ducres, dgcres], writes=[acres])
                            deferred.append((j, uc, ucres, gc, gcres))
                    dj, duc, ducres, dgc, dgcres = deferred.pop()
                    S.op("dve", lambda e, duc=duc, dgc=dgc, dj=dj, ac=ac: e.scalar_tensor_tensor(out=ac[:, dj, :], in0=duc, scalar=1.0 / 1.702, in1=dgc, op0=ALU.mult, op1=ALU.mult),
                         reads=[ducres, dgcres], writes=[acres])
                    if c_ == NCH - 1:
                        W.release(wp["g0"][2], wp["u0"][2], wp["g1"][2], wp["u1"][2])
                    if n + 1 < len(chunks):
                        hslot[n + 1] = h2r.next()
                        partB(pend.pop(n + 1), *hslot[n + 1])
                    if c_ == 0:
                        wp["d"] = [W.get(wdv[:, :, hf * 512:(hf + 1) * 512]) for hf in range(2)]
                    dpieces = wp["d"]
                    for sb in range(4):
                        yt, ytres = ysr.next()
                        for hf in range(2):
                            pb, pres = PB.next()
                            pairs = [(onesb[0:1, :], bdr[0:1, hf * 512:(hf + 1) * 512])] + \
                                    [(ac[:, kc, sb * 128:(sb + 1) * 128], dpieces[hf][0][:, kc, :]) for kc in range(8)]
                            mm_group(pb, pres, pairs, [dpieces[hf][1], acres, bdres, "onesb"])
                            S.op("act", lambda e, pb=pb, yt=yt, hf=hf: e.activation(out=yt[:, hf * 512:(hf + 1) * 512], in_=pb, func=AF.Copy), reads=[pres], writes=[ytres])
                        s0 = ex_i * CAP + c_ * 512 + sb * 128
                        yr = "ys_%d_%d_%d" % (ex_i, c_, sb)
                        S.op("sp", lambda e, yt=yt, s0=s0: e.dma_start(out=ys_d[s0:s0 + 128, :], in_=yt), reads=[ytres], writes=[yr], dma="st_" + ytres)
                        ys_res.append(yr)
                    if c_ == NCH - 1:
                        W.release(dpieces[0][2], dpieces[1][2])
                S.barrier()
                A.cur = mark_persist
                ykr = Ring("ykr", [A.alloc([128, D], F32) for _ in range(12)])
                xsub = Ring("xsubc", [A.alloc([128, D], F32) for _ in range(4)])
                accr = Ring("accr", [A.alloc([128, D], F32) for _ in range(3)])
                junk = A.alloc([128, D], BF16)
                def issue_gathers(tt):
                    lst = []
                    for k in range(4):
                        yk, ykres = ykr.next()
                        S.op("dve", lambda e, yk=yk: e.memset(yk, 0.0), writes=[ykres])
                        S.op("pool", lambda e, yk=yk, tt=tt, k=k: e.indirect_dma_start(out=yk, out_offset=None, in_=ys_d[:, :],
                                                                                        in_offset=bass.IndirectOffsetOnAxis(ap=sloti[:, tt, k:k + 1], axis=0),
                                                                                        bounds_check=pool_regs["bc"], oob_is_err=False),
                             reads=[ykres], writes=[ykres], dma=ykres)
                        lst.append((yk, ykres))
                    return lst

                gq = {0: issue_gathers(0), 1: issue_gathers(1)}
                for tt in range(NT):
                    if tt + 2 < NT:
                        gq[tt + 2] = issue_gathers(tt + 2)
                    acc, accres = accr.next()
                    for k, (yk, ykres) in enumerate(gq.pop(tt)):
                        if k == 0:
                            S.op("dve", lambda e, yk=yk, acc=acc, tt=tt: e.tensor_scalar(out=acc, in0=yk, scalar1=w4all[:, tt, 0:1], scalar2=None, op0=ALU.mult),
                                 reads=[ykres], writes=[accres])
                        else:
                            S.op("dve", lambda e, yk=yk, acc=acc, tt=tt, k=k: e.scalar_tensor_tensor(out=acc, in0=yk, scalar=w4all[:, tt, k:k + 1], in1=acc, op0=ALU.mult, op1=ALU.add),
                                 reads=[ykres, accres], writes=[accres])
                    xa, xres = xsub.next()
                    r0 = tt * 128
                    S.op("sp", lambda e, xa=xa, r0=r0: e.dma_start(out=xa, in_=xs_d[r0:r0 + 128, :]), reads=["xs%d" % tt], writes=[xres], dma=xres)
                    S.op("dve", lambda e, acc=acc: e.tensor_tensor(out=acc, in0=acc, in1=GB, op=ALU.mult), reads=[accres, "GB"], writes=[accres])
                    S.op("dve", lambda e, acc=acc, xa=xa: e.tensor_tensor(out=xa, in0=xa, in1=acc, op=ALU.add), reads=[accres, xres], writes=[xres])
                    if do_final:
                        rsa, rres = rms_rstd(xa, xres, junk, "junk")
                        S.op("dve", lambda e, xa=xa, rsa=rsa: e.scalar_tensor_tensor(out=xa, in0=xa, scalar=rsa, in1=finalg, op0=ALU.mult, op1=ALU.mult),
                             reads=[xres, rres, "finalg"], writes=[xres])
                    S.op("sp", lambda e, xa=xa, r0=r0: e.dma_start(out=dst_d[r0:r0 + 128, :], in_=xa),
                         reads=[xres], writes=["%s%d" % (dst_name, tt), "XS_G"], dma="st_" + xres)

            first = True
            for li, l in enumerate(layers):
                layer_prologue(l)
                if do_mixer:
                    mixer_phase(l, x_in if first else xs_d, "xin" if first else "xs", xs_d if do_moe else out_d)
                    S.barrier()
                last = (li == len(layers) - 1)
                if do_moe:
                    (moe_sparse_phase if SPARSE else moe_phase)(l, out_d if last else xs_d, "out" if last else "xs", do_final=(final and last))
                    S.barrier()
                first = False
            return W

        Wd = program(DummySched(), None)
        S = Sched(nc)
        program(S, Wd.rec)
        S.emit()
        build.last_stats = {e: len(S.stream[e]) for e in S.ENGS}
    return nc


def _pp(v):
    return np.ascontiguousarray(v.reshape(-1, 128).T)


def _bc(v):
    return np.ascontiguousarray(np.broadcast_to(v[None, :], (128, v.shape[0])))


def _consts():
    ident = np.eye(128, dtype=np.float32)
    triu = np.triu(np.ones((128, 128), dtype=np.float32))
    rc = np.zeros((128, 4, 16), dtype=np.float32)
    for gi, w in enumerate(POOL_WINDOWS):
        rc[:, gi, :] = 1.0 / np.minimum(np.arange(16) + 1, w)
    return ident, triu, rc


def _consts2():
    ustr = np.triu(np.ones((128, 128), dtype=np.float32), k=1)
    ebase = np.ascontiguousarray(np.broadcast_to((np.arange(NE, dtype=np.float32) * CAP)[None, :], (128, NE)))
    tokid = (np.arange(32, dtype=np.int32)[None, :] * 128 + np.arange(128, dtype=np.int32)[:, None]).astype(np.int32)
    return {"ustr": ustr, "ebase": ebase, "tokid": np.ascontiguousarray(tokid)}


def layer_inputs(inp, l):
    f = lambda k: np.asarray(inp[k][l], dtype=np.float32)
    ada_b = f("ada_b")
    d = {
        "ada_w%d" % l: f("ada_w"),
        "ada_bP%d" % l: np.ascontiguousarray(ada_b.reshape(48, 128).T),
        "adab_g1B%d" % l: _bc(ada_b[2048:3072]),
        "adab_g2B%d" % l: _bc(ada_b[5120:6144]),
        "n1gP%d" % l: _pp(f("norm1_g")),
        "n2gP%d" % l: _pp(f("norm2_g")),
        "w_in%d" % l: f("w_in"),
        "gnormB%d" % l: _bc(f("gmlp_norm_g")),
        "ws%d" % l: f("gmlp_ws"),
        "bsB%d" % l: _bc(f("gmlp_bs").reshape(-1)),
        "w_proj_a%d" % l: f("w_proj_a"),
        "pool_w%d" % l: f("pool_w"),
        "pscP%d" % l: _pp(f("pool_scale")),
        "convP%d" % l: np.ascontiguousarray(f("conv_w").reshape(3, 8, 128).transpose(2, 0, 1)),
        "w_proj_c%d" % l: f("w_proj_c"),
        "w_out%d" % l: f("w_out"),
        "router_w%d" % l: f("router_w"),
        "rbB%d" % l: _bc(f("router_b")),
        "w_gu%d" % l: f("exp_w_gu"),
        "bguP%d" % l: np.ascontiguousarray(f("exp_b_gu").reshape(NE, 16, 128).transpose(2, 0, 1)),
        "w_down%d" % l: f("exp_w_down"),
        "b_down%d" % l: f("exp_b_down"),
    }
    return d


_NC_CACHE = {}


def _get_nc(key, **kw):
    if key not in _NC_CACHE:
        _NC_CACHE[key] = build(**kw)
    return _NC_CACHE[key]


FUSED = True


def kernel(**inputs):
    x = np.asarray(inputs["x"], dtype=np.float32)
    c = np.asarray(inputs["c"], dtype=np.float32)
    ident, triu, rc = _consts()
    n = x.shape[0]
    common = {"ident": ident, "triu": triu, "rc": rc, "finalgB": _bc(np.asarray(inputs["final_g"], dtype=np.float32))}
    common.update(_consts2())
    if FUSED:
        plan = [((0, 1), True)]
    else:
        plan = [((0,), False), ((1,), True)]
    cur = [np.ascontiguousarray(x[b]) for b in range(n)]
    for layers, fin in plan:
        nc = _get_nc((layers, fin), layers=layers, final=fin)
        shared = dict(common)
        for l in layers:
            shared.update(layer_inputs(inputs, l))
        in_maps = []
        for b in range(n):
            m = dict(shared)
            m["x"] = cur[b]
            m["cT"] = _pp(c[b])
            in_maps.append(m)
        res = run_bass_kernel_spmd(nc, in_maps, core_ids=list(range(n)))
        cur = [np.asarray(res.results[b]["out"], dtype=np.float32) for b in range(n)]
    return np.stack(cur, axis=0)
```

```python
from contextlib import ExitStack
import numpy as np
import concourse.bass as bass
import concourse.mybir as mybir
from concourse.bass_utils import run_bass_kernel_spmd

F32 = mybir.dt.float32
BF16 = mybir.dt.bfloat16
U8 = mybir.dt.uint8
I32 = mybir.dt.int32
AF = mybir.ActivationFunctionType
ALU = mybir.AluOpType

D = 1024
SEQ = 4096
NE = 32
EPS = 1e-5
POOL_WINDOWS = (2, 4, 8, 16)
ARENA_BYTES = 210944
CAP = 1536
SPARSE = True


class Sched:
    ENGS = ("pe", "act", "dve", "pool", "sp")

    def __init__(self, nc):
        self.nc = nc
        self.ops = []
        self.lastw = {}
        self.readers = {}
        self.stream = {e: [] for e in self.ENGS}
        self.dma_keys = {}

    def op(self, eng, fn, reads=(), writes=(), dma=None, extra=()):
        i = len(self.ops)
        deps = set(extra)
        for r in reads:
            w = self.lastw.get(r)
            if w is not None:
                deps.add(w)
        for w_ in writes:
            w = self.lastw.get(w_)
            if w is not None:
                deps.add(w)
            for rd in self.readers.get(w_, ()):
                deps.add(rd)
        deps.discard(i)
        for r in reads:
            self.readers.setdefault(r, []).append(i)
        for w_ in writes:
            self.lastw[w_] = i
            self.readers[w_] = []
        o = dict(i=i, eng=eng, fn=fn, dma=dma, deps=deps, pos=len(self.stream[eng]),
                 inc=False, waits=[])
        self.ops.append(o)
        self.stream[eng].append(i)
        if dma is not None:
            self.dma_keys.setdefault(dma, []).append(i)
            o["dcount"] = 16 * len(self.dma_keys[dma])
        return i

    def barrier(self):
        lasts = []
        for e in self.ENGS:
            for i in reversed(self.stream[e]):
                if self.ops[i]["dma"] is None and self.ops[i]["fn"] is not None:
                    lasts.append(i)
                    break
        for k, lst in self.dma_keys.items():
            lasts.append(lst[-1])
        for e in self.ENGS:
            self.op(e, None, extra=lasts)
        self.lastw = {}
        self.readers = {}

    def finalize(self):
        ops = self.ops
        need = {}
        for o in ops:
            lst = []
            for p in o["deps"]:
                P = ops[p]
                if P["fn"] is None:
                    continue
                if P["dma"] is not None:
                    lst.append(p)
                elif P["eng"] == o["eng"] and o["dma"] is None:
                    if o["eng"] == "pe":
                        continue
                    if o["pos"] - P["pos"] <= 3:
                        lst.append(p)
                        P["inc"] = True
                else:
                    lst.append(p)
                    P["inc"] = True
            need[o["i"]] = lst
        for e in self.ENGS:
            c = 0
            for i in self.stream[e]:
                o = ops[i]
                if o["dma"] is None and o["inc"]:
                    c += 1
                    o["count"] = c
        waited = {e: {} for e in self.ENGS}
        for e in self.ENGS:
            for i in self.stream[e]:
                o = ops[i]
                ws = {}
                for p in need[i]:
                    P = ops[p]
                    if P["dma"] is not None:
                        s, v = "D_" + P["dma"], P["dcount"]
                    else:
                        s, v = "E_" + P["eng"], P["count"]
                    if v > ws.get(s, 0):
                        ws[s] = v
                for s, v in ws.items():
                    if waited[e].get(s, 0) >= v:
                        continue
                    waited[e][s] = v
                    o["waits"].append((s, v))

    def emit(self):
        self.finalize()
        nc = self.nc
        names = ["E_" + e for e in self.ENGS] + ["D_" + k for k in self.dma_keys]
        with ExitStack() as es:
            sems = {n: es.enter_context(nc.semaphore(n)) for n in names}
            block = es.enter_context(nc.Block())
            ops = self.ops

            def make(en):
                def body(e):
                    for i in self.stream[en]:
                        o = ops[i]
                        for s, v in o["waits"]:
                            e.wait_ge(sems[s], v)
                        if o["fn"] is None:
                            continue
                        ins = o["fn"](e)
                        if o["dma"] is not None:
                            ins.then_inc(sems["D_" + o["dma"]], 16)
                        elif o["inc"]:
                            ins.then_inc(sems["E_" + en], 1)
                return body

            block.tensor(make("pe"))
            block.scalar(make("act"))
            block.vector(make("dve"))
            block.gpsimd(make("pool"))
            block.sync(make("sp"))


class DummySched:
    def op(self, *a, **k):
        return 0

    def barrier(self):
        pass


class Ring:
    def __init__(self, name, aps, names=None):
        self.name, self.aps, self.i, self.names = name, aps, 0, names

    def next(self):
        k = self.i % len(self.aps)
        self.i += 1
        return self.aps[k], (self.names[k] if self.names else "%s%d" % (self.name, k))


class WStream:
    def __init__(self, S, slots, plan=None):
        self.S, self.slots, self.n = S, slots, len(slots)
        self.plan = plan
        self.rec = []
        self.issued = 0
        self.cons = 0
        self.released = set()

    def _pump(self):
        if self.plan is None:
            return
        while self.issued < len(self.plan) and self.issued < self.cons + self.n:
            k = self.issued
            if k >= self.n and (k - self.n) not in self.released:
                break
            slot = k % self.n
            dst = self.slots[slot]
            s_ = self.plan[k]
            self.S.op("pool", lambda e, dst=dst, s_=s_: e.dma_start(out=dst, in_=s_),
                      writes=["W%d" % slot], dma="W%d" % slot)
            self.issued += 1

    def get(self, src):
        i = self.cons
        self.cons += 1
        if self.plan is None:
            self.rec.append(src)
        else:
            self._pump()
            assert self.issued > i, "weight piece not issued (missing release?)"
        return self.slots[i % self.n], "W%d" % (i % self.n), i

    def release(self, *hs):
        for h in hs:
            self.released.add(h)
        self._pump()


class Arena:
    def __init__(self, ar):
        self.ar, self.cur = ar, 0

    def alloc(self, shape, dt, parts=128):
        esz = 4 if dt in (F32, I32) else 2
        n = 1
        for s in shape[1:]:
            n *= s
        nb = n * esz
        nb_al = (nb + 63) // 64 * 64
        off = self.cur
        self.cur += nb_al
        assert self.cur <= ARENA_BYTES, ("SBUF arena overflow", self.cur)
        v = self.ar[0:parts, off:off + nb].bitcast(dt)
        if len(shape) == 3:
            v = v.rearrange("p (a b) -> p a b", a=shape[1])
        elif len(shape) == 4:
            v = v.rearrange("p (a b c) -> p a b c", a=shape[1], b=shape[2])
        return v


def build(layers=(0, 1), final=True, n_tiles=8, n_super=4, n_exp=NE, do_mixer=True, do_moe=True, debug=False):
    nc = bass.Bass("TRN2", target_bir_lowering=False)

    def din(name, shape, dt=F32):
        return nc.dram_tensor(name, list(shape), dt, kind="ExternalInput").ap()

    x_in = din("x", [SEQ, D])
    cT = din("cT", [128, 8])
    ident_d = din("ident", [128, 128])
    triu_d = din("triu", [128, 128])
    rc_d = din("rc", [128, 4, 16])
    ustr_d = din("ustr", [128, 128])
    ebase_d = din("ebase", [128, NE])
    tokid_d = din("tokid", [128, 32], I32)
    finalg_d = din("finalgB", [128, D])
    L = {}
    for l in layers:
        L[l] = dict(
            ada_w=din("ada_w%d" % l, [D, 6 * D]),
            ada_bP=din("ada_bP%d" % l, [128, 48]),
            adab_g1B=din("adab_g1B%d" % l, [128, D]),
            adab_g2B=din("adab_g2B%d" % l, [128, D]),
            n1gP=din("n1gP%d" % l, [128, 8]),
            n2gP=din("n2gP%d" % l, [128, 8]),
            w_in=din("w_in%d" % l, [D, 9 * D]),
            gnormB=din("gnormB%d" % l, [128, D]),
            ws=din("ws%d" % l, [8, 128, 128]),
            bsB=din("bsB%d" % l, [128, D]),
            w_proj_a=din("w_proj_a%d" % l, [D, D]),
            pool_w=din("pool_w%d" % l, [4, 256, 256]),
            pscP=din("pscP%d" % l, [128, 8]),
            convP=din("convP%d" % l, [128, 3, 8]),
            w_proj_c=din("w_proj_c%d" % l, [D, D]),
            w_out=din("w_out%d" % l, [D, D]),
            router_w=din("router_w%d" % l, [D, NE]),
            rbB=din("rbB%d" % l, [128, NE]),
            w_gu=din("w_gu%d" % l, [NE, D, 2 * D]),
            bguP=din("bguP%d" % l, [128, NE, 16]),
            w_down=din("w_down%d" % l, [NE, D, D]),
            b_down=din("b_down%d" % l, [NE, D]),
        )
    out_d = nc.dram_tensor("out", [SEQ, D], F32, kind="ExternalOutput").ap()
    xs_d = nc.dram_tensor("xs_scratch", [SEQ, D], F32, kind="Internal").ap()
    ys_d = nc.dram_tensor("ys_scratch", [NE * CAP, D], F32, kind="Internal").ap()
    tokslot_d = nc.dram_tensor("tokslot_scratch", [NE * CAP, 1], I32, kind="Internal").ap()

    dbg_t = {}

    def dump(S, name, ap, reads):
        if not debug:
            return
        if name not in dbg_t:
            dbg_t[name] = nc.dram_tensor("dbg_" + name, list(ap.shape), ap.dtype, kind="ExternalOutput").ap()
        d = dbg_t[name]
        S.op("sp", lambda e: e.dma_start(out=d, in_=ap), reads=reads, writes=["dbg_" + name], dma="dbg_" + name)

    def kview(w):
        return w.rearrange("(kc p) n -> p kc n", p=128)

    with ExitStack() as es:
        arena_t = es.enter_context(nc.sbuf_tensor("arena", [128, ARENA_BYTES], U8))
        banks = [es.enter_context(nc.psum_tensor("pb%d" % i, [128, 512], F32)) for i in range(6)]
        tpb = es.enter_context(nc.psum_tensor("tpb", [128, 8, 128], BF16))
        tpb2 = es.enter_context(nc.psum_tensor("tpb2", [128, 8, 128], BF16))

        def program(S, plan):
            A = Arena(arena_t)
            pool_regs = {}
            wslots = [A.alloc([128, 8, 512], BF16) for _ in range(6)]
            W = WStream(S, wslots, plan)
            identf = A.alloc([128, 128], F32)
            identb = A.alloc([128, 128], BF16)
            triu = A.alloc([128, 128], F32)
            rc = A.alloc([128, 4, 16], F32)
            cTf = A.alloc([128, 8], F32)
            condf = A.alloc([128, 8], F32)
            condb = A.alloc([128, 8], BF16)
            condB = A.alloc([128, 8, 128], BF16)
            modsP = A.alloc([128, 48], F32)
            adabP = A.alloc([128, 48], F32)
            n1g = A.alloc([128, 8], F32)
            n2g = A.alloc([128, 8], F32)
            A1 = A.alloc([128, 8], F32)
            A2 = A.alloc([128, 8], F32)
            GB = A.alloc([128, D], F32)
            abrow = A.alloc([128, D], F32)
            sscol = A.alloc([128, 64], F32)
            rscol = A.alloc([128, 64], F32)
            statc = [0]
            mark_common = A.cur

            PB = Ring("P", [b[:] for b in banks])

            def stat_col():
                k = statc[0] % 64
                statc[0] += 1
                return k

            def mm_group(out_ap, out_res, pairs, reads):
                n = len(pairs)
                for i, (lt, rh) in enumerate(pairs):
                    S.op("pe", lambda e, o=out_ap, lt=lt, rh=rh, st=(i == 0), sp_=(i == n - 1):
                         e.matmul(o, lhsT=lt, rhs=rh, start=st, stop=sp_),
                         reads=reads, writes=[out_res])

            S.op("sp", lambda e: e.dma_start(out=identf, in_=ident_d), writes=["identf"], dma="identf")
            S.op("sp", lambda e: e.dma_start(out=triu, in_=triu_d), writes=["triu"], dma="triu")
            S.op("sp", lambda e: e.dma_start(out=rc, in_=rc_d), writes=["rc"], dma="rc")
            S.op("sp", lambda e: e.dma_start(out=cTf, in_=cT), writes=["cTf"], dma="cTf")
            S.op("dve", lambda e: e.tensor_copy(out=identb, in_=identf), reads=["identf"], writes=["identb"])
            S.op("act", lambda e: e.activation(out=condf, in_=cTf, func=AF.Silu), reads=["cTf"], writes=["condf"])
            S.op("dve", lambda e: e.tensor_copy(out=condb, in_=condf), reads=["condf"], writes=["condb"])
            for kc in range(8):
                S.op("dve", lambda e, kc=kc: e.tensor_copy(out=condB[:, kc, :], in_=condf[:, kc:kc + 1].to_broadcast([128, 128])),
                     reads=["condf"], writes=["condB"])

            def rms_rstd(src_ap, src_res, junk_ap, junk_res):
                k = stat_col()
                ssa, rsa = sscol[:, k:k + 1], rscol[:, k:k + 1]
                sres, rres = "ss%d" % k, "rs%d" % k
                S.op("dve", lambda e: e.memset(ssa, 0.0), writes=[sres])
                S.op("act", lambda e: e.activation(out=junk_ap, in_=src_ap, func=AF.Square, accum_out=ssa),
                     reads=[src_res], writes=[junk_res, sres])
                S.op("dve", lambda e: e.tensor_scalar(out=rsa, in0=ssa, scalar1=1.0 / D, scalar2=EPS, op0=ALU.mult, op1=ALU.add),
                     reads=[sres], writes=[rres])
                S.op("act", lambda e: e.activation(out=rsa, in_=rsa, func=AF.Sqrt), reads=[rres], writes=[rres])
                S.op("dve", lambda e: e.reciprocal(out=rsa, in_=rsa), reads=[rres], writes=[rres])
                return rsa, rres

            def mods_rows(l, qs):
                src_b = L[l]["adab_g1B"] if qs[0] == 4 else L[l]["adab_g2B"]
                S.op("sp", lambda e: e.dma_start(out=abrow, in_=src_b), writes=["abrow"], dma="abrow")
                for hi, q in enumerate(qs):
                    wp, wres, wh = W.get(kview(L[l]["ada_w"])[:, :, q * 512:(q + 1) * 512])
                    pb, pres = PB.next()
                    mm_group(pb, pres, [(condB[:, kc, :], wp[:, kc, :]) for kc in range(8)], [wres, "condB"])
                    W.release(wh)
                    S.op("dve", lambda e, pb=pb, hi=hi: e.tensor_tensor(out=GB[:, hi * 512:(hi + 1) * 512], in0=pb,
                                                                          in1=abrow[:, hi * 512:(hi + 1) * 512], op=ALU.add),
                         reads=[pres, "abrow"], writes=["GB"])

            def layer_prologue(l):
                P = L[l]
                S.op("sp", lambda e: e.dma_start(out=adabP, in_=P["ada_bP"]), writes=["adabP"], dma="adabP")
                S.op("sp", lambda e: e.dma_start(out=n1g, in_=P["n1gP"]), writes=["n1g"], dma="n1g")
                S.op("sp", lambda e: e.dma_start(out=n2g, in_=P["n2gP"]), writes=["n2g"], dma="n2g")
                mp, mres = PB.next()
                for q in range(12):
                    wp, wres, wh = W.get(kview(P["ada_w"])[:, :, q * 512:(q + 1) * 512])
                    for jj in range(4):
                        j = 4 * q + jj
                        mm_group(mp[:, j:j + 1], mres,
                                 [(wp[:, kc, jj * 128:(jj + 1) * 128], condb[:, kc:kc + 1]) for kc in range(8)],
                                 [wres, "condb"])
                    W.release(wh)
                S.op("dve", lambda e: e.tensor_tensor(out=modsP, in0=mp[:, 0:48], in1=adabP, op=ALU.add),
                     reads=[mres, "adabP"], writes=["modsP"])
                S.op("dve", lambda e: e.scalar_tensor_tensor(out=A1, in0=modsP[:, 8:16], scalar=1.0, in1=n1g, op0=ALU.add, op1=ALU.mult),
                     reads=["modsP", "n1g"], writes=["A1"])
                S.op("dve", lambda e: e.scalar_tensor_tensor(out=A2, in0=modsP[:, 32:40], scalar=1.0, in1=n2g, op0=ALU.add, op1=ALU.mult),
                     reads=["modsP", "n2g"], writes=["A2"])

            def norm_transpose(xa, xres, xn_ring, junk, Ascale, Ares, shoff, hT, hres, col0):
                rsa, rres = rms_rstd(xa, xres, junk, "junk")
                xn, xnres = xn_ring.next()
                S.op("act", lambda e: e.activation(out=xn, in_=xa, func=AF.Identity, scale=rsa),
                     reads=[xres, rres], writes=[xnres])
                for kc in range(8):
                    S.op("pe", lambda e, kc=kc: e.transpose(out=tpb[:, kc, :], in_=xn[:, kc * 128:(kc + 1) * 128], identity=identb),
                         reads=[xnres, "identb"], writes=["tpb"])
                for kc in range(8):
                    dst = hT[:, kc, col0:col0 + 128]
                    if kc % 2 == 0:
                        S.op("dve", lambda e, kc=kc, dst=dst: e.tensor_scalar(out=dst, in0=tpb[:, kc, :], scalar1=Ascale[:, kc:kc + 1],
                                                                               scalar2=modsP[:, shoff + kc:shoff + kc + 1], op0=ALU.mult, op1=ALU.add),
                             reads=["tpb", Ares, "modsP"], writes=[hres])
                    else:
                        S.op("act", lambda e, kc=kc, dst=dst: e.activation(out=dst, in_=tpb[:, kc, :], func=AF.Identity,
                                                                            bias=modsP[:, shoff + kc:shoff + kc + 1], scale=Ascale[:, kc:kc + 1]),
                             reads=["tpb", Ares, "modsP"], writes=[hres])

            def mixer_phase(l, src_d, src_name, mdst_d):
                P = L[l]
                A.cur = mark_common
                xsub = Ring("xsub", [A.alloc([128, D], F32) for _ in range(5)])
                xnr = Ring("xn", [A.alloc([128, D], BF16) for _ in range(2)])
                junk = A.alloc([128, D], BF16)
                hT = A.alloc([128, 8, 512], BF16)
                gvr = Ring("gv", [A.alloc([128, D], F32) for _ in range(2)])
                vn = A.alloc([128, 4, D], BF16)
                gu = A.alloc([128, 8, 512], BF16)
                a8 = Ring("a8", [A.alloc([128, 8, 512], BF16) for _ in range(2)])
                mix = A.alloc([128, 8, 512], F32)
                T2 = Ring("T2", [A.alloc([128, 528], F32) for _ in range(9)])
                sgr = Ring("sg", [A.alloc([128, 512], BF16) for _ in range(3)])
                phalo = A.alloc([128, 8, 16], F32)
                zhalo = A.alloc([128, 8, 2], F32)
                poolW = A.alloc([128, 8, 256], BF16)
                WsT = A.alloc([128, 8, 128], BF16)
                wsb = A.alloc([128, 8, 128], BF16)
                gnormB = A.alloc([128, D], F32)
                bsb = A.alloc([128, 8, 128], F32)
                pscP = A.alloc([128, 8], F32)
                convP = A.alloc([128, 3, 8], F32)
                w_in_v = kview(P["w_in"])

                mods_rows(l, (4, 5))
                wsraw, wsres = gvr.next()
                wsraw3 = wsraw.rearrange("p (h s) -> p h s", h=8)
                S.op("sp", lambda e: e.dma_start(out=wsraw3, in_=P["ws"].rearrange("h t s -> t h s")), writes=[wsres], dma="wsraw")
                S.op("sp", lambda e: e.dma_start(out=gnormB, in_=P["gnormB"]), writes=["gnormB"], dma="gnormB")
                S.op("sp", lambda e: e.dma_start(out=bsb.rearrange("p h t -> p (h t)"), in_=P["bsB"]), writes=["bsb"], dma="bsb")
                S.op("sp", lambda e: e.dma_start(out=pscP, in_=P["pscP"]), writes=["pscP"], dma="pscP")
                S.op("sp", lambda e: e.dma_start(out=convP, in_=P["convP"]), writes=["convP"], dma="convP")
                S.op("pool", lambda e: e.dma_start(out=poolW, in_=P["pool_w"].rearrange("g (k p) n -> p (g k) n", p=128)),
                     writes=["poolW"], dma="poolW")
                S.op("dve", lambda e: e.tensor_copy(out=wsb, in_=wsraw3), reads=[wsres], writes=["wsb"])
                for h in range(8):
                    S.op("pe", lambda e, h=h: e.transpose(out=tpb[:, h, :], in_=wsb[:, h, :], identity=identb),
                         reads=["wsb", "identb"], writes=["tpb"])
                S.op("dve", lambda e: e.tensor_tensor(out=WsT, in0=tpb[:], in1=triu[:, None, :].to_broadcast([128, 8, 128]), op=ALU.mult),
                     reads=["tpb", "triu"], writes=["WsT"])
                S.op("dve", lambda e: e.memset(phalo, 0.0), writes=["phalo"])
                S.op("dve", lambda e: e.memset(zhalo, 0.0), writes=["zhalo"])

                def zchunk(wp, wres, jj):
                    pb, pres = PB.next()
                    mm_group(pb, pres, [(wp[:, kc, jj * 128:(jj + 1) * 128], hT[:, kc, :]) for kc in range(8)], [wres, "hT"])
                    return pb, pres

                for tt in range(n_tiles):
                    t0 = tt * 512
                    xs_list = []
                    for s in range(4):
                        xa, xres = xsub.next()
                        r0 = t0 + s * 128
                        dres = "%s%d" % (src_name, r0 // 128)
                        S.op("sp", lambda e, xa=xa, r0=r0: e.dma_start(out=xa, in_=src_d[r0:r0 + 128, :]),
                             reads=[dres], writes=[xres], dma=xres)
                        xs_list.append((xa, xres))
                        norm_transpose(xa, xres, xnr, junk, A1, "A1", 0, hT, "hT", s * 128)
                    if tt == 0:
                        dump(S, "modsP", modsP, ["modsP"]); dump(S, "GB", GB, ["GB"]); dump(S, "hT", hT, ["hT"])
                        dump(S, "WsT", WsT, ["WsT"]); dump(S, "A1", A1, ["A1"])
                    wv = [W.get(w_in_v[:, :, (2 + hf) * 512:(3 + hf) * 512]) for hf in range(2)]
                    for s in range(4):
                        gv, gvres = gvr.next()
                        for hf in range(2):
                            pb, pres = PB.next()
                            mm_group(pb, pres, [(hT[:, kc, s * 128:(s + 1) * 128], wv[hf][0][:, kc, :]) for kc in range(8)],
                                     [wv[hf][1], "hT"])
                            S.op("act", lambda e, pb=pb, gv=gv, hf=hf: e.activation(out=gv[:, hf * 512:(hf + 1) * 512], in_=pb, func=AF.Gelu_apprx_tanh),
                                 reads=[pres], writes=[gvres])
                        rsa, rres = rms_rstd(gv, gvres, junk, "junk")
                        S.op("dve", lambda e, gv=gv, rsa=rsa, s=s: e.scalar_tensor_tensor(out=vn[:, s, :], in0=gv, scalar=rsa, in1=gnormB,
                                                                                            op0=ALU.mult, op1=ALU.mult),
                             reads=[gvres, rres, "gnormB"], writes=["vn%d" % s])
                    W.release(wv[0][2], wv[1][2])
                    for hf in range(2):
                        wp, wres, wh = W.get(w_in_v[:, :, hf * 512:(hf + 1) * 512])
                        for jj in range(4):
                            j = hf * 4 + jj
                            pb, pres = zchunk(wp, wres, jj)
                            S.op("act", lambda e, pb=pb, j=j: e.activation(out=gu[:, j, :], in_=pb, func=AF.Gelu_apprx_tanh),
                                 reads=[pres], writes=["gu"])
                        W.release(wh)
                    if tt == 0:
                        dump(S, "vn", vn, ["vn0", "vn1", "vn2", "vn3"]); dump(S, "gu", gu, ["gu"])
                    ain, ares = a8.next()
                    for s in range(4):
                        for hg in range(2):
                            pb, pres = PB.next()
                            for hh in range(4):
                                h = hg * 4 + hh
                                S.op("pe", lambda e, pb=pb, hh=hh, h=h, s=s: e.matmul(pb[:, hh * 128:(hh + 1) * 128], lhsT=vn[:, s, h * 128:(h + 1) * 128],
                                                                                       rhs=WsT[:, h, :], start=True, stop=True),
                                     reads=["vn%d" % s, "WsT"], writes=[pres])
                            t1, t1res = T2.next()
                            S.op("dve", lambda e, pb=pb, t1=t1, hg=hg: e.tensor_tensor(out=t1[:, 0:512], in0=pb,
                                                                                        in1=bsb[:, hg * 4:(hg + 1) * 4, :].rearrange("p h t -> p (h t)"), op=ALU.add),
                                 reads=[pres, "bsb"], writes=[t1res])
                            S.op("dve", lambda e, t1=t1, hg=hg, s=s, ain=ain: e.tensor_tensor(
                                out=ain[:, hg * 4:(hg + 1) * 4, s * 128:(s + 1) * 128],
                                in0=t1[:, 0:512].rearrange("p (h t) -> p h t", h=4),
                                in1=gu[:, hg * 4:(hg + 1) * 4, s * 128:(s + 1) * 128], op=ALU.mult),
                                 reads=[t1res, "gu"], writes=[ares])
                    for n in range(8):
                        if n % 4 == 0:
                            if n:
                                W.release(wa1[2], wg1[2])
                            wa1 = W.get(kview(P["w_proj_a"])[:, :, (n // 4) * 512:(n // 4 + 1) * 512])
                            wg1 = W.get(w_in_v[:, :, (12 + n // 4) * 512:(13 + n // 4) * 512])
                            wa = {n // 4: wa1}
                            wg = {n // 4: wg1}
                        gp, gres = zchunk(wg[n // 4][0], wg[n // 4][1], n % 4)
                        sg, sgres = sgr.next()
                        S.op("act", lambda e, gp=gp, sg=sg: e.activation(out=sg, in_=gp, func=AF.Sigmoid), reads=[gres], writes=[sgres])
                        pb, pres = PB.next()
                        mm_group(pb, pres, [(wa[n // 4][0][:, kc, (n % 4) * 128:(n % 4 + 1) * 128], ain[:, kc, :]) for kc in range(8)],
                                 [wa[n // 4][1], ares])
                        S.op("dve", lambda e, pb=pb, sg=sg, n=n: e.tensor_tensor(out=mix[:, n, :], in0=pb, in1=sg, op=ALU.mult),
                             reads=[pres, sgres], writes=["mix%d" % n])
                    W.release(wa1[2], wg1[2])
                    if tt == 0:
                        dump(S, "ain", ain, [ares]); dump(S, "mixA", mix, ["mix%d" % n for n in range(8)])
                    dT, dres = a8.next()
                    for hf in range(2):
                        wp, wres, wph = W.get(w_in_v[:, :, (4 + hf) * 512:(5 + hf) * 512])
                        for jj in range(4):
                            j = hf * 4 + jj
                            wdw = POOL_WINDOWS[j // 2]
                            pp, ppres = zchunk(wp, wres, jj)
                            pbuf, pbres = T2.next()
                            S.op("dve", lambda e, pbuf=pbuf, j=j: e.tensor_copy(out=pbuf[:, 0:16], in_=phalo[:, j, :]), reads=["phalo"], writes=[pbres])
                            S.op("act", lambda e, pbuf=pbuf, pp=pp: e.activation(out=pbuf[:, 16:528], in_=pp, func=AF.Copy), reads=[ppres], writes=[pbres])
                            S.op("dve", lambda e, pbuf=pbuf, j=j: e.tensor_copy(out=phalo[:, j, :], in_=pbuf[:, 512:528]), reads=[pbres], writes=["phalo"])
                            cur, cres = pbuf, pbres
                            sh, lo = 1, 0
                            while sh < wdw:
                                nx, nres = T2.next()
                                S.op("pool", lambda e, nx=nx, cur=cur, sh=sh, lo=lo: e.tensor_tensor(out=nx[:, lo + sh:528], in0=cur[:, lo + sh:528],
                                                                                                      in1=cur[:, lo:528 - sh], op=ALU.add),
                                     reads=[cres], writes=[nres])
                                cur, cres = nx, nres
                                lo += sh
                                sh *= 2
                            S.op("dve", lambda e, cur=cur, pbuf=pbuf, j=j, wdw=wdw, dT=dT: e.scalar_tensor_tensor(
                                out=dT[:, j, :], in0=cur[:, 16:528], scalar=1.0 / wdw, in1=pbuf[:, 16:528], op0=ALU.mult, op1=ALU.subtract),
                                 reads=[cres, pbres], writes=[dres])
                            if tt == 0:
                                fx, fres = T2.next()
                                S.op("dve", lambda e, fx=fx, cur=cur, j=j: e.tensor_tensor(out=fx[:, 0:16], in0=cur[:, 16:32], in1=rc[:, j // 2, :], op=ALU.mult),
                                     reads=[cres, "rc"], writes=[fres])
                                S.op("dve", lambda e, fx=fx, pbuf=pbuf, j=j, dT=dT: e.tensor_tensor(out=dT[:, j, 0:16], in0=fx[:, 0:16], in1=pbuf[:, 16:32], op=ALU.subtract),
                                     reads=[fres, pbres, dres], writes=[dres])
                        W.release(wph)
                    for n in range(8):
                        g = n // 2
                        if n % 4 == 0:
                            if n:
                                W.release(wgb1[2])
                            wgb1 = W.get(w_in_v[:, :, (14 + n // 4) * 512:(15 + n // 4) * 512])
                            wgb = {n // 4: wgb1}
                        gp, gres = zchunk(wgb[n // 4][0], wgb[n // 4][1], n % 4)
                        sg, sgres = sgr.next()
                        S.op("act", lambda e, gp=gp, sg=sg: e.activation(out=sg, in_=gp, func=AF.Sigmoid), reads=[gres], writes=[sgres])
                        pb, pres = PB.next()
                        mm_group(pb, pres, [(poolW[:, 2 * g + k2, (n % 2) * 128:(n % 2 + 1) * 128], dT[:, 2 * g + k2, :]) for k2 in range(2)],
                                 ["poolW", dres])
                        tm, tres = T2.next()
                        S.op("dve", lambda e, pb=pb, sg=sg, n=n, tm=tm: e.scalar_tensor_tensor(out=tm[:, 0:512], in0=pb, scalar=pscP[:, n:n + 1], in1=sg,
                                                                                                op0=ALU.mult, op1=ALU.mult),
                             reads=[pres, sgres, "pscP"], writes=[tres])
                        S.op("pool", lambda e, tm=tm, n=n: e.tensor_tensor(out=mix[:, n, :], in0=mix[:, n, :], in1=tm[:, 0:512], op=ALU.add),
                             reads=[tres, "mix%d" % n], writes=["mix%d" % n])
                    W.release(wgb1[2])
                    if tt == 0:
                        dump(S, "dT", dT, [dres]); dump(S, "mixB", mix, ["mix%d" % n for n in range(8)])
                    bc, bcres = a8.next()
                    for hf in range(2):
                        wx, wxres, wxh = W.get(w_in_v[:, :, (6 + hf) * 512:(7 + hf) * 512])
                        wb, wbres, wbh = W.get(w_in_v[:, :, (8 + hf) * 512:(9 + hf) * 512])
                        wc, wcres, wch = W.get(w_in_v[:, :, (10 + hf) * 512:(11 + hf) * 512])
                        for jj in range(4):
                            j = hf * 4 + jj
                            cp, cpres = zchunk(wc, wcres, jj)
                            cs, csres = T2.next()
                            S.op("act", lambda e, cs=cs, cp=cp: e.activation(out=cs[:, 0:512], in_=cp, func=AF.Copy), reads=[cpres], writes=[csres])
                            xp, xpres = zchunk(wx, wxres, jj)
                            zc, zcres = T2.next()
                            S.op("dve", lambda e, zc=zc, j=j: e.tensor_copy(out=zc[:, 0:2], in_=zhalo[:, j, :]), reads=["zhalo"], writes=[zcres])
                            S.op("dve", lambda e, zc=zc, xp=xp, cs=cs: e.tensor_tensor(out=zc[:, 2:514], in0=xp, in1=cs[:, 0:512], op=ALU.mult),
                                 reads=[xpres, csres], writes=[zcres])
                            S.op("dve", lambda e, zc=zc, j=j: e.tensor_copy(out=zhalo[:, j, :], in_=zc[:, 512:514]), reads=[zcres], writes=["zhalo"])
                            ca, cares = T2.next()
                            S.op("dve", lambda e, ca=ca, zc=zc, j=j: e.tensor_scalar(out=ca[:, 0:512], in0=zc[:, 2:514], scalar1=convP[:, 2, j:j + 1], scalar2=None, op0=ALU.mult),
                                 reads=[zcres, "convP"], writes=[cares])
                            S.op("dve", lambda e, ca=ca, zc=zc, j=j: e.scalar_tensor_tensor(out=ca[:, 0:512], in0=zc[:, 1:513], scalar=convP[:, 1, j:j + 1], in1=ca[:, 0:512],
                                                                                             op0=ALU.mult, op1=ALU.add),
                                 reads=[zcres, "convP", cares], writes=[cares])
                            S.op("dve", lambda e, ca=ca, zc=zc, j=j: e.scalar_tensor_tensor(out=ca[:, 0:512], in0=zc[:, 0:512], scalar=convP[:, 0, j:j + 1], in1=ca[:, 0:512],
                                                                                             op0=ALU.mult, op1=ALU.add),
                                 reads=[zcres, "convP", cares], writes=[cares])
                            bp, bpres = zchunk(wb, wbres, jj)
                            S.op("dve", lambda e, bp=bp, ca=ca, j=j, bc=bc: e.tensor_tensor(out=bc[:, j, :], in0=bp, in1=ca[:, 0:512], op=ALU.mult),
                                 reads=[bpres, cares], writes=[bcres])
                        W.release(wxh, wbh, wch)
                    mixb, mbres = a8.next()
                    for n in range(8):
                        if n % 4 == 0:
                            if n:
                                W.release(wcc1[2], wgc1[2])
                            wcc1 = W.get(kview(P["w_proj_c"])[:, :, (n // 4) * 512:(n // 4 + 1) * 512])
                            wgc1 = W.get(w_in_v[:, :, (16 + n // 4) * 512:(17 + n // 4) * 512])
                            wcc = {n // 4: wcc1}
                            wgc = {n // 4: wgc1}
                        gp, gres = zchunk(wgc[n // 4][0], wgc[n // 4][1], n % 4)
                        sg, sgres = sgr.next()
                        S.op("act", lambda e, gp=gp, sg=sg: e.activation(out=sg, in_=gp, func=AF.Sigmoid), reads=[gres], writes=[sgres])
                        pb, pres = PB.next()
                        mm_group(pb, pres, [(wcc[n // 4][0][:, kc, (n % 4) * 128:(n % 4 + 1) * 128], bc[:, kc, :]) for kc in range(8)],
                                 [wcc[n // 4][1], bcres])
                        tm, tres = T2.next()
                        S.op("dve", lambda e, pb=pb, sg=sg, tm=tm: e.tensor_tensor(out=tm[:, 0:512], in0=pb, in1=sg, op=ALU.mult),
                             reads=[pres, sgres], writes=[tres])
                        S.op("pool", lambda e, tm=tm, n=n, mixb=mixb: e.tensor_tensor(out=mixb[:, n, :], in0=mix[:, n, :], in1=tm[:, 0:512], op=ALU.add),
                             reads=[tres, "mix%d" % n], writes=[mbres])
                    W.release(wcc1[2], wgc1[2])
                    if tt == 0:
                        dump(S, "bc", bc, [bcres]); dump(S, "mixb", mixb, [mbres])
                    wo = [W.get(kview(P["w_out"])[:, :, hf * 512:(hf + 1) * 512]) for hf in range(2)]
                    for s in range(4):
                        xa, xres = xs_list[s]
                        for hf in range(2):
                            pb, pres = PB.next()
                            mm_group(pb, pres, [(mixb[:, kc, s * 128:(s + 1) * 128], wo[hf][0][:, kc, :]) for kc in range(8)],
                                     [wo[hf][1], mbres])
                            tm, tres = T2.next()
                            S.op("dve", lambda e, pb=pb, tm=tm, hf=hf: e.tensor_tensor(out=tm[:, 0:512], in0=pb, in1=GB[:, hf * 512:(hf + 1) * 512], op=ALU.mult),
                                 reads=[pres, "GB"], writes=[tres])
                            S.op("dve", lambda e, tm=tm, xa=xa, hf=hf: e.tensor_tensor(out=xa[:, hf * 512:(hf + 1) * 512], in0=xa[:, hf * 512:(hf + 1) * 512],
                                                                                        in1=tm[:, 0:512], op=ALU.add),
                                 reads=[tres, xres], writes=[xres])
                        r0 = t0 + s * 128
                        S.op("sp", lambda e, xa=xa, r0=r0: e.dma_start(out=mdst_d[r0:r0 + 128, :], in_=xa),
                             reads=[xres], writes=["xs%d" % (r0 // 128)], dma="st_" + xres)
                    W.release(wo[0][2], wo[1][2])

            def moe_phase(l, dst_d, dst_name, do_final):
                P = L[l]
                A.cur = mark_common
                xsub = Ring("xsub", [A.alloc([128, D], F32) for _ in range(3)])
                xnr = Ring("xn", [A.alloc([128, D], BF16) for _ in range(2)])
                junk = A.alloc([128, D], BF16)
                h2T = A.alloc([128, 8, 1024], BF16)
                actT = A.alloc([128, 8, 1024], BF16)
                acc = A.alloc([128, 8, D], F32)
                T2 = Ring("T2", [A.alloc([128, 512], F32) for _ in range(10)])
                Gtok = A.alloc([128, 8, NE], F32)
                GT = A.alloc([128, 8, 128], F32)
                lgr = Ring("lg", [A.alloc([128, NE], F32) for _ in range(2)])
                exr = Ring("ex", [A.alloc([128, NE], F32) for _ in range(2)])
                mkr = Ring("mk", [A.alloc([128, NE], F32) for _ in range(2)])
                m8r = Ring("m8", [A.alloc([128, 8], F32) for _ in range(2)])
                smr = Ring("sm", [A.alloc([128, 4], F32) for _ in range(2)])
                routerW = A.alloc([128, 8, NE], BF16)
                rbB = A.alloc([128, NE], F32)
                bguP = A.alloc([128, NE, 16], F32)
                bdown = A.alloc([128, D], F32)
                finalg = A.alloc([128, D], F32) if do_final else None

                mods_rows(l, (10, 11))
                S.op("pool", lambda e: e.dma_start(out=routerW, in_=kview(P["router_w"])), writes=["routerW"], dma="routerW")
                S.op("sp", lambda e: e.dma_start(out=rbB, in_=P["rbB"]), writes=["rbB"], dma="rbB")
                S.op("sp", lambda e: e.dma_start(out=bguP, in_=P["bguP"]), writes=["bguP"], dma="bguP")
                S.op("sp", lambda e: e.dma_start(out=bdown[0:NE, :], in_=P["b_down"]), writes=["bdown"], dma="bdown")
                if do_final:
                    S.op("sp", lambda e: e.dma_start(out=finalg, in_=finalg_d), writes=["finalg"], dma="finalg")

                for st in range(n_super):
                    t0 = st * 1024
                    for s in range(8):
                        xa, xres = xsub.next()
                        r0 = t0 + s * 128
                        S.op("sp", lambda e, xa=xa, r0=r0: e.dma_start(out=xa, in_=xs_d[r0:r0 + 128, :]),
                             reads=["xs%d" % (r0 // 128)], writes=[xres], dma=xres)
                        norm_transpose(xa, xres, xnr, junk, A2, "A2", 24, h2T, "h2T", s * 128)
                        pb, pres = PB.next()
                        mm_group(pb[:, 0:NE], pres, [(h2T[:, kc, s * 128:(s + 1) * 128], routerW[:, kc, :]) for kc in range(8)],
                                 ["h2T", "routerW"])
                        lg, lgres = lgr.next()
                        ex, exres = exr.next()
                        mk, mkres = mkr.next()
                        m8, m8res = m8r.next()
                        sm, smres = smr.next()
                        S.op("dve", lambda e, lg=lg, pb=pb: e.tensor_tensor(out=lg, in0=pb[:, 0:NE], in1=rbB, op=ALU.add),
                             reads=[pres, "rbB"], writes=[lgres])
                        S.op("dve", lambda e, lg=lg, m8=m8: e.max(out=m8, in_=lg), reads=[lgres], writes=[m8res])
                        S.op("dve", lambda e, lg=lg, m8=m8, mk=mk: e.tensor_scalar(out=mk, in0=lg, scalar1=m8[:, 3:4], scalar2=None, op0=ALU.is_ge),
                             reads=[lgres, m8res], writes=[mkres])
                        S.op("dve", lambda e, sm=sm, m8=m8: e.tensor_scalar(out=sm[:, 0:1], in0=m8[:, 0:1], scalar1=-1.0, scalar2=None, op0=ALU.mult),
                             reads=[m8res], writes=[smres])
                        S.op("act", lambda e, ex=ex, lg=lg, sm=sm: e.activation(out=ex, in_=lg, func=AF.Exp, bias=sm[:, 0:1], scale=1.0),
                             reads=[lgres, smres], writes=[exres])
                        S.op("dve", lambda e, sm=sm: e.memset(sm[:, 1:2], 0.0), reads=[smres], writes=[smres])
                        S.op("dve", lambda e, ex=ex, mk=mk, sm=sm: e.scalar_tensor_tensor(out=ex, in0=ex, scalar=1.0, in1=mk, op0=ALU.mult, op1=ALU.mult,
                                                                                           accum_out=sm[:, 1:2]),
                             reads=[exres, mkres, smres], writes=[exres, smres])
                        S.op("dve", lambda e, sm=sm: e.reciprocal(out=sm[:, 2:3], in_=sm[:, 1:2]), reads=[smres], writes=[smres])
                        S.op("dve", lambda e, ex=ex, sm=sm, s=s: e.tensor_scalar(out=Gtok[:, s, :], in0=ex, scalar1=sm[:, 2:3], scalar2=None, op0=ALU.mult),
                             reads=[exres, smres], writes=["Gtok%d" % s])
                        pt, ptres = PB.next()
                        S.op("pe", lambda e, pt=pt, s=s: e.transpose(out=pt[0:NE, 0:128], in_=Gtok[:, s, :], identity=identf),
                             reads=["Gtok%d" % s, "identf"], writes=[ptres])
                        S.op("act", lambda e, pt=pt, s=s: e.activation(out=GT[0:NE, s, :], in_=pt[0:NE, 0:128], func=AF.Copy),
                             reads=[ptres], writes=["GT%d" % s])
                        for hf in range(2):
                            pb2, p2res = PB.next()
                            S.op("pe", lambda e, pb2=pb2, s=s, hf=hf: e.matmul(pb2, lhsT=GT[0:NE, s, :], rhs=bdown[0:NE, hf * 512:(hf + 1) * 512],
                                                                                start=True, stop=True),
                                 reads=["GT%d" % s, "bdown"], writes=[p2res])
                            S.op("act", lambda e, pb2=pb2, s=s, hf=hf: e.activation(out=acc[:, s, hf * 512:(hf + 1) * 512], in_=pb2, func=AF.Copy),
                                 reads=[p2res], writes=["acc%d_%d" % (s, hf)])
                    for ex_i in range(n_exp):
                        wgv = kview(P["w_gu"][ex_i])
                        wdv = kview(P["w_down"][ex_i])
                        for hfj in range(2):
                            gw, gwres, gwh = W.get(wgv[:, :, hfj * 512:(hfj + 1) * 512])
                            uw, uwres, uwh = W.get(wgv[:, :, (2 + hfj) * 512:(3 + hfj) * 512])
                            for th in range(2):
                                for jj in range(4):
                                    j = hfj * 4 + jj
                                    gp, gpres = PB.next()
                                    mm_group(gp, gpres, [(gw[:, kc, jj * 128:(jj + 1) * 128], h2T[:, kc, th * 512:(th + 1) * 512]) for kc in range(8)],
                                             [gwres, "h2T"])
                                    up, upres = PB.next()
                                    mm_group(up, upres, [(uw[:, kc, jj * 128:(jj + 1) * 128], h2T[:, kc, th * 512:(th + 1) * 512]) for kc in range(8)],
                                             [uwres, "h2T"])
                                    gc, gcres = T2.next()
                                    sg, sgres = T2.next()
                                    uc, ucres = T2.next()
                                    rr, rrres = T2.next()
                                    S.op("dve", lambda e, gc=gc, gp=gp, j=j, ex_i=ex_i: e.tensor_scalar(out=gc, in0=gp, scalar1=bguP[:, ex_i, j:j + 1], scalar2=7.0,
                                                                                                        op0=ALU.add, op1=ALU.min),
                                         reads=[gpres, "bguP"], writes=[gcres])
                                    S.op("act", lambda e, sg=sg, gc=gc: e.activation(out=sg, in_=gc, func=AF.Silu, scale=1.702),
                                         reads=[gcres], writes=[sgres])
                                    S.op("dve", lambda e, uc=uc, up=up, j=j, ex_i=ex_i: e.tensor_scalar(out=uc, in0=up, scalar1=bguP[:, ex_i, 8 + j:9 + j], scalar2=7.0,
                                                                                                        op0=ALU.add, op1=ALU.min),
                                         reads=[upres, "bguP"], writes=[ucres])
                                    S.op("dve", lambda e, rr=rr, uc=uc: e.tensor_scalar(out=rr, in0=uc, scalar1=-7.0, scalar2=1.0, op0=ALU.max, op1=ALU.add),
                                         reads=[ucres], writes=[rrres])
                                    S.op("dve", lambda e, rr=rr, sg=sg, j=j, th=th: e.scalar_tensor_tensor(out=actT[:, j, th * 512:(th + 1) * 512], in0=rr,
                                                                                                        scalar=1.0 / 1.702, in1=sg, op0=ALU.mult, op1=ALU.mult),
                                         reads=[rrres, sgres], writes=["actT%d" % th])
                            W.release(gwh, uwh)
                        dpieces = [W.get(wdv[:, :, hf * 512:(hf + 1) * 512]) for hf in range(2)]
                        for s in range(8):
                            th = s // 4
                            for hf in range(2):
                                pb, pres = PB.next()
                                mm_group(pb, pres, [(actT[:, kc, s * 128:(s + 1) * 128], dpieces[hf][0][:, kc, :]) for kc in range(8)],
                                         [dpieces[hf][1], "actT%d" % th])
                                ar_ = "acc%d_%d" % (s, hf)
                                S.op("dve", lambda e, pb=pb, s=s, hf=hf, ex_i=ex_i: e.scalar_tensor_tensor(
                                    out=acc[:, s, hf * 512:(hf + 1) * 512], in0=pb, scalar=Gtok[:, s, ex_i:ex_i + 1],
                                    in1=acc[:, s, hf * 512:(hf + 1) * 512], op0=ALU.mult, op1=ALU.add),
                                     reads=[pres, "Gtok%d" % s, ar_], writes=[ar_])
                        W.release(dpieces[0][2], dpieces[1][2])
                    for s in range(8):
                        xa, xres = xsub.next()
                        r0 = t0 + s * 128
                        S.op("sp", lambda e, xa=xa, r0=r0: e.dma_start(out=xa, in_=xs_d[r0:r0 + 128, :]),
                             reads=["xs%d" % (r0 // 128)], writes=[xres], dma=xres)
                        S.op("dve", lambda e, s=s: e.tensor_tensor(out=acc[:, s, :], in0=acc[:, s, :], in1=GB, op=ALU.mult),
                             reads=["acc%d_0" % s, "acc%d_1" % s, "GB"], writes=["acc%d_0" % s, "acc%d_1" % s])
                        S.op("dve", lambda e, s=s, xa=xa: e.tensor_tensor(out=xa, in0=xa, in1=acc[:, s, :], op=ALU.add),
                             reads=["acc%d_0" % s, "acc%d_1" % s, xres], writes=[xres])
                        if do_final:
                            rsa, rres = rms_rstd(xa, xres, junk, "junk")
                            S.op("dve", lambda e, xa=xa, rsa=rsa: e.scalar_tensor_tensor(out=xa, in0=xa, scalar=rsa, in1=finalg, op0=ALU.mult, op1=ALU.mult),
                                 reads=[xres, rres, "finalg"], writes=[xres])
                        S.op("sp", lambda e, xa=xa, r0=r0: e.dma_start(out=dst_d[r0:r0 + 128, :], in_=xa),
                             reads=[xres], writes=["%s%d" % (dst_name, r0 // 128)], dma="st_" + xres)


            def moe_sparse_phase(l, dst_d, dst_name, do_final):
                P = L[l]
                A.cur = mark_common
                NT = SEQ // 128
                lgall = A.alloc([128, NT, NE], F32)
                m8all = A.alloc([128, NT, 8], F32)
                w4all = A.alloc([128, NT, 4], F32)
                mkb = A.alloc([128, NT, NE], BF16)
                slotf = A.alloc([128, NT, 4], F32)
                sloti = A.alloc([128, NT, 4], I32)
                tokid = A.alloc([128, NT], I32)
                ebase = A.alloc([128, NE], F32)
                ustrf = A.alloc([128, 128], F32)
                onesb = A.alloc([128, 128], BF16)
                Ub = A.alloc([128, 128], BF16)
                onesf = A.alloc([128, 128], F32)
                zeros_i = A.alloc([128, NE * CAP // 128], I32)
                smr = Ring("sm", [A.alloc([128, 4], F32) for _ in range(2)])
                routerW = A.alloc([128, 8, NE], BF16)
                rbB = A.alloc([128, NE], F32)
                bguP = A.alloc([128, NE, 16], F32)
                finalg = A.alloc([128, D], F32) if do_final else None
                junk32 = A.alloc([128, NE], F32)
                mark_persist = A.cur
                xsub = Ring("xsub", [A.alloc([128, D], F32) for _ in range(4)])
                xnr = Ring("xn", [A.alloc([128, D], BF16) for _ in range(8)])
                junk = A.alloc([128, D], BF16)
                hTr = Ring("hTr", [A.alloc([128, 8, 128], BF16) for _ in range(2)])
                h2r = Ring("h2c", [A.alloc([128, 8, 512], BF16) for _ in range(4)])
                actr = Ring("actc", [A.alloc([128, 8, 512], BF16) for _ in range(2)])
                tpr = Ring("tp", [tpb, tpb2], names=["tpb", "tpb2"])
                T2 = Ring("T2", [A.alloc([128, 512], F32) for _ in range(8)])
                tslr = Ring("tsl", [A.alloc([128, 1], I32) for _ in range(4)])
                ysr = Ring("ysr", [A.alloc([128, D], F32) for _ in range(4)])
                bdrow = Ring("bdrow", [A.alloc([128, D], BF16) for _ in range(2)])
                NSLOT = NE * CAP
                if "bc" not in pool_regs:
                    pool_regs["bc"] = None

                    def _init_bc(e):
                        r = e.alloc_register("bc")
                        pool_regs["bc"] = r
                        return e.reg_mov(r, NSLOT - 1)
                    S.op("pool", _init_bc)

                mods_rows(l, (10, 11))
                S.op("pool", lambda e: e.dma_start(out=routerW, in_=kview(P["router_w"])), writes=["routerW"], dma="routerW")
                S.op("sp", lambda e: e.dma_start(out=rbB, in_=P["rbB"]), writes=["rbB"], dma="rbB")
                S.op("sp", lambda e: e.dma_start(out=bguP, in_=P["bguP"]), writes=["bguP"], dma="bguP")
                S.op("sp", lambda e: e.dma_start(out=ebase, in_=ebase_d), writes=["ebase"], dma="ebase")
                S.op("sp", lambda e: e.dma_start(out=ustrf, in_=ustr_d), writes=["ustrf"], dma="ustrf")
                S.op("sp", lambda e: e.dma_start(out=tokid, in_=tokid_d), writes=["tokid"], dma="tokid")
                if do_final:
                    S.op("sp", lambda e: e.dma_start(out=finalg, in_=finalg_d), writes=["finalg"], dma="finalg")
                S.op("dve", lambda e: e.tensor_copy(out=Ub, in_=ustrf), reads=["ustrf"], writes=["Ub"])
                S.op("dve", lambda e: e.memset(onesb, 1.0), writes=["onesb"])
                S.op("dve", lambda e: e.memset(onesf, 1.0), writes=["onesf"])
                S.op("dve", lambda e: e.memset(zeros_i, 0), writes=["zeros_i"])
                S.op("sp", lambda e: e.dma_start(out=tokslot_d.rearrange("(p f) o -> p (f o)", p=128), in_=zeros_i),
                     reads=["zeros_i"], writes=["tokslot0"], dma="tokslot0")

                def partA1(e_, c_):
                    xs_ = []
                    for sb in range(4):
                        tsl, tslres = tslr.next()
                        s0 = e_ * CAP + c_ * 512 + sb * 128
                        S.op("sp", lambda e, tsl=tsl, s0=s0: e.dma_start(out=tsl, in_=tokslot_d[s0:s0 + 128, :]), reads=scat_res + ["tokslot0"], writes=[tslres], dma=tslres)
                        xa, xres = xsub.next()
                        S.op("pool", lambda e, xa=xa, tsl=tsl: e.indirect_dma_start(out=xa, out_offset=None, in_=xs_d[:, :],
                                                                                     in_offset=bass.IndirectOffsetOnAxis(ap=tsl[:, 0:1], axis=0)),
                             reads=[tslres, "XS_G"], writes=[xres], dma=xres)
                        xs_.append((xa, xres))
                    return xs_

                def partA2(xs_):
                    outs = []
                    for xa, xres in xs_:
                        xn, xnres = xnr.next()
                        rsa, rres = rms_rstd(xa, xres, xn, xnres)
                        S.op("act", lambda e, xn=xn, xa=xa, rsa=rsa: e.activation(out=xn, in_=xa, func=AF.Identity, scale=rsa),
                             reads=[xres, rres], writes=[xnres])
                        outs.append((xn, xnres))
                    return outs

                def partA(e_, c_):
                    return partA2(partA1(e_, c_))
                def nstage(xs_):
                    st = []
                    for xa, xres in xs_:
                        k = stat_col()
                        xn, xnres = xnr.next()
                        st.append(dict(xa=xa, xres=xres, xn=xn, xnres=xnres, ssa=sscol[:, k:k + 1], rsa=rscol[:, k:k + 1], sres="ss%d" % k, rres="rs%d" % k))

                    def sM():
                        for t in st:
                            S.op("dve", lambda e, t=t: e.memset(t["ssa"], 0.0), writes=[t["sres"]])

                    def sS1():
                        for t in st:
                            S.op("act", lambda e, t=t: e.activation(out=t["xn"], in_=t["xa"], func=AF.Square, accum_out=t["ssa"]),
                                 reads=[t["xres"]], writes=[t["xnres"], t["sres"]])

                    def sT():
                        for t in st:
                            S.op("dve", lambda e, t=t: e.tensor_scalar(out=t["rsa"], in0=t["ssa"], scalar1=1.0 / D, scalar2=EPS, op0=ALU.mult, op1=ALU.add),
                                 reads=[t["sres"]], writes=[t["rres"]])

                    def sS2():
                        for t in st:
                            S.op("act", lambda e, t=t: e.activation(out=t["rsa"], in_=t["rsa"], func=AF.Sqrt), reads=[t["rres"]], writes=[t["rres"]])

                    def sR():
                        for t in st:
                            S.op("dve", lambda e, t=t: e.reciprocal(out=t["rsa"], in_=t["rsa"]), reads=[t["rres"]], writes=[t["rres"]])

                    def sX():
                        for t in st:
                            S.op("act", lambda e, t=t: e.activation(out=t["xn"], in_=t["xa"], func=AF.Identity, scale=t["rsa"]),
                                 reads=[t["xres"], t["rres"]], writes=[t["xnres"]])
                        return [(t["xn"], t["xnres"]) for t in st]
                    return {0: sM, 1: sS1, 3: sT, 4: sS2, 6: sR, 7: sX}


                def partB(outs, hc, hres):
                    for sb, (xn, xnres) in enumerate(outs):
                        tp, tpres = tpr.next()
                        for kc in range(8):
                            S.op("pe", lambda e, kc=kc, tp=tp, xn=xn: e.transpose(out=tp[:, kc, :], in_=xn[:, kc * 128:(kc + 1) * 128], identity=identb),
                                 reads=[xnres, "identb"], writes=[tpres])
                        for kc in range(8):
                            dst = hc[:, kc, sb * 128:(sb + 1) * 128]
                            if False:
                                S.op("dve", lambda e, kc=kc, dst=dst, tp=tp: e.tensor_scalar(out=dst, in0=tp[:, kc, :], scalar1=A2[:, kc:kc + 1],
                                                                                              scalar2=modsP[:, 24 + kc:25 + kc], op0=ALU.mult, op1=ALU.add),
                                     reads=[tpres, "A2", "modsP"], writes=[hres])
                            else:
                                S.op("act", lambda e, kc=kc, dst=dst, tp=tp: e.activation(out=dst, in_=tp[:, kc, :], func=AF.Identity,
                                                                                           bias=modsP[:, 24 + kc:25 + kc], scale=A2[:, kc:kc + 1]),
                                     reads=[tpres, "A2", "modsP"], writes=[hres])

                for tt in range(NT):
                    xa, xres = xsub.next()
                    r0 = tt * 128
                    S.op("sp", lambda e, xa=xa, r0=r0: e.dma_start(out=xa, in_=xs_d[r0:r0 + 128, :]), reads=["xs%d" % tt], writes=[xres], dma=xres)
                    hs, hsres = hTr.next()
                    partB(partA2([(xa, xres)]), hs, hsres)
                    pb, pres = PB.next()
                    mm_group(pb[:, 0:NE], pres, [(hs[:, kc, :], routerW[:, kc, :]) for kc in range(8)], [hsres, "routerW"])
                    lg = lgall[:, tt, :]
                    m8 = m8all[:, tt, :]
                    sm, smres = smr.next()
                    rt = "rt%d" % tt
                    S.op("dve", lambda e, lg=lg, pb=pb: e.tensor_tensor(out=lg, in0=pb[:, 0:NE], in1=rbB, op=ALU.add), reads=[pres, "rbB"], writes=[rt])
                    S.op("dve", lambda e, lg=lg, m8=m8: e.max(out=m8, in_=lg), reads=[rt], writes=[rt])
                    S.op("dve", lambda e, lg=lg, m8=m8, tt=tt: e.tensor_scalar(out=mkb[:, tt, :], in0=lg, scalar1=m8[:, 3:4], scalar2=None, op0=ALU.is_ge),
                         reads=[rt], writes=["mkb%d" % tt])
                    S.op("dve", lambda e, sm=sm, m8=m8: e.tensor_scalar(out=sm[:, 0:1], in0=m8[:, 0:1], scalar1=-1.0, scalar2=None, op0=ALU.mult),
                         reads=[rt], writes=[smres])
                    S.op("dve", lambda e, sm=sm: e.memset(sm[:, 1:2], 0.0), reads=[smres], writes=[smres])
                    S.op("act", lambda e, m8=m8, sm=sm, tt=tt: e.activation(out=w4all[:, tt, :], in_=m8[:, 0:4], func=AF.Exp, bias=sm[:, 0:1], scale=1.0,
                                                                          accum_out=sm[:, 1:2]),
                         reads=[rt, smres], writes=["w4_%d" % tt, smres])
                    S.op("dve", lambda e, sm=sm: e.reciprocal(out=sm[:, 2:3], in_=sm[:, 1:2]), reads=[smres], writes=[smres])
                    S.op("dve", lambda e, sm=sm, tt=tt: e.tensor_scalar(out=w4all[:, tt, :], in0=w4all[:, tt, :], scalar1=sm[:, 2:3], scalar2=None, op0=ALU.mult),
                         reads=["w4_%d" % tt, smres], writes=["w4_%d" % tt])
                scat_res = []
                for tt in range(NT):
                    pr, prres = PB.next()
                    pairs = [(onesb, mkb[:, t2, :]) for t2 in range(tt)] + [(Ub, mkb[:, tt, :])]
                    mm_group(pr[:, 0:NE], prres, pairs, ["mkb%d" % t2 for t2 in range(tt + 1)] + ["onesb", "Ub"])
                    sf, sfres = T2.next()
                    ov, ovres = T2.next()
                    S.op("dve", lambda e, sf=sf, pr=pr: e.tensor_tensor(out=sf[:, 0:NE], in0=pr[:, 0:NE], in1=ebase, op=ALU.add), reads=[prres, "ebase"], writes=[sfres])
                    S.op("dve", lambda e, ov=ov, pr=pr: e.tensor_scalar(out=ov[:, 0:NE], in0=pr[:, 0:NE], scalar1=float(CAP), scalar2=1.0e7, op0=ALU.is_ge, op1=ALU.mult),
                         reads=[prres], writes=[ovres])
                    S.op("dve", lambda e, sf=sf, ov=ov: e.tensor_tensor(out=sf[:, 0:NE], in0=sf[:, 0:NE], in1=ov[:, 0:NE], op=ALU.add), reads=[sfres, ovres], writes=[sfres])
                    S.op("dve", lambda e, tt=tt: e.memset(slotf[:, tt, :], 0.0), writes=["slotf%d" % tt])
                    for k in range(4):
                        S.op("dve", lambda e, tt=tt, k=k, sf=sf: e.scalar_tensor_tensor(out=junk32, in0=lgall[:, tt, :], scalar=m8all[:, tt, k:k + 1], in1=sf[:, 0:NE],
                                                                                         op0=ALU.is_equal, op1=ALU.mult, accum_out=slotf[:, tt, k:k + 1]),
                             reads=["rt%d" % tt, sfres, "slotf%d" % tt], writes=["junk32", "slotf%d" % tt])
                    S.op("dve", lambda e, tt=tt: e.tensor_copy(out=sloti[:, tt, :], in_=slotf[:, tt, :]), reads=["slotf%d" % tt], writes=["sloti%d" % tt])
                    for k in range(4):
                        sr = "scat%d_%d" % (tt, k)
                        S.op("pool", lambda e, tt=tt, k=k: e.indirect_dma_start(out=tokslot_d[:, :], out_offset=bass.IndirectOffsetOnAxis(ap=sloti[:, tt, k:k + 1], axis=0),
                                                                               in_=tokid[:, tt:tt + 1], in_offset=None, bounds_check=pool_regs["bc"], oob_is_err=False),
                             reads=["sloti%d" % tt, "tokid", "tokslot0"], writes=[sr], dma="scat")
                        scat_res.append(sr)
                ys_res = []
                NCH = CAP // 512
                chunks = [(e_, c_) for e_ in range(n_exp) for c_ in range(NCH)]

                pend = {}
                hslot = {}
                pend[0] = partA(*chunks[0])
                hslot[0] = h2r.next()
                partB(pend.pop(0), *hslot[0])
                if len(chunks) > 1:
                    pend[1] = partA(*chunks[1])
                wp = {}
                for n, (ex_i, c_) in enumerate(chunks):
                    pa1 = partA1(*chunks[n + 2]) if n + 2 < len(chunks) else None
                    hc, hres = hslot.pop(n)
                    wgv = kview(P["w_gu"][ex_i])
                    wdv = kview(P["w_down"][ex_i])
                    if c_ == 0:
                        for hfj in range(2):
                            wp["g%d" % hfj] = W.get(wgv[:, :, hfj * 512:(hfj + 1) * 512])
                            wp["u%d" % hfj] = W.get(wgv[:, :, (2 + hfj) * 512:(3 + hfj) * 512])
                        bdr, bdres = bdrow.next()
                        S.op("pool", lambda e, bdr=bdr, ex_i=ex_i: e.dma_start(out=bdr[0:1, :], in_=P["b_down"][ex_i:ex_i + 1, :]), writes=[bdres], dma=bdres)
                    ac, acres = actr.next()
                    deferred = []
                    stages = nstage(pa1) if pa1 is not None else {}
                    for hfj in range(2):
                        gw, gwres, gwh = wp["g%d" % hfj]
                        uw, uwres, uwh = wp["u%d" % hfj]
                        for jj in range(4):
                            j = hfj * 4 + jj
                            if j in stages:
                                r_ = stages[j]()
                                if j == 7:
                                    pend[n + 2] = r_
                            gp, gpres = PB.next()
                            mm_group(gp, gpres, [(gw[:, kc, jj * 128:(jj + 1) * 128], hc[:, kc, :]) for kc in range(8)], [gwres, hres])
                            up, upres = PB.next()
                            mm_group(up, upres, [(uw[:, kc, jj * 128:(jj + 1) * 128], hc[:, kc, :]) for kc in range(8)], [uwres, hres])
                            gc, gcres = T2.next()
                            uc, ucres = T2.next()
                            S.op("dve", lambda e, gc=gc, gp=gp, j=j, ex_i=ex_i: e.tensor_scalar(out=gc, in0=gp, scalar1=bguP[:, ex_i, j:j + 1], scalar2=7.0, op0=ALU.add, op1=ALU.min),
                                 reads=[gpres, "bguP"], writes=[gcres])
                            S.op("act", lambda e, gc=gc: e.activation(out=gc, in_=gc, func=AF.Silu, scale=1.702), reads=[gcres], writes=[gcres])
                            S.op("dve", lambda e, uc=uc, up=up, j=j, ex_i=ex_i: e.tensor_scalar(out=uc, in0=up, scalar1=bguP[:, ex_i, 8 + j:9 + j], scalar2=7.0, op0=ALU.add, op1=ALU.min),
                                 reads=[upres, "bguP"], writes=[ucres])
                            S.op("dve", lambda e, uc=uc: e.tensor_scalar(out=uc, in0=uc, scalar1=-7.0, scalar2=1.0, op0=ALU.max, op1=ALU.add), reads=[ucres], writes=[ucres])
                            if deferred:
                                dj, duc, ducres, dgc, dgcres = deferred.pop()
                                S.op("dve", lambda e, duc=duc, dgc=dgc, dj=dj, ac=ac: e.scalar_tensor_tensor(out=ac[:, dj, :], in0=duc, scalar=1.0 / 1.702, in1=dgc, op0=ALU.mult, op1=ALU.mult),
                                     reads=[ducres, dgcres], writes=[acres])
                            deferred.append((j, uc, ucres, gc, gcres))
                    dj, duc, ducres, dgc, dgcres = deferred.pop()
                    S.op("dve", lambda e, duc=duc, dgc=dgc, dj=dj, ac=ac: e.scalar_tensor_tensor(out=ac[:, dj, :], in0=duc, scalar=1.0 / 1.702, in1=dgc, op0=ALU.mult, op1=ALU.mult),
                         reads=[ducres, dgcres], writes=[acres])
                    if c_ == NCH - 1:
                        W.release(wp["g0"][2], wp["u0"][2], wp["g1"][2], wp["u1"][2])
                    if n + 1 < len(chunks):
                        hslot[n + 1] = h2r.next()
                        partB(pend.pop(n + 1), *hslot[n + 1])
                    if c_ == 0:
                        wp["d"] = [W.get(wdv[:, :, hf * 512:(hf + 1) * 512]) for hf in range(2)]
                    dpieces = wp["d"]
                    for sb in range(4):
                        yt, ytres = ysr.next()
                        for hf in range(2):
                            pb, pres = PB.next()
                            pairs = [(onesb[0:1, :], bdr[0:1, hf * 512:(hf + 1) * 512])] + \
                                    [(ac[:, kc, sb * 128:(sb + 1) * 128], dpieces[hf][0][:, kc, :]) for kc in range(8)]
                            mm_group(pb, pres, pairs, [dpieces[hf][1], acres, bdres, "onesb"])
                            S.op("act", lambda e, pb=pb, yt=yt, hf=hf: e.activation(out=yt[:, hf * 512:(hf + 1) * 512], in_=pb, func=AF.Copy), reads=[pres], writes=[ytres])
                        s0 = ex_i * CAP + c_ * 512 + sb * 128
                        yr = "ys_%d_%d_%d" % (ex_i, c_, sb)
                        S.op("sp", lambda e, yt=yt, s0=s0: e.dma_start(out=ys_d[s0:s0 + 128, :], in_=yt), reads=[ytres], writes=[yr], dma="st_" + ytres)
                        ys_res.append(yr)
                    if c_ == NCH - 1:
                        W.release(dpieces[0][2], dpieces[1][2])
                S.barrier()
                A.cur = mark_persist
                ykr = Ring("ykr", [A.alloc([128, D], F32) for _ in range(12)])
                xsub = Ring("xsubc", [A.alloc([128, D], F32) for _ in range(4)])
                accr = Ring("accr", [A.alloc([128, D], F32) for _ in range(3)])
                junk = A.alloc([128, D], BF16)
                def issue_gathers(tt):
                    lst = []
                    for k in range(4):
                        yk, ykres = ykr.next()
                        S.op("dve", lambda e, yk=yk: e.memset(yk, 0.0), writes=[ykres])
                        S.op("pool", lambda e, yk=yk, tt=tt, k=k: e.indirect_dma_start(out=yk, out_offset=None, in_=ys_d[:, :],
                                                                                        in_offset=bass.IndirectOffsetOnAxis(ap=sloti[:, tt, k:k + 1], axis=0),
                                                                                        bounds_check=pool_regs["bc"], oob_is_err=False),
                             reads=[ykres], writes=[ykres], dma=ykres)
                        lst.append((yk, ykres))
                    return lst

                gq = {0: issue_gathers(0), 1: issue_gathers(1)}
                for tt in range(NT):
                    if tt + 2 < NT:
                        gq[tt + 2] = issue_gathers(tt + 2)
                    acc, accres = accr.next()
                    for k, (yk, ykres) in enumerate(gq.pop(tt)):
                        if k == 0:
                            S.op("dve", lambda e, yk=yk, acc=acc, tt=tt: e.tensor_scalar(out=acc, in0=yk, scalar1=w4all[:, tt, 0:1], scalar2=None, op0=ALU.mult),
                                 reads=[ykres], writes=[accres])
                        else:
                            S.op("dve", lambda e, yk=yk, acc=acc, tt=tt, k=k: e.scalar_tensor_tensor(out=acc, in0=yk, scalar=w4all[:, tt, k:k + 1], in1=acc, op0=ALU.mult, op1=ALU.add),
                                 reads=[ykres, accres], writes=[accres])
                    xa, xres = xsub.next()
                    r0 = tt * 128
                    S.op("sp", lambda e, xa=xa, r0=r0: e.dma_start(out=xa, in_=xs_d[r0:r0 + 128, :]), reads=["xs%d" % tt], writes=[xres], dma=xres)
                    S.op("dve", lambda e, acc=acc: e.tensor_tensor(out=acc, in0=acc, in1=GB, op=ALU.mult), reads=[accres, "GB"], writes=[accres])
                    S.op("dve", lambda e, acc=acc, xa=xa: e.tensor_tensor(out=xa, in0=xa, in1=acc, op=ALU.add), reads=[accres, xres], writes=[xres])
                    if do_final:
                        rsa, rres = rms_rstd(xa, xres, junk, "junk")
                        S.op("dve", lambda e, xa=xa, rsa=rsa: e.scalar_tensor_tensor(out=xa, in0=xa, scalar=rsa, in1=finalg, op0=ALU.mult, op1=ALU.mult),
                             reads=[xres, rres, "finalg"], writes=[xres])
                    S.op("sp", lambda e, xa=xa, r0=r0: e.dma_start(out=dst_d[r0:r0 + 128, :], in_=xa),
                         reads=[xres], writes=["%s%d" % (dst_name, tt), "XS_G"], dma="st_" + xres)

            first = True
            for li, l in enumerate(layers):
                layer_prologue(l)
                if do_mixer:
                    mixer_phase(l, x_in if first else xs_d, "xin" if first else "xs", xs_d if do_moe else out_d)
                    S.barrier()
                last = (li == len(layers) - 1)
                if do_moe:
                    (moe_sparse_phase if SPARSE else moe_phase)(l, out_d if last else xs_d, "out" if last else "xs", do_final=(final and last))
                    S.barrier()
                first = False
            return W

        Wd = program(DummySched(), None)
        S = Sched(nc)
        program(S, Wd.rec)
        S.emit()
        build.last_stats = {e: len(S.stream[e]) for e in S.ENGS}
    return nc


def _pp(v):
    return np.ascontiguousarray(v.reshape(-1, 128).T)


def _bc(v):
    return np.ascontiguousarray(np.broadcast_to(v[None, :], (128, v.shape[0])))


def _consts():
    ident = np.eye(128, dtype=np.float32)
    triu = np.triu(np.ones((128, 128), dtype=np.float32))
    rc = np.zeros((128, 4, 16), dtype=np.float32)
    for gi, w in enumerate(POOL_WINDOWS):
        rc[:, gi, :] = 1.0 / np.minimum(np.arange(16) + 1, w)
    return ident, triu, rc


def _consts2():
    ustr = np.triu(np.ones((128, 128), dtype=np.float32), k=1)
    ebase = np.ascontiguousarray(np.broadcast_to((np.arange(NE, dtype=np.float32) * CAP)[None, :], (128, NE)))
    tokid = (np.arange(32, dtype=np.int32)[None, :] * 128 + np.arange(128, dtype=np.int32)[:, None]).astype(np.int32)
    return {"ustr": ustr, "ebase": ebase, "tokid": np.ascontiguousarray(tokid)}


def layer_inputs(inp, l):
    f = lambda k: np.asarray(inp[k][l], dtype=np.float32)
    ada_b = f("ada_b")
    d = {
        "ada_w%d" % l: f("ada_w"),
        "ada_bP%d" % l: np.ascontiguousarray(ada_b.reshape(48, 128).T),
        "adab_g1B%d" % l: _bc(ada_b[2048:3072]),
        "adab_g2B%d" % l: _bc(ada_b[5120:6144]),
        "n1gP%d" % l: _pp(f("norm1_g")),
        "n2gP%d" % l: _pp(f("norm2_g")),
        "w_in%d" % l: f("w_in"),
        "gnormB%d" % l: _bc(f("gmlp_norm_g")),
        "ws%d" % l: f("gmlp_ws"),
        "bsB%d" % l: _bc(f("gmlp_bs").reshape(-1)),
        "w_proj_a%d" % l: f("w_proj_a"),
        "pool_w%d" % l: f("pool_w"),
        "pscP%d" % l: _pp(f("pool_scale")),
        "convP%d" % l: np.ascontiguousarray(f("conv_w").reshape(3, 8, 128).transpose(2, 0, 1)),
        "w_proj_c%d" % l: f("w_proj_c"),
        "w_out%d" % l: f("w_out"),
        "router_w%d" % l: f("router_w"),
        "rbB%d" % l: _bc(f("router_b")),
        "w_gu%d" % l: f("exp_w_gu"),
        "bguP%d" % l: np.ascontiguousarray(f("exp_b_gu").reshape(NE, 16, 128).transpose(2, 0, 1)),
        "w_down%d" % l: f("exp_w_down"),
        "b_down%d" % l: f("exp_b_down"),
    }
    return d


_NC_CACHE = {}


def _get_nc(key, **kw):
    if key not in _NC_CACHE:
        _NC_CACHE[key] = build(**kw)
    return _NC_CACHE[key]


FUSED = True


def kernel(**inputs):
    x = np.asarray(inputs["x"], dtype=np.float32)
    c = np.asarray(inputs["c"], dtype=np.float32)
    ident, triu, rc = _consts()
    n = x.shape[0]
    common = {"ident": ident, "triu": triu, "rc": rc, "finalgB": _bc(np.asarray(inputs["final_g"], dtype=np.float32))}
    common.update(_consts2())
    if FUSED:
        plan = [((0, 1), True)]
    else:
        plan = [((0,), False), ((1,), True)]
    cur = [np.ascontiguousarray(x[b]) for b in range(n)]
    for layers, fin in plan:
        nc = _get_nc((layers, fin), layers=layers, final=fin)
        shared = dict(common)
        for l in layers:
            shared.update(layer_inputs(inputs, l))
        in_maps = []
        for b in range(n):
            m = dict(shared)
            m["x"] = cur[b]
            m["cT"] = _pp(c[b])
            in_maps.append(m)
        res = run_bass_kernel_spmd(nc, in_maps, core_ids=list(range(n)))
        cur = [np.asarray(res.results[b]["out"], dtype=np.float32) for b in range(n)]
    return np.stack(cur, axis=0)
```
